# Optimizing a Trainium2 kernel written in Bass

```python
import jax, jax.numpy as jnp
from jax import lax
import numpy as np

D_MODEL = 1024
BATCH = 2
SEQ = 8192
DEPTH = 1

MIX_WIDTH = D_MODEL
HGRN_WIDTH = MIX_WIDTH // 2
CONV_WIDTH = MIX_WIDTH - HGRN_WIDTH
HGRN_HEAD_DIM = 128
HGRN_HEADS = HGRN_WIDTH // HGRN_HEAD_DIM
CONV_GROUPS = 4
CONV_GROUP_DIM = CONV_WIDTH // CONV_GROUPS
CONV_TAPS = 3
CHUNK = 64
IN_COLS = 4 * HGRN_WIDTH + 3 * CONV_WIDTH
PEER_HEADS = 8
PEER_NKEYS = 128
PEER_EXPERTS = PEER_NKEYS * PEER_NKEYS
PEER_QDIM = 256
PEER_HALF = PEER_QDIM // 2
PEER_TOPK = 16
TOKEN_BLOCK = 128
EPS = 1e-6

kernel_name = "hymba_hgrn2_shortconv_peer_adaln"


def rmsnorm(x, w):
    xf = x.astype(jnp.float32)
    y = xf * lax.rsqrt(jnp.mean(xf * xf, axis=-1, keepdims=True) + EPS)
    return (y * w.astype(jnp.float32)).astype(x.dtype)


def group_rmsnorm(x, w, groups):
    shp = x.shape
    xg = x.reshape(shp[:-1] + (groups, shp[-1] // groups))
    return rmsnorm(xg, w.reshape(groups, shp[-1] // groups)).reshape(shp)


def hgrn2_chunkwise(q, k, v, log_f):
    B, H, S, DK = q.shape
    DV = v.shape[-1]
    n = S // CHUNK

    def to_chunks(t):
        return t.reshape(B, H, n, CHUNK, t.shape[-1]).transpose(2, 0, 1, 3, 4)

    qc, kc, vc, gc = to_chunks(q), to_chunks(k), to_chunks(v), to_chunks(log_f)
    causal = jnp.tril(jnp.ones((CHUNK, CHUNK), dtype=bool))[:, :, None]

    def step(state, inp):
        qb, kb, vb, gb = inp
        b = jnp.cumsum(gb, axis=2)
        diff = b[:, :, :, None, :] - b[:, :, None, :, :]
        decay = jnp.exp(jnp.where(causal, diff, -jnp.inf))
        scores = jnp.einsum('bhtd,bhsd,bhtsd->bhts', qb, kb, decay)
        o = (jnp.einsum('bhts,bhsv->bhtv', scores, vb)
             + jnp.einsum('bhtd,bhdv->bhtv', qb * jnp.exp(b), state))
        b_last = b[:, :, -1:, :]
        new_state = (jnp.exp(b_last[:, :, 0, :])[..., None] * state
                     + jnp.einsum('bhsd,bhsv->bhdv', kb * jnp.exp(b_last - b), vb))
        return new_state, o

    s0 = jnp.zeros((B, H, DK, DV), jnp.float32)
    _, o = lax.scan(step, s0, (qc, kc, vc, gc))
    return o.transpose(1, 2, 0, 3, 4).reshape(B, H, S, DV)


def hgrn2_group(p, lb, onorm_w):
    B, S, _ = p.shape
    pf = p.astype(jnp.float32)
    q, f_raw, i, g = jnp.split(pf, 4, axis=-1)
    f = lb + (1.0 - lb) * jax.nn.sigmoid(f_raw)
    log_f = jnp.log(f)
    k = (1.0 - lb) * jax.nn.sigmoid(-f_raw)

    def heads(t):
        return t.reshape(B, S, HGRN_HEADS, HGRN_HEAD_DIM).transpose(0, 2, 1, 3)

    o = hgrn2_chunkwise(heads(q), heads(k), heads(i), heads(log_f))
    o = o.transpose(0, 2, 1, 3).reshape(B, S, HGRN_WIDTH)
    o = group_rmsnorm(o, onorm_w, HGRN_HEADS) * jax.nn.silu(g)
    return o.astype(p.dtype)


def short_conv_group(p, conv_w, onorm_w):
    b_gate, c_gate, xin = jnp.split(p, 3, axis=-1)
    u = c_gate * xin
    y = lax.conv_general_dilated(
        u, conv_w[:, None, :].astype(u.dtype), window_strides=(1,),
        padding=[(CONV_TAPS - 1, 0)], dimension_numbers=('NWC', 'WIO', 'NWC'),
        feature_group_count=CONV_WIDTH)
    y = b_gate * y
    return group_rmsnorm(y, onorm_w, CONV_GROUPS)


def peer(h, w_query, sub_keys, expert_u, expert_v):
    B, S, D = h.shape
    hb = h.reshape(B * S // TOKEN_BLOCK, TOKEN_BLOCK, D)

    def block(xb):
        T = xb.shape[0]
        q = (xb @ w_query).reshape(T, PEER_HEADS, 2, PEER_HALF)
        s = jnp.einsum('thpk,hpnk->thpn', q, sub_keys).astype(jnp.float32)
        top_s, top_i = lax.top_k(s, PEER_TOPK)
        cand_s = top_s[:, :, 0, :, None] + top_s[:, :, 1, None, :]
        cand_i = top_i[:, :, 0, :, None] * PEER_NKEYS + top_i[:, :, 1, None, :]
        best_s, best_pos = lax.top_k(cand_s.reshape(T, PEER_HEADS, PEER_TOPK * PEER_TOPK), PEER_TOPK)
        idx = jnp.take_along_axis(cand_i.reshape(T, PEER_HEADS, PEER_TOPK * PEER_TOPK), best_pos, axis=-1)
        gate = jax.nn.softmax(best_s, axis=-1)
        u = expert_u[idx]
        v = expert_v[idx]
        act = jax.nn.gelu(jnp.einsum('td,thkd->thk', xb, u).astype(jnp.float32), approximate=False)
        return jnp.einsum('thk,thkd->td', (gate * act).astype(xb.dtype), v)

    return lax.map(block, hb).reshape(B, S, D)


def setup_inputs(seed: int = 0) -> dict:
    key = jax.random.key(seed)
    ks = jax.random.split(key, 17)
    f32 = jnp.float32
    D = D_MODEL
    return {
        "x": jax.random.normal(ks[0], (BATCH, SEQ, D), f32),
        "c": jax.random.normal(ks[1], (BATCH, D), f32),
        "w_ada": jax.random.normal(ks[2], (DEPTH, D, 6 * D), f32) * (0.5 * D ** -0.5),
        "b_ada": jax.random.normal(ks[3], (DEPTH, 6 * D), f32) * 0.02,
        "norm1_w": 1.0 + 0.02 * jax.random.normal(ks[4], (DEPTH, D), f32),
        "w_in": jax.random.normal(ks[5], (DEPTH, D, IN_COLS), f32) * D ** -0.5,
        "hgrn_lb_logits": jax.random.normal(ks[6], (DEPTH + 1, HGRN_WIDTH), f32) * 0.5,
        "hgrn_onorm_w": 1.0 + 0.02 * jax.random.normal(ks[7], (DEPTH, HGRN_WIDTH), f32),
        "conv_w": jax.random.normal(ks[8], (DEPTH, CONV_TAPS, CONV_WIDTH), f32) * CONV_TAPS ** -0.5,
        "conv_onorm_w": 1.0 + 0.02 * jax.random.normal(ks[9], (DEPTH, CONV_WIDTH), f32),
        "w_out": jax.random.normal(ks[10], (DEPTH, MIX_WIDTH, D), f32) * MIX_WIDTH ** -0.5,
        "norm2_w": 1.0 + 0.02 * jax.random.normal(ks[11], (DEPTH, D), f32),
        "peer_w_query": jax.random.normal(ks[12], (DEPTH, D, PEER_HEADS * PEER_QDIM), f32) * D ** -0.5,
        "peer_sub_keys": jax.random.normal(ks[13], (DEPTH, PEER_HEADS, 2, PEER_NKEYS, PEER_HALF), f32) * PEER_HALF ** -0.5,
        "peer_u": jax.random.normal(ks[14], (DEPTH, PEER_EXPERTS, D), f32) * D ** -0.5,
        "peer_v": jax.random.normal(ks[15], (DEPTH, PEER_EXPERTS, D), f32),
        "final_norm_w": 1.0 + 0.02 * jax.random.normal(ks[16], (D,), f32),
    }


def reference(x, c, w_ada, b_ada, norm1_w, w_in, hgrn_lb_logits, hgrn_onorm_w, conv_w,
              conv_onorm_w, w_out, norm2_w, peer_w_query, peer_sub_keys, peer_u, peer_v,
              final_norm_w):
    lower_bounds = jnp.cumsum(jax.nn.softmax(hgrn_lb_logits.astype(jnp.float32), axis=0), axis=0)
    c_act = jax.nn.silu(c)
    for l in range(DEPTH):
        mod = (c_act @ w_ada[l] + b_ada[l])[:, None, :]
        shift1, scale1, gate1, shift2, scale2, gate2 = jnp.split(mod, 6, axis=-1)

        h = rmsnorm(x, norm1_w[l]) * (1.0 + scale1) + shift1
        proj = h @ w_in[l]
        y_a = hgrn2_group(proj[..., :4 * HGRN_WIDTH], lower_bounds[l], hgrn_onorm_w[l])
        y_b = short_conv_group(proj[..., 4 * HGRN_WIDTH:], conv_w[l], conv_onorm_w[l])
        mix = jnp.concatenate([y_a, y_b], axis=-1) @ w_out[l]
        x = x + gate1 * mix

        h = rmsnorm(x, norm2_w[l]) * (1.0 + scale2) + shift2
        x = x + gate2 * peer(h, peer_w_query[l], peer_sub_keys[l], peer_u[l], peer_v[l])
    return rmsnorm(x, final_norm_w)
```

```python
import contextlib
import numpy as np
import concourse.bass as bass
import concourse.mybir as mybir
from concourse.bass_utils import run_bass_kernel_spmd

F32 = mybir.dt.float32
BF16 = mybir.dt.bfloat16
ALU = mybir.AluOpType
AF = mybir.ActivationFunctionType
STRICT_SAME_ENGINE = True


class Buf:
    __slots__ = ("name", "w", "r", "sem", "cnt")

    def __init__(self, name):
        self.name = name
        self.w = None
        self.r = {}
        self.sem = None
        self.cnt = 0


class Op:
    __slots__ = ("eng", "fn", "deps", "flag", "val", "sem", "isdma", "name")

    def __init__(self, eng, fn, isdma=False, name=""):
        self.eng = eng
        self.fn = fn
        self.deps = []
        self.flag = False
        self.val = None
        self.sem = None
        self.isdma = isdma
        self.name = name


class Sched:
    ENGS = ("pe", "act", "dve", "pool", "sp")

    def __init__(self, nc, stack):
        self.nc = nc
        self.stack = stack
        self.q = {e: [] for e in self.ENGS}
        self.esem = {e: stack.enter_context(nc.semaphore("s_" + e)) for e in self.ENGS}
        self.nsem = len(self.ENGS)
        self.ndma = 0

    def newsem(self, name):
        self.nsem += 1
        return self.stack.enter_context(self.nc.semaphore(name))

    def _deps(self, op, reads, writes):
        deps = []
        for b in reads:
            if b.w is not None:
                deps.append(b.w)
        for b in writes:
            if b.w is not None:
                deps.append(b.w)
            deps.extend(b.r.values())
        seen = set()
        for d in deps:
            if d is op or id(d) in seen:
                continue
            seen.add(id(d))
            if (not d.isdma) and d.eng == op.eng and (op.eng == "pe" or not STRICT_SAME_ENGINE):
                continue
            op.deps.append(d)
        for b in reads:
            key = ("dma", id(op)) if op.isdma else op.eng
            b.r[key] = op
        for b in writes:
            b.w = op
            b.r = {}

    def emit(self, eng, fn, reads=(), writes=(), name=""):
        op = Op(eng, fn, name=name)
        self._deps(op, reads, writes)
        self.q[eng].append(op)
        return op

    def dma(self, eng, fn, sbuf, reads=(), writes=(), nparts=1, name=""):
        op = Op(eng, fn, isdma=True, name=name)
        self._deps(op, reads, writes)
        if sbuf.sem is None:
            sbuf.sem = self.newsem("d_" + sbuf.name)
        sbuf.cnt += 16 * nparts
        op.sem = sbuf.sem
        op.val = sbuf.cnt
        self.q[eng].append(op)
        self.ndma += nparts
        return op

    def finalize(self, final_ops):
        for e in self.ENGS:
            for op in self.q[e]:
                for d in op.deps:
                    if not d.isdma:
                        d.flag = True
        for e in self.ENGS:
            c = 0
            for op in self.q[e]:
                if not op.isdma and op.flag:
                    c += 1
                    op.val = c
                    op.sem = self.esem[e]
        self.final_ops = final_ops

    def replay(self, eng, e):
        seen = {}
        nwait = 0
        for op in self.q[eng]:
            for d in op.deps:
                k = id(d.sem)
                if seen.get(k, 0) >= d.val:
                    continue
                seen[k] = d.val
                e.wait_ge(d.sem, d.val)
                nwait += 1
            r = op.fn(e)
            if op.isdma:
                lst = r if isinstance(r, (list, tuple)) else [r]
                for ins in lst:
                    ins.then_inc(op.sem, 16)
            elif op.flag:
                r.then_inc(op.sem, 1)
        if eng == "sp":
            for d in self.final_ops:
                e.wait_ge(d.sem, d.val)
        return nwait

    def run(self):
        nc = self.nc
        with nc.Block() as block:
            @block.sync
            def _(e):
                self.replay("sp", e)

            @block.scalar
            def _(e):
                self.replay("act", e)

            @block.vector
            def _(e):
                self.replay("dve", e)

            @block.gpsimd
            def _(e):
                self.replay("pool", e)

            @block.tensor
            def _(e):
                self.replay("pe", e)


def ap_of(t):
    return t if isinstance(t, bass.AP) else t[:]


def mk(base, offset_elems, dims):
    b = ap_of(base)
    pstep, pcnt = b.ap[0]
    return bass.AP(b.tensor, b.offset + offset_elems, [[pstep, pcnt]] + [list(d) for d in dims])

D = 1024
TOK = 2048
NPRE = 3
EPS = 1e-6
NEG = -1.0e30


def build_nc(npre=NPRE, dbg=False, do_peer=True, ngroups=16):
    nc = bass.Bass("TRN2", target_bir_lowering=False)

    def din(name, shape):
        return nc.dram_tensor(name, shape, F32, kind="ExternalInput").ap()

    x_d = din("x", [TOK, D])
    xpre_d = din("xpre", [max(npre, 1) * TOK, D])
    msk_d = din("msk", [128, 4])
    cT_d = din("cT", [128, 8])
    wada_d = din("wada", [128, 8, 6 * D])
    bada_d = din("bada", [1, 6 * D])
    n1w_d = din("n1w", [1, D])
    n2w_d = din("n2w", [1, D])
    fw_d = din("fw", [1, D])
    win_d = din("win", [128, 8, 3584])
    lbl_d = din("lbl", [128, 2, 4])
    onw_d = din("onw", [128, 4])
    cnw_d = din("cnw", [128, 4])
    cw_d = din("cw", [128, 3, 4])
    wout_d = din("wout", [128, 8, D])
    wq_d = din("wq", [16, 128, 8, 128])
    skt_d = din("skt", [128, 16, 128])
    ut_d = din("ut", [128, 128, 8, 128])
    v_d = din("v", [16384, D])
    out_d = nc.dram_tensor("out", [TOK, D], F32, kind="ExternalOutput").ap()
    x1s_d = nc.dram_tensor("x1s", [TOK, D], F32, kind=("ExternalOutput" if dbg else "Internal")).ap()
    uts_d = nc.dram_tensor("uts", [128, 128, 8, 128], BF16, kind="Internal").ap()
    vs_d = nc.dram_tensor("vs", [16384, D], BF16, kind="Internal").ap()
    wqs_d = nc.dram_tensor("wqs", [16, 128, 8, 128], BF16, kind="Internal").ap()

    with contextlib.ExitStack() as st:
        S = Sched(nc, st)

        def sb(name, shape, dt=F32):
            return st.enter_context(nc.sbuf_tensor("t_" + name, shape, dt))

        def ps(name, shape, dt=F32):
            return st.enter_context(nc.psum_tensor("t_" + name, shape, dt))

        def E(eng, fn, r=(), w=()):
            return S.emit(eng, fn, reads=r, writes=w)

        dumps = []

        def dump(name, ap, shape, buf, dt=F32):
            if not dbg:
                return
            dd = nc.dram_tensor("dbg_" + name, shape, dt, kind="ExternalOutput").ap()
            dumps.append(S.dma("sp", lambda e: e.dma_start(out=dd, in_=ap), buf, reads=[buf]))

        pT0 = ps("pT0", [128, 1024], BF16); bT0 = Buf("pT0")
        pT1 = ps("pT1", [128, 1024], BF16); bT1 = Buf("pT1")
        PA = ps("PA", [128, 1024], F32); bA = [Buf("PA0"), Buf("PA1")]
        PB = ps("PB", [128, 1024], F32); bB = [Buf("PB0"), Buf("PB1")]
        PC = ps("PC", [128, 1024], F32); bC = [Buf("PC0"), Buf("PC1")]

        def bank(P, i):
            return P[:, i * 512:(i + 1) * 512]

        identf = sb("identf", [128, 128]); ident = sb("ident", [128, 128], BF16); bId = Buf("ident")
        E("pool", lambda e: e.memset(identf[:], 1.0), w=[bId])
        E("pool", lambda e: e.affine_select(out=identf[:], in_=identf[:], pattern=[[-1, 128]], compare_op=ALU.is_equal, fill=0.0, base=0, channel_multiplier=1), r=[bId], w=[bId])
        E("pool", lambda e: e.tensor_copy(out=ident[:], in_=identf[:]), r=[bId], w=[bId])
        onesf = sb("onesf", [128, 128]); ones_bf = sb("ones_bf", [128, 128], BF16); bOn = Buf("ones")
        E("pool", lambda e: e.memset(onesf[:], 1.0), w=[bOn])
        E("pool", lambda e: e.tensor_copy(out=ones_bf[:], in_=onesf[:]), r=[bOn], w=[bOn])
        maskST = sb("maskST", [128, 128]); bMk = Buf("maskST")
        E("pool", lambda e: e.memset(maskST[:], 1.0), w=[bMk])
        E("pool", lambda e: e.affine_select(out=maskST[:], in_=maskST[:], pattern=[[1, 128]], compare_op=ALU.is_ge, fill=0.0, base=0, channel_multiplier=-1), r=[bMk], w=[bMk])
        maskI = sb("maskI", [128, 128], mybir.dt.int32)
        E("pool", lambda e: e.tensor_copy(out=maskI[:], in_=maskST[:]), r=[bMk], w=[bMk])

        small = {}

        def load_small(name, src, shape):
            t = sb(name, shape); b = Buf(name)
            S.dma("sp", lambda e: e.dma_start(out=t[:], in_=src), b, writes=[b])
            small[name] = (t, b)
            return t, b

        msk, bMsk = load_small("msk", msk_d, [128, 4])
        cT, bcT = load_small("cT", cT_d, [128, 8])
        lbl, bLbl = load_small("lbl", lbl_d, [128, 2, 4])
        onw, bOnw = load_small("onw", onw_d, [128, 4])
        cnw, bCnw = load_small("cnw", cnw_d, [128, 4])
        cw, bCw = load_small("cw", cw_d, [128, 3, 4])

        lb = sb("lb", [128, 4]); oml = sb("oml", [128, 4]); noml = sb("noml", [128, 4]); bLb = Buf("lb")
        E("dve", lambda e: e.tensor_tensor(out=lb[:], in0=lbl[:, 0, :], in1=lbl[:, 1, :], op=ALU.subtract), r=[bLbl], w=[bLb])
        E("act", lambda e: e.activation(out=lb[:], in_=lb[:], func=AF.Sigmoid), r=[bLb], w=[bLb])
        E("dve", lambda e: e.tensor_scalar(out=oml[:], in0=lb[:], scalar1=-1.0, scalar2=1.0, op0=ALU.mult, op1=ALU.add), r=[bLb], w=[bLb])
        E("dve", lambda e: e.tensor_scalar(out=noml[:], in0=lb[:], scalar1=-1.0, scalar2=None, op0=ALU.add), r=[bLb], w=[bLb])

        g2b = sb("g2b", [128, D]); fwb = sb("fwb", [128, D]); sh2b = sb("sh2b", [128, D]); gt2b = sb("gt2b", [128, D])
        bG2 = Buf("g2b"); bFw = Buf("fwb"); bSh2 = Buf("sh2b"); bGt2 = Buf("gt2b")
        junk = sb("junk", [128, D]); bJunk = Buf("junk")
        ssq = sb("ssq", [128, 1]); bSsq = Buf("ssq")
        hb = sb("hb", [128, D], BF16); bHb = Buf("hb")

        def norm_mod(xap, bX, gb, bG, shiftap, bShift):
            E("dve", lambda e: e.memset(ssq[:], 0.0), w=[bSsq])
            E("act", lambda e: e.activation(out=junk[:], in_=xap, func=AF.Square, accum_out=ssq[:, 0:1]), r=[bX, bSsq], w=[bJunk, bSsq])
            E("dve", lambda e: e.tensor_scalar(out=ssq[:], in0=ssq[:], scalar1=1.0 / D, scalar2=EPS, op0=ALU.mult, op1=ALU.add), r=[bSsq], w=[bSsq])
            E("act", lambda e: e.activation(out=ssq[:], in_=ssq[:], func=AF.Sqrt), r=[bSsq], w=[bSsq])
            E("dve", lambda e: e.reciprocal(out=ssq[:], in_=ssq[:]), r=[bSsq], w=[bSsq])
            E("dve", lambda e: e.scalar_tensor_tensor(out=junk[:], in0=xap, scalar=ssq[:, 0:1], in1=gb[:], op0=ALU.mult, op1=ALU.mult), r=[bX, bSsq, bG, bJunk], w=[bJunk])
            E("pool", lambda e: e.tensor_tensor(out=hb[:], in0=junk[:], in1=shiftap, op=ALU.add), r=[bJunk, bShift], w=[bHb])

        def transpose_hb(dst3, bDst):
            for c in range(8):
                E("pe", lambda e, c=c: e.transpose(out=pT0[:, c * 128:(c + 1) * 128], in_=hb[:, c * 128:(c + 1) * 128], identity=ident[:]), r=[bHb, bId], w=[bT0])
            E("act", lambda e: e.copy(out=dst3, in_=pT0[:].rearrange("p (c t) -> p c t", c=8)), r=[bT0], w=[bDst])

        with contextlib.ExitStack() as st2:
            def sb2(name, shape, dt=F32):
                return st2.enter_context(nc.sbuf_tensor("t_" + name, shape, dt))
            win = sb2("win", [128, 8, 3584], BF16); bWin = Buf("win")
            for c in range(8):
                S.dma("pool", lambda e, c=c: e.dma_start(out=win[:, c, :], in_=win_d[:, c, :]), bWin, writes=[bWin])
            wout = sb2("wout", [128, 8, D], BF16); bWout = Buf("wout")
            for c in range(0, 8, 4):
                S.dma("pool", lambda e, c=c: e.dma_start(out=wout[:, c:c + 4, :], in_=wout_d[:, c:c + 4, :]), bWout, writes=[bWout])
            bUs = [Buf("uts%d" % i) for i in range(16)]; bVs = [Buf("vs%d" % i) for i in range(16)]
            xt = sb2("xt", [128, 4, D]); bXt = [Buf("xt%d" % i) for i in range(4)]
            h1T = sb2("h1T", [128, 8, 512], BF16); bH1T = [Buf("h1T%d" % i) for i in range(4)]
            itok = sb2("itok", [128, 4, 512], BF16); bItok = [Buf("itok%d" % i) for i in range(4)]
            sg = sb2("sg", [128, 512]); bSg = Buf("sg")
            lf = sb2("lf", [128, 512]); bLf = Buf("lf")
            kk = sb2("kk", [128, 512]); bKk = Buf("kk")
            qs = sb2("qs", [128, 512]); bQs = Buf("qs")
            gs = sb2("gs", [128, 512]); bGs = Buf("gs")
            bt = sb2("bt", [128, 128]); bBt = Buf("bt")
            nbm = sb2("nbm", [128, 1]); bNbm = Buf("nbm")
            eb = sb2("eb", [128, 1]); bEb = Buf("eb")
            ex = [sb2("ex%d" % i, [128, 128]) for i in range(4)]; bEx = [Buf("ex%d" % i) for i in range(4)]
            qt = sb2("qt", [128, 128], BF16); bQt = Buf("qt")
            kt = sb2("kt", [128, 128], BF16); bKt = Buf("kt")
            qh = sb2("qh", [128, 128], BF16); bQh = Buf("qh")
            khT = sb2("khT", [128, 128], BF16); bKhT = Buf("khT")
            kh = sb2("kh", [128, 128], BF16); bKh = Buf("kh")
            scm = sb2("scm", [128, 128], BF16); bScm = Buf("scm")
            Sst = sb2("Sst", [128, 4, 128]); bS = [Buf("S%d" % i) for i in range(4)]
            Sbf = sb2("Sbf", [128, 4, 128], BF16); bSbf = [Buf("Sbf%d" % i) for i in range(4)]
            sq = sb2("sq", [128, 512], BF16); bSq = Buf("sq")
            rb = sb2("rb", [128, 512]); bRb = Buf("rb")
            yv = sb2("yv", [128, 512]); bYv = Buf("yv")
            ymT = sb2("ymT", [128, 8, 512], BF16); bYm = [Buf("ym%d" % i) for i in range(8)]
            u = sb2("u", [128, 4, 514]); bU = [Buf("u%d" % i) for i in range(4)]
            csb = sg; bCsb = bSg
            y0 = lf; bY0 = bLf
            y1 = kk; bY1 = bKk
            t1 = junk; bTt1 = bJunk

            modb = sb2("modb", [128, 6 * D]); bMod = Buf("modb")
            cact = sb2("cact", [128, 8]); crep = sb2("crep", [128, 8, 128]); bCrep = Buf("crep")
            E("act", lambda e: e.activation(out=cact[:], in_=cT[:], func=AF.Silu), r=[bcT], w=[bCrep])
            for c in range(8):
                E("dve", lambda e, c=c: e.tensor_scalar(out=crep[:, c, :], in0=onesf[:], scalar1=cact[:, c:c + 1], scalar2=None, op0=ALU.mult), r=[bOn, bCrep], w=[bCrep])
            S.dma("sp", lambda e: e.dma_start(out=modb[:], in_=bada_d.partition_broadcast(128)), bMod, writes=[bMod])
            wst = [xt[:, 0:2, :].rearrange("p a (b n) -> p (a b) n", n=256), xt[:, 2:4, :].rearrange("p a (b n) -> p (a b) n", n=256)]
            bWst = [[bXt[0], bXt[1]], [bXt[2], bXt[3]]]
            for g in range(24):
                k = g % 2
                S.dma("sp", lambda e, g=g, k=k: e.dma_start(out=wst[k], in_=wada_d[:, :, g * 256:(g + 1) * 256]), bWst[k][0], writes=bWst[k])
                for c in range(8):
                    E("pe", lambda e, c=c, k=k: e.matmul(bank(PA, k)[:, 0:256], lhsT=crep[:, c, :], rhs=wst[k][:, c, :], start=(c == 0), stop=(c == 7)), r=[bCrep] + bWst[k], w=[bA[k]])
                E("dve", lambda e, g=g, k=k: e.tensor_tensor(out=modb[:, g * 256:(g + 1) * 256], in0=bank(PA, k)[:, 0:256], in1=modb[:, g * 256:(g + 1) * 256], op=ALU.add), r=[bA[k], bMod], w=[bMod])
            g1b = sb2("g1b", [128, D]); bG1 = Buf("g1b")
            S.dma("sp", lambda e: e.dma_start(out=g1b[:], in_=n1w_d.partition_broadcast(128)), bG1, writes=[bG1])
            S.dma("sp", lambda e: e.dma_start(out=g2b[:], in_=n2w_d.partition_broadcast(128)), bG2, writes=[bG2])
            S.dma("sp", lambda e: e.dma_start(out=fwb[:], in_=fw_d.partition_broadcast(128)), bFw, writes=[bFw])
            E("dve", lambda e: e.scalar_tensor_tensor(out=g1b[:], in0=modb[:, D:2 * D], scalar=1.0, in1=g1b[:], op0=ALU.add, op1=ALU.mult), r=[bMod, bG1], w=[bG1])
            E("dve", lambda e: e.scalar_tensor_tensor(out=g2b[:], in0=modb[:, 4 * D:5 * D], scalar=1.0, in1=g2b[:], op0=ALU.add, op1=ALU.mult), r=[bMod, bG2], w=[bG2])
            shift1 = modb[:, 0:D]; gate1 = modb[:, 2 * D:3 * D]
            dump("modb", modb[:], [128, 6 * D], bMod)
            dump("g1b", g1b[:], [128, D], bG1)
            dump("lb", lb[:], [128, 4], bLb)
            E("dve", lambda e: e.tensor_copy(out=sh2b[:], in_=modb[:, 3 * D:4 * D]), r=[bMod], w=[bSh2])
            E("dve", lambda e: e.tensor_copy(out=gt2b[:], in_=modb[:, 5 * D:6 * D]), r=[bMod], w=[bGt2])

            bWqs = [Buf("wqs%d" % i) for i in range(2)]
            if do_peer:
                for i in range(2):
                    S.dma("pool", lambda e, i=i: e.dma_start(out=wqs_d[i * 8:(i + 1) * 8], in_=wq_d[i * 8:(i + 1) * 8]), bWqs[i], reads=[bSh2, bGt2, bG1], writes=[bWqs[i]])
                for i in range(16):
                    S.dma("pool", lambda e, i=i: e.dma_start(out=uts_d[i * 8:(i + 1) * 8], in_=ut_d[i * 8:(i + 1) * 8]), bUs[i], reads=[bSh2, bGt2, bG1], writes=[bUs[i]])
                    S.dma("pool", lambda e, i=i: e.dma_start(out=vs_d[i * 1024:(i + 1) * 1024, :], in_=v_d[i * 1024:(i + 1) * 1024, :]), bVs[i], reads=[bSh2, bGt2, bG1], writes=[bVs[i]])
            E("pool", lambda e: e.memset(scm[:], 0.0), w=[bScm])
            for h in range(4):
                E("dve", lambda e, h=h: e.memset(Sst[:, h, :], 0.0), w=[bS[h]])
                E("pool", lambda e, h=h: e.memset(Sbf[:, h, :], 0.0), w=[bSbf[h]])
                E("pool", lambda e, h=h: e.memset(u[:, h, 0:2], 0.0), w=[bU[h]])

            rot = [0]

            def projbank():
                k = rot[0] % 2
                rot[0] += 1
                return k

            def proj_fm(col0, n):
                k = projbank()
                for c in range(8):
                    E("pe", lambda e, c=c, k=k: e.matmul(bank(PA, k)[:, 0:n], lhsT=win[:, c, col0:col0 + 128], rhs=h1T[:, c, 0:n], start=(c == 0), stop=(c == 7)), r=[bWin] + bH1T, w=[bA[k]])
                return bank(PA, k)[:, 0:n], bA[k]

            def hgrn_head(h, ntile, mode):
                n = ntile * 128
                fp, bf_ = proj_fm(512 + h * 128, n)
                E("act", lambda e: e.activation(out=sg[:, 0:n], in_=fp, func=AF.Sigmoid), r=[bf_], w=[bSg])
                E("act", lambda e: e.activation(out=lf[:, 0:n], in_=sg[:, 0:n], func=AF.Ln, scale=oml[:, h:h + 1], bias=lb[:, h:h + 1]), r=[bSg, bLb], w=[bLf])
                E("dve", lambda e: e.tensor_scalar(out=kk[:, 0:n], in0=sg[:, 0:n], scalar1=noml[:, h:h + 1], scalar2=oml[:, h:h + 1], op0=ALU.mult, op1=ALU.add), r=[bSg, bLb], w=[bKk])
                if mode == "main":
                    qp, bq_ = proj_fm(h * 128, n)
                    E("act", lambda e: e.copy(out=qs[:, 0:n], in_=qp), r=[bq_], w=[bQs])
                    gp, bg_ = proj_fm(1536 + h * 128, n)
                    E("act", lambda e: e.activation(out=gs[:, 0:n], in_=gp, func=AF.Silu), r=[bg_], w=[bGs])
                def do_chunk(ck):
                    cs = slice(ck * 128, (ck + 1) * 128)
                    E("dve", lambda e: e.tensor_tensor_scan(out=bt[:], data0=onesf[:], data1=lf[:, cs], initial=0.0, op0=ALU.mult, op1=ALU.add), r=[bOn, bLf], w=[bBt])
                    E("act", lambda e: e.activation(out=ex[3][:], in_=bt[:], func=AF.Exp, scale=-1.0, bias=bt[:, 127:128]), r=[bBt], w=[bEx[3]])
                    E("dve", lambda e: e.tensor_tensor(out=khT[:], in0=kk[:, cs], in1=ex[3][:], op=ALU.mult), r=[bKk, bEx[3]], w=[bKhT])
                    E("pe", lambda e: e.transpose(out=pT1[:, 0:128], in_=khT[:], identity=ident[:]), r=[bKhT, bId], w=[bT1])
                    E("act", lambda e: e.copy(out=kh[:], in_=pT1[:, 0:128]), r=[bT1], w=[bKh])
                    isl = itok[:, ck, h * 128:(h + 1) * 128]
                    if mode == "main":
                        E("dve", lambda e: e.tensor_scalar(out=nbm[:], in0=bt[:, 63:64], scalar1=-1.0, scalar2=None, op0=ALU.mult), r=[bBt], w=[bNbm])
                        E("act", lambda e: e.activation(out=ex[0][:], in_=bt[:], func=AF.Exp, bias=nbm[:, 0:1]), r=[bBt, bNbm], w=[bEx[0]])
                        E("act", lambda e: e.activation(out=ex[1][:], in_=bt[:], func=AF.Exp, scale=-1.0, bias=bt[:, 63:64]), r=[bBt], w=[bEx[1]])
                        E("act", lambda e: e.activation(out=ex[2][:], in_=bt[:], func=AF.Exp), r=[bBt], w=[bEx[2]])
                        E("dve", lambda e: e.tensor_tensor(out=qt[:], in0=qs[:, cs], in1=ex[0][:], op=ALU.mult), r=[bQs, bEx[0]], w=[bQt])
                        E("pool", lambda e: e.tensor_tensor(out=kt[:], in0=kk[:, cs], in1=ex[1][:], op=ALU.mult), r=[bKk, bEx[1]], w=[bKt])
                        E("pool", lambda e: e.tensor_tensor(out=qh[:], in0=qs[:, cs], in1=ex[2][:], op=ALU.mult), r=[bQs, bEx[2]], w=[bQh])
                        E("pe", lambda e: e.matmul(bank(PB, 0)[:, 0:128], lhsT=kt[:], rhs=qt[:], start=True, stop=True), r=[bKt, bQt], w=[bB[0]])
                        E("dve", lambda e: e.copy_predicated(out=scm[:], mask=maskI[:], data=bank(PB, 0)[:, 0:128]), r=[bB[0], bMk, bScm], w=[bScm])
                        E("pe", lambda e: e.matmul(bank(PB, 1)[:, cs], lhsT=isl, rhs=scm[:], start=True, stop=False), r=[bItok[ck], bScm], w=[bB[1]])
                        E("pe", lambda e: e.matmul(bank(PB, 1)[:, cs], lhsT=Sbf[:, h, :], rhs=qh[:], start=False, stop=True), r=[bSbf[h], bQh], w=[bB[1]])
                    E("pe", lambda e: e.matmul(bank(PC, 0)[:, 0:128], lhsT=kh[:], rhs=isl, start=True, stop=True), r=[bKh, bItok[ck]], w=[bC[0]])
                    E("act", lambda e: e.activation(out=eb[:], in_=bt[:, 127:128], func=AF.Exp), r=[bBt], w=[bEb])
                    E("dve", lambda e: e.scalar_tensor_tensor(out=Sst[:, h, :], in0=Sst[:, h, :], scalar=eb[:, 0:1], in1=bank(PC, 0)[:, 0:128], op0=ALU.mult, op1=ALU.add), r=[bS[h], bEb, bC[0]], w=[bS[h]])
                    if mode == "main":
                        E("pool", lambda e: e.tensor_copy(out=Sbf[:, h, :], in_=Sst[:, h, :]), r=[bS[h]], w=[bSbf[h]])
                for ck in range(ntile):
                    do_chunk(ck)
                if mode == "main":
                    groupnorm_out(bank(PB, 1), bB[1], onw[:, h:h + 1], bOnw, h, mulgs=True)

            def groupnorm_out(src, bSrc, wcol, bW, slot, mulgs):
                E("act", lambda e: e.activation(out=sq[:], in_=src, func=AF.Square), r=[bSrc], w=[bSq])
                E("pe", lambda e: e.matmul(bank(PC, 1), lhsT=ones_bf[:], rhs=sq[:], start=True, stop=True), r=[bOn, bSq], w=[bC[1]])
                E("dve", lambda e: e.tensor_scalar(out=rb[:], in0=bank(PC, 1), scalar1=1.0 / 128, scalar2=EPS, op0=ALU.mult, op1=ALU.add), r=[bC[1]], w=[bRb])
                E("act", lambda e: e.activation(out=rb[:], in_=rb[:], func=AF.Sqrt), r=[bRb], w=[bRb])
                E("dve", lambda e: e.reciprocal(out=rb[:], in_=rb[:]), r=[bRb], w=[bRb])
                if mulgs:
                    E("dve", lambda e: e.scalar_tensor_tensor(out=yv[:], in0=src, scalar=wcol, in1=rb[:], op0=ALU.mult, op1=ALU.mult), r=[bSrc, bW, bRb], w=[bYv])
                    E("pool", lambda e: e.tensor_tensor(out=ymT[:, slot, :], in0=yv[:], in1=gs[:], op=ALU.mult), r=[bYv, bGs], w=[bYm[slot]])
                else:
                    E("dve", lambda e: e.scalar_tensor_tensor(out=ymT[:, slot, :], in0=src, scalar=wcol, in1=rb[:], op0=ALU.mult, op1=ALU.mult), r=[bSrc, bW, bRb], w=[bYm[slot]])

            def conv_group(g, mode):
                if mode == "halo":
                    cp, bc_ = proj_fm(2560 + g * 128, 128)
                    E("act", lambda e: e.copy(out=csb[:, 0:128], in_=cp), r=[bc_], w=[bCsb])
                    xp, bx_ = proj_fm(3072 + g * 128, 128)
                    E("dve", lambda e: e.tensor_tensor(out=y0[:, 0:128], in0=csb[:, 0:128], in1=xp, op=ALU.mult), r=[bCsb, bx_], w=[bY0])
                    E("dve", lambda e: e.tensor_scalar(out=u[:, g, 0:2], in0=y0[:, 126:128], scalar1=msk[:, 3:4], scalar2=None, op0=ALU.mult), r=[bY0, bMsk], w=[bU[g]])
                    return
                cp, bc_ = proj_fm(2560 + g * 128, 512)
                E("act", lambda e: e.copy(out=csb[:], in_=cp), r=[bc_], w=[bCsb])
                xp, bx_ = proj_fm(3072 + g * 128, 512)
                E("dve", lambda e: e.tensor_tensor(out=u[:, g, 2:514], in0=csb[:], in1=xp, op=ALU.mult), r=[bCsb, bx_], w=[bU[g]])
                bp, bb_ = proj_fm(2048 + g * 128, 512)
                E("dve", lambda e: e.tensor_scalar(out=y0[:], in0=u[:, g, 2:514], scalar1=cw[:, 2, g:g + 1], scalar2=None, op0=ALU.mult), r=[bU[g], bCw], w=[bY0])
                E("dve", lambda e: e.scalar_tensor_tensor(out=y1[:], in0=u[:, g, 1:513], scalar=cw[:, 1, g:g + 1], in1=y0[:], op0=ALU.mult, op1=ALU.add), r=[bU[g], bCw, bY0], w=[bY1])
                E("dve", lambda e: e.scalar_tensor_tensor(out=y0[:], in0=u[:, g, 0:512], scalar=cw[:, 0, g:g + 1], in1=y1[:], op0=ALU.mult, op1=ALU.add), r=[bU[g], bCw, bY1], w=[bY0])
                E("dve", lambda e: e.tensor_tensor(out=y1[:], in0=y0[:], in1=bp, op=ALU.mult), r=[bY0, bb_], w=[bY1])
                E("pool", lambda e: e.tensor_copy(out=u[:, g, 0:2], in_=u[:, g, 512:514]), r=[bU[g]], w=[bU[g]])
                groupnorm_out(y1[:], bY1, cnw[:, g:g + 1], bCnw, 4 + g, mulgs=False)

            def mixer_macro(src_d, row0, ntile, mode):
                n = ntile * 128
                for tt in range(ntile):
                    S.dma("sp", lambda e, tt=tt: e.dma_start(out=xt[:, tt, :], in_=src_d[row0 + tt * 128: row0 + (tt + 1) * 128, :]), bXt[tt], writes=[bXt[tt]])
                    norm_mod(xt[:, tt, :], bXt[tt], g1b, bG1, shift1, bMod)
                    transpose_hb(h1T[:, :, tt * 128:(tt + 1) * 128], bH1T[tt])
                if mode == "halo":
                    for g in range(4):
                        conv_group(g, "halo")
                    return
                for tt in range(ntile):
                    k = projbank()
                    for c in range(8):
                        E("pe", lambda e, c=c, k=k, tt=tt: e.matmul(bank(PA, k), lhsT=h1T[:, c, tt * 128:(tt + 1) * 128], rhs=win[:, c, 1024:1536], start=(c == 0), stop=(c == 7)), r=[bWin, bH1T[tt]], w=[bA[k]])
                    E("act", lambda e, k=k, tt=tt: e.copy(out=itok[:, tt, :], in_=bank(PA, k)), r=[bA[k]], w=[bItok[tt]])
                for h in range(4):
                    hgrn_head(h, ntile, mode)
                if mode != "main":
                    return
                for g in range(4):
                    conv_group(g, "main")
                for tt in range(ntile):
                    for half in range(2):
                        for cc in range(8):
                            E("pe", lambda e, cc=cc, half=half, tt=tt: e.matmul(bank(PA, half), lhsT=ymT[:, cc, tt * 128:(tt + 1) * 128], rhs=wout[:, cc, half * 512:(half + 1) * 512], start=(cc == 0), stop=(cc == 7)), r=[bYm[cc], bWout], w=[bA[half]])
                    for half in range(2):
                        hs = slice(half * 512, (half + 1) * 512)
                        E("dve", lambda e, half=half, hs=hs: e.tensor_tensor(out=t1[:, hs], in0=bank(PA, half), in1=gate1[:, hs], op=ALU.mult), r=[bA[half], bMod, bTt1], w=[bTt1])
                    E("pool", lambda e, tt=tt: e.tensor_tensor(out=xt[:, tt, :], in0=t1[:], in1=xt[:, tt, :], op=ALU.add), r=[bTt1, bXt[tt]], w=[bXt[tt]])
                    S.dma("sp", lambda e, tt=tt: e.dma_start(out=x1s_d[row0 + tt * 128: row0 + (tt + 1) * 128, :], in_=xt[:, tt, :]), bXt[tt], reads=[bXt[tt]], writes=[bX1s[(row0 + tt * 128) // 128]])

            bX1s = [Buf("x1s%d" % i) for i in range(16)]
            for seg in range(npre):
                for m in range(4):
                    mixer_macro(xpre_d, seg * TOK + m * 512, 4, "pre")
                for h in range(4):
                    E("dve", lambda e, h=h, seg=seg: e.tensor_scalar(out=Sst[:, h, :], in0=Sst[:, h, :], scalar1=msk[:, seg:seg + 1], scalar2=None, op0=ALU.mult), r=[bS[h], bMsk], w=[bS[h]])
            if npre > 0:
                mixer_macro(xpre_d, npre * TOK - 128, 1, "halo")
            for h in range(4):
                E("pool", lambda e, h=h: e.tensor_copy(out=Sbf[:, h, :], in_=Sst[:, h, :]), r=[bS[h]], w=[bSbf[h]])
            for m in range(4):
                mixer_macro(x_d, m * 512, 4, "main")
                if m == 0:
                    dump("ymT", ymT[:], [128, 8, 512], bYm[7], BF16)
                    dump("h1T", h1T[:], [128, 8, 512], bH1T[3], BF16)
                    dump("itok", itok[:], [128, 4, 512], bItok[3], BF16)
                    dump("Sst", Sst[:], [128, 4, 128], bS[3])
                    dump("u", u[:], [128, 4, 514], bU[3])
            mixer_tail_bufs = [bWin, bWout] + bXt + bH1T + bItok + [bSg, bLf, bKk, bQs, bGs, bBt, bNbm, bEb] + bEx + [bQt, bKt, bQh, bKhT, bKh, bScm] + bS + bSbf + [bSq, bRb, bYv] + bYm + bU + [bMod, bG1, bCrep]

        final_ops = []
        with contextlib.ExitStack() as st3:
            def sb3(name, shape, dt=F32):
                return st3.enter_context(nc.sbuf_tensor("t_" + name, shape, dt))
            bFence = Buf("fence")
            E("dve", lambda e: e.memset(ssq[:], 0.0), w=mixer_tail_bufs + [bFence, bSsq])
            E("act", lambda e: e.copy(out=junk[:, 0:1], in_=ssq[:]), r=[bFence, bSsq], w=[bFence, bJunk])
            E("pool", lambda e: e.tensor_copy(out=junk[:, 1:2], in_=ssq[:]), r=[bFence, bSsq], w=[bFence, bJunk])
            E("pe", lambda e: e.transpose(out=pT0[:, 0:128], in_=ident[:], identity=ident[:]), r=[bFence, bId], w=[bFence, bT0])
            S.dma("sp", lambda e: e.dma_start(out=msk[:], in_=msk_d), bMsk, reads=[bFence], writes=[bFence, bMsk])
            S.dma("pool", lambda e: e.dma_start(out=msk[:], in_=msk_d), bMsk, reads=[bFence], writes=[bFence, bMsk])

            skt = sb3("skt", [128, 16, 128], BF16); bSkt = Buf("skt")
            S.dma("pool", lambda e: e.dma_start(out=skt[:], in_=skt_d), bSkt, reads=[bFence], writes=[bSkt])
            wqb = [sb3("wqb%d" % i, [128, 8, 128], BF16) for i in range(2)]; bWq = [Buf("wqb%d" % i) for i in range(2)]
            x1t = sb3("x1t", [128, D]); bX1 = Buf("x1t")
            h2T = sb3("h2T", [128, 8, 128], BF16); bH2T = Buf("h2T")
            qT = sb3("qT", [128, 16, 128], BF16); bQT = Buf("qT")
            ssb = sb3("ssb", [128, 16, 128]); bSs = Buf("ssb")
            stmp = sb3("stmp", [128, 16, 128]); bStmp = Buf("stmp")
            tv = sb3("tv", [128, 16, 16]); bTv = Buf("tv")
            c8 = sb3("c8", [128, 8, 16]); bC8 = Buf("c8")
            negm = sb3("negm", [128, 8]); bNegm = Buf("negm")
            Zs = sb3("Zs", [128, 8]); bZs = Buf("Zs")
            rZ = sb3("rZ", [128, 8]); bRZ = Buf("rZ")
            ez = sb3("ez", [128, 16, 128]); bEz = Buf("ez")
            cand = stmp[:].rearrange("p (h a) b -> p h (a b)", a=2); bCand = bStmp
            cand2 = ez[:].rearrange("p (h a) b -> p h (a b)", a=2); bCand2 = bEz
            bigA = sb3("bigA", [128, 128, 128], BF16); bBigAh = [Buf("bigA%d" % i) for i in range(8)]
            XT = sb3("XT", [128, 128, 128], BF16); bXT = Buf("XT")
            YT = sb3("YT", [128, 128, 128], BF16); bYT = Buf("YT")
            NUV = 2
            utb = [sb3("utb%d" % i, [128, 4, 8, 128], BF16) for i in range(NUV)]; bUt = [Buf("utb%d" % i) for i in range(NUV)]
            vb = [sb3("vb%d" % i, [128, 4, D], BF16) for i in range(NUV)]; bVb = [Buf("vb%d" % i) for i in range(NUV)]
            ga = [sb3("ga%d" % i, [128, 512], BF16) for i in range(2)]; bGa = [Buf("ga%d" % i) for i in range(2)]
            wT = [sb3("wT%d" % i, [128, 4, 128], BF16) for i in range(2)]; bWT = [Buf("wT%d" % i) for i in range(2)]

            x1ts = [x1t, sb3("x1tB", [128, D])]; bX1b = [bX1, Buf("x1tB")]
            h2Ts = [h2T, sb3("h2TB", [128, 8, 128], BF16)]; bH2Tb = [bH2T, Buf("h2TB")]
            bYTh = [Buf("YTh%d" % i) for i in range(8)]
            zbufs = [(stmp, bStmp), (ez, bEz)]

            def transposes(srcTok, bSrc, dstT, bDstW):
                for i8 in range(16):
                    pt, bpt = (pT0, bT0) if i8 % 2 == 0 else (pT1, bT1)
                    for j in range(8):
                        i = i8 * 8 + j
                        E("pe", lambda e, i=i, j=j, pt=pt: e.transpose(out=pt[:, j * 128:(j + 1) * 128], in_=srcTok[:, :, i], identity=ident[:]), r=bSrc + [bId], w=[bpt])
                    src = pt[:]
                    dst = dstT[:, i8 * 8:(i8 + 1) * 8, :].rearrange("p a b -> p (a b)")
                    if i8 % 2 == 0:
                        E("act", lambda e, src=src, dst=dst: e.copy(out=dst, in_=src), r=[bpt], w=bDstW)
                    else:
                        E("dve", lambda e, src=src, dst=dst: e.tensor_copy(out=dst, in_=src), r=[bpt], w=bDstW)
                    yield

            def front(gi):
                r0 = gi * 128
                xt_, bx_ = x1ts[gi % 2], bX1b[gi % 2]
                hT_, bh_ = h2Ts[gi % 2], bH2Tb[gi % 2]
                S.dma("act", lambda e: e.dma_start(out=xt_[:], in_=x1s_d[r0:r0 + 128, :]), bx_, reads=[bX1s[gi]], writes=[bx_])
                norm_mod(xt_[:], bx_, g2b, bG2, sh2b[:], bSh2)
                transpose_hb(hT_[:], bh_)
                yield
                for hp in range(16):
                    k = hp % 2
                    S.dma("act", lambda e, hp=hp, k=k: e.dma_start(out=wqb[k][:], in_=wqs_d[hp]), bWq[k], reads=[bWqs[hp // 8]], writes=[bWq[k]])
                    bk = bB[(hp // 4) % 2]
                    dst = bank(PB, (hp // 4) % 2)[:, (hp % 4) * 128:(hp % 4 + 1) * 128]
                    for c in range(8):
                        E("pe", lambda e, c=c, k=k, dst=dst: e.matmul(dst, lhsT=wqb[k][:, c, :], rhs=hT_[:, c, :], start=(c == 0), stop=(c == 7)), r=[bWq[k], bh_], w=[bk])
                    if hp % 4 == 3:
                        q4 = hp // 4
                        E("act", lambda e, q4=q4: e.copy(out=qT[:, q4 * 4:(q4 + 1) * 4, :], in_=bank(PB, q4 % 2).rearrange("p (a t) -> p a t", a=4)), r=[bk], w=[bQT])
                    yield
                for hp in range(16):
                    bk = bB[(hp // 4) % 2]
                    dst = bank(PB, (hp // 4) % 2)[:, (hp % 4) * 128:(hp % 4 + 1) * 128]
                    E("pe", lambda e, hp=hp, dst=dst: e.matmul(dst, lhsT=qT[:, hp, :], rhs=skt[:, hp, :], start=True, stop=True), r=[bQT, bSkt], w=[bk])
                    if hp % 4 == 3:
                        q4 = hp // 4
                        E("act", lambda e, q4=q4: e.copy(out=ssb[:, q4 * 4:(q4 + 1) * 4, :], in_=bank(PB, q4 % 2).rearrange("p (a t) -> p a t", a=4)), r=[bk], w=[bSs])
                        yield
                bTvh = [Buf("tvh%d" % i) for i in range(16)]; bStmph = [Buf("stmph%d" % i) for i in range(16)]
                for hp in range(16):
                    E("dve", lambda e, hp=hp: e.max(out=tv[:, hp, 0:8], in_=ssb[:, hp, :]), r=[bSs, bTv], w=[bTvh[hp]])
                    if hp % 4 == 3:
                        yield
                for hp in range(16):
                    E("dve", lambda e, hp=hp: e.match_replace(out=stmp[:, hp, :], in_to_replace=tv[:, hp, 0:8], in_values=ssb[:, hp, :], imm_value=NEG), r=[bSs, bTvh[hp], bStmp], w=[bStmph[hp]])
                    if hp % 4 == 3:
                        yield
                for hp in range(16):
                    E("dve", lambda e, hp=hp: e.max(out=tv[:, hp, 8:16], in_=stmp[:, hp, :]), r=[bStmph[hp]], w=[bTvh[hp]])
                    if hp % 4 == 3:
                        yield
                E("dve", lambda e: e.memset(ssq[:], 0.0), r=bTvh + bStmph, w=[bTv, bStmp, bSsq])
                in0 = mk(tv, 0, [[32, 8], [1, 16], [0, 16]])
                in1 = mk(tv, 16, [[32, 8], [0, 16], [1, 16]])
                E("dve", lambda e: e.tensor_tensor(out=cand.rearrange("p h (a b) -> p h a b", a=16), in0=in0, in1=in1, op=ALU.add), r=[bTv], w=[bCand])
                yield
                bC8h = [Buf("c8h%d" % i) for i in range(8)]; bCand2h = [Buf("cand2h%d" % i) for i in range(8)]
                for h in range(8):
                    E("dve", lambda e, h=h: e.max(out=c8[:, h, 0:8], in_=cand[:, h, :]), r=[bCand, bC8], w=[bC8h[h]])
                yield
                for h in range(8):
                    E("dve", lambda e, h=h: e.match_replace(out=cand2[:, h, :], in_to_replace=c8[:, h, 0:8], in_values=cand[:, h, :], imm_value=NEG), r=[bCand, bC8h[h], bCand2], w=[bCand2h[h]])
                yield
                for h in range(8):
                    E("dve", lambda e, h=h: e.max(out=c8[:, h, 8:16], in_=cand2[:, h, :]), r=[bCand2h[h]], w=[bC8h[h]])
                E("dve", lambda e: e.memset(ssq[:], 0.0), r=bC8h + bCand2h, w=[bC8, bCand2, bSsq])
                E("dve", lambda e: e.tensor_scalar(out=negm[:], in0=c8[:, :, 0], scalar1=-1.0, scalar2=None, op0=ALU.mult), r=[bC8], w=[bNegm])
                E("dve", lambda e: e.memset(Zs[:], 0.0), w=[bZs])
                yield
                for h in range(8):
                    zb, bZb = zbufs[h % 2]
                    zin0 = mk(ssb, (2 * h + 1) * 128, [[0, 16], [1, 128]])
                    zin1 = mk(tv, (2 * h) * 16, [[1, 16], [0, 128]])
                    slot = YT[:, h * 16:(h + 1) * 16, :].rearrange("p a b -> p (a b)")
                    E("pool", lambda e, zin0=zin0, zin1=zin1, zb=zb: e.tensor_tensor(out=zb[:], in0=zin0, in1=zin1, op=ALU.add), r=[bSs, bTv], w=[bZb])
                    E("act", lambda e, h=h, zb=zb, slot=slot: e.activation(out=slot, in_=zb[:].rearrange("p a b -> p (a b)"), func=AF.Exp, bias=negm[:, h:h + 1]), r=[bZb, bNegm], w=[bYTh[h]])
                    E("dve", lambda e, h=h, zb=zb, slot=slot: e.scalar_tensor_tensor(out=slot, in0=zb[:].rearrange("p a b -> p (a b)"), scalar=c8[:, h, 15:16], in1=slot, op0=ALU.is_ge, op1=ALU.mult, accum_out=Zs[:, h:h + 1]), r=[bZb, bC8, bYTh[h], bZs], w=[bYTh[h], bZs])
                    yield
                E("dve", lambda e: e.reciprocal(out=rZ[:], in_=Zs[:]), r=[bZs], w=[bRZ])
                yield from transposes(YT, bYTh, XT, [bXT])

            def tail(gi):
                for h in range(8):
                    zb, bZb = zbufs[h % 2]
                    yin0 = mk(ssb, (2 * h) * 128, [[0, 16], [1, 128]])
                    yin1 = mk(tv, (2 * h) * 16, [[1, 16], [0, 128]])
                    E("dve", lambda e, yin0=yin0, yin1=yin1, zb=zb: e.tensor_tensor(out=zb[:], in0=yin0, in1=yin1, op=ALU.is_equal), r=[bSs, bTv], w=[bZb])
                    E("dve", lambda e, h=h, zb=zb: e.tensor_scalar(out=bigA[:, h * 16:(h + 1) * 16, :].rearrange("p a b -> p (a b)"), in0=zb[:].rearrange("p a b -> p (a b)"), scalar1=rZ[:, h:h + 1], scalar2=None, op0=ALU.mult), r=[bZb, bRZ], w=[bBigAh[h]])
                for _ in transposes(bigA, bBigAh, YT, bYTh):
                    pass
                for t4 in range(32):
                    P, bP = (PA, bA) if (t4 // 2) % 2 == 0 else (PB, bB)
                    half = t4 % 2
                    for j in range(4):
                        t = t4 * 4 + j
                        E("pe", lambda e, t=t, j=j, P=P, half=half: e.matmul(bank(P, half)[:, j * 128:(j + 1) * 128], lhsT=XT[:, :, t], rhs=YT[:, :, t], start=True, stop=True), r=[bXT] + bYTh, w=[bP[half]])
                    src = bank(P, half)
                    dst = bigA[:, t4 * 4:(t4 + 1) * 4, :].rearrange("p a b -> p (a b)")
                    if t4 % 2 == 0:
                        E("act", lambda e, src=src, dst=dst: e.copy(out=dst, in_=src), r=[bP[half]], w=bBigAh)
                    else:
                        E("dve", lambda e, src=src, dst=dst: e.tensor_copy(out=dst, in_=src), r=[bP[half]], w=bBigAh)
                if gi == 0:
                    dump("ssb", ssb[:], [128, 16, 128], bSs)
                    dump("tv", tv[:], [128, 16, 16], bTv)
                    dump("c8", c8[:], [128, 8, 16], bC8)
                    dump("Zs", Zs[:], [128, 8], bZs)
                    dump("GT", bigA[:], [128, 128, 128], bBigAh[0], BF16)

            def mainloop(gi):
                hT_, bh_ = h2Ts[gi % 2], bH2Tb[gi % 2]
                for j4 in range(32):
                    k = j4 % NUV
                    k2 = j4 % 2
                    S.dma("sp", lambda e, j4=j4, k=k: e.dma_start(out=utb[k][:], in_=uts_d[j4 * 4:(j4 + 1) * 4].rearrange("j p c e -> p j c e")), bUt[k], reads=[bUs[j4 // 2]], writes=[bUt[k]])
                    S.dma("sp", lambda e, j4=j4, k=k: e.dma_start(out=vb[k][:], in_=vs_d[j4 * 512:(j4 + 1) * 512, :].rearrange("(j p) d -> p j d", p=128)), bVb[k], reads=[bVs[j4 // 2]], writes=[bVb[k]])
                    for jj in range(4):
                        for c in range(8):
                            E("pe", lambda e, c=c, k=k, k2=k2, jj=jj: e.matmul(bank(PA, k2)[:, jj * 128:(jj + 1) * 128], lhsT=utb[k][:, jj, c, :], rhs=hT_[:, c, :], start=(c == 0), stop=(c == 7)), r=[bUt[k], bh_], w=[bA[k2]])
                    E("act", lambda e, k2=k2: e.activation(out=ga[k2][:], in_=bank(PA, k2), func=AF.Gelu), r=[bA[k2]], w=[bGa[k2]])
                    E("dve", lambda e, k2=k2, j4=j4: e.tensor_tensor(out=wT[k2][:], in0=ga[k2][:].rearrange("p (a b) -> p a b", a=4), in1=mk(bigA, j4 * 4, [[1, 4], [128, 128]]), op=ALU.mult), r=[bGa[k2]] + bBigAh, w=[bWT[k2]])
                    for jj in range(4):
                        j = j4 * 4 + jj
                        for half in range(2):
                            E("pe", lambda e, half=half, k=k, k2=k2, j=j, jj=jj: e.matmul(bank(PC, half), lhsT=wT[k2][:, jj, :], rhs=vb[k][:, jj, half * 512:(half + 1) * 512], start=(j == 0), stop=(j == 127)), r=[bWT[k2], bVb[k]], w=[bC[half]])
                    yield

            def epilogue(gi):
                r0 = gi * 128
                ot_, bo_ = x1ts[gi % 2], bX1b[gi % 2]
                if do_peer:
                    for half in range(2):
                        hs = slice(half * 512, (half + 1) * 512)
                        E("dve", lambda e, half=half, hs=hs: e.tensor_tensor(out=junk[:, hs], in0=bank(PC, half), in1=gt2b[:, hs], op=ALU.mult), r=[bC[half], bGt2, bJunk], w=[bJunk])
                    E("pool", lambda e: e.tensor_tensor(out=ot_[:], in0=junk[:], in1=ot_[:], op=ALU.add), r=[bJunk, bo_], w=[bo_])
                E("dve", lambda e: e.memset(ssq[:], 0.0), w=[bSsq])
                E("act", lambda e: e.activation(out=junk[:], in_=ot_[:], func=AF.Square, accum_out=ssq[:, 0:1]), r=[bo_, bSsq], w=[bJunk, bSsq])
                E("dve", lambda e: e.tensor_scalar(out=ssq[:], in0=ssq[:], scalar1=1.0 / D, scalar2=EPS, op0=ALU.mult, op1=ALU.add), r=[bSsq], w=[bSsq])
                E("act", lambda e: e.activation(out=ssq[:], in_=ssq[:], func=AF.Sqrt), r=[bSsq], w=[bSsq])
                E("dve", lambda e: e.reciprocal(out=ssq[:], in_=ssq[:]), r=[bSsq], w=[bSsq])
                E("dve", lambda e: e.scalar_tensor_tensor(out=ot_[:], in0=ot_[:], scalar=ssq[:, 0:1], in1=fwb[:], op0=ALU.mult, op1=ALU.mult), r=[bo_, bSsq, bFw], w=[bo_])
                final_ops.append(S.dma("sp", lambda e: e.dma_start(out=out_d[r0:r0 + 128, :], in_=ot_[:]), bo_, reads=[bo_]))

            def run_all(g):
                for _ in g:
                    pass

            if not do_peer:
                for gi in range(ngroups):
                    r0 = gi * 128
                    S.dma("sp", lambda e, r0=r0, gi=gi: e.dma_start(out=x1ts[gi % 2][:], in_=x1s_d[r0:r0 + 128, :]), bX1b[gi % 2], reads=[bX1s[gi]], writes=[bX1b[gi % 2]])
                    epilogue(gi)
            else:
                run_all(front(0))
                tail(0)
                if dbg:
                    dump("h2T", h2Ts[0][:], [128, 8, 128], bH2Tb[0], BF16)
                for gi in range(ngroups):
                    nxt = front(gi + 1) if gi + 1 < ngroups else iter(())
                    for _ in mainloop(gi):
                        for _k in range(3):
                            next(nxt, None)
                    run_all(nxt)
                    epilogue(gi)
                    if gi + 1 < ngroups:
                        tail(gi + 1)

            S.finalize(final_ops + dumps)
            S.run()
    return nc


def _prep_shared(w_ada, b_ada, norm1_w, w_in, hgrn_lb_logits, hgrn_onorm_w, conv_w, conv_onorm_w, w_out,
                 norm2_w, peer_w_query, peer_sub_keys, peer_u, peer_v, final_norm_w):
    f = np.float32
    c = np.ascontiguousarray

    def pm(w):
        return c(np.asarray(w, f).reshape(8, 128, -1).transpose(1, 0, 2))
    sh = {}
    sh["wada"] = pm(w_ada[0])
    sh["bada"] = c(np.asarray(b_ada, f).reshape(1, -1))
    sh["n1w"] = c(np.asarray(norm1_w, f).reshape(1, -1))
    sh["n2w"] = c(np.asarray(norm2_w, f).reshape(1, -1))
    sh["fw"] = c(np.asarray(final_norm_w, f).reshape(1, -1))
    sh["win"] = pm(w_in[0])
    sh["lbl"] = c(np.asarray(hgrn_lb_logits, f).reshape(2, 4, 128).transpose(2, 0, 1))
    sh["onw"] = c(np.asarray(hgrn_onorm_w, f).reshape(4, 128).T)
    sh["cnw"] = c(np.asarray(conv_onorm_w, f).reshape(4, 128).T)
    sh["cw"] = c(np.asarray(conv_w, f).reshape(3, 4, 128).transpose(2, 0, 1))
    sh["wout"] = pm(w_out[0])
    sh["wq"] = c(np.asarray(peer_w_query[0], f).reshape(8, 128, 16, 128).transpose(2, 1, 0, 3))
    sh["skt"] = c(np.asarray(peer_sub_keys, f).reshape(16, 128, 128).transpose(2, 0, 1))
    sh["ut"] = c(np.asarray(peer_u, f).reshape(128, 128, 8, 128).transpose(0, 3, 2, 1))
    sh["v"] = c(np.asarray(peer_v, f).reshape(16384, 1024))
    return sh


def _in_maps(x, c, sh, npre):
    f = np.float32
    x = np.asarray(x, f); cc = np.asarray(c, f)
    maps = []
    for core in range(8):
        b, jj = core // 4, core % 4
        m = dict(sh)
        m["x"] = np.ascontiguousarray(x[b, jj * TOK:(jj + 1) * TOK])
        xpre = np.zeros((max(npre, 1) * TOK, D), f)
        msk = np.zeros((128, 4), f)
        for s in range(npre):
            src = jj - npre + s
            if src >= 0:
                xpre[s * TOK:(s + 1) * TOK] = x[b, src * TOK:(src + 1) * TOK]
                msk[:, s] = 1.0
        msk[:, 3] = 1.0 if (jj >= 1 and npre >= 1) else 0.0
        m["xpre"] = xpre
        m["msk"] = msk
        m["cT"] = np.ascontiguousarray(cc[b].reshape(8, 128).T)
        maps.append(m)
    return maps


_NC_CACHE = {}


def kernel(x, c, w_ada, b_ada, norm1_w, w_in, hgrn_lb_logits, hgrn_onorm_w, conv_w, conv_onorm_w, w_out,
           norm2_w, peer_w_query, peer_sub_keys, peer_u, peer_v, final_norm_w):
    sh = _prep_shared(w_ada, b_ada, norm1_w, w_in, hgrn_lb_logits, hgrn_onorm_w, conv_w, conv_onorm_w, w_out,
                      norm2_w, peer_w_query, peer_sub_keys, peer_u, peer_v, final_norm_w)
    maps = _in_maps(x, c, sh, NPRE)
    nc = build_nc(NPRE)
    res = run_bass_kernel_spmd(nc, maps, core_ids=list(range(8)))
    out = np.zeros((2, 8192, D), np.float32)
    for core in range(8):
        b, jj = core // 4, core % 4
        out[b, jj * TOK:(jj + 1) * TOK] = res.results[core]["out"]
    return out
```

```python
import contextlib
import numpy as np
import concourse.bass as bass
import concourse.mybir as mybir
from concourse.bass_utils import run_bass_kernel_spmd

F32 = mybir.dt.float32
BF16 = mybir.dt.bfloat16
ALU = mybir.AluOpType
AF = mybir.ActivationFunctionType
STRICT_SAME_ENGINE = True


class Buf:
    __slots__ = ("name", "w", "r", "sem", "cnt")

    def __init__(self, name):
        self.name = name
        self.w = None
        self.r = {}
        self.sem = None
        self.cnt = 0


class Op:
    __slots__ = ("eng", "fn", "deps", "flag", "val", "sem", "isdma", "name")

    def __init__(self, eng, fn, isdma=False, name=""):
        self.eng = eng
        self.fn = fn
        self.deps = []
        self.flag = False
        self.val = None
        self.sem = None
        self.isdma = isdma
        self.name = name


class Sched:
    ENGS = ("pe", "act", "dve", "pool", "sp")

    def __init__(self, nc, stack):
        self.nc = nc
        self.stack = stack
        self.q = {e: [] for e in self.ENGS}
        self.esem = {e: stack.enter_context(nc.semaphore("s_" + e)) for e in self.ENGS}
        self.nsem = len(self.ENGS)
        self.ndma = 0

    def newsem(self, name):
        self.nsem += 1
        return self.stack.enter_context(self.nc.semaphore(name))

    def _deps(self, op, reads, writes):
        deps = []
        for b in reads:
            if b.w is not None:
                deps.append(b.w)
        for b in writes:
            if b.w is not None:
                deps.append(b.w)
            deps.extend(b.r.values())
        seen = set()
        for d in deps:
            if d is op or id(d) in seen:
                continue
            seen.add(id(d))
            if (not d.isdma) and d.eng == op.eng and (op.eng == "pe" or not STRICT_SAME_ENGINE):
                continue
            op.deps.append(d)
        for b in reads:
            key = ("dma", id(op)) if op.isdma else op.eng
            b.r[key] = op
        for b in writes:
            b.w = op
            b.r = {}

    def emit(self, eng, fn, reads=(), writes=(), name=""):
        op = Op(eng, fn, name=name)
        self._deps(op, reads, writes)
        self.q[eng].append(op)
        return op

    def dma(self, eng, fn, sbuf, reads=(), writes=(), nparts=1, name=""):
        op = Op(eng, fn, isdma=True, name=name)
        self._deps(op, reads, writes)
        if sbuf.sem is None:
            sbuf.sem = self.newsem("d_" + sbuf.name)
        sbuf.cnt += 16 * nparts
        op.sem = sbuf.sem
        op.val = sbuf.cnt
        self.q[eng].append(op)
        self.ndma += nparts
        return op

    def finalize(self, final_ops):
        for e in self.ENGS:
            for op in self.q[e]:
                for d in op.deps:
                    if not d.isdma:
                        d.flag = True
        for e in self.ENGS:
            c = 0
            for op in self.q[e]:
                if not op.isdma and op.flag:
                    c += 1
                    op.val = c
                    op.sem = self.esem[e]
        self.final_ops = final_ops

    def replay(self, eng, e):
        seen = {}
        nwait = 0
        for op in self.q[eng]:
            for d in op.deps:
                k = id(d.sem)
                if seen.get(k, 0) >= d.val:
                    continue
                seen[k] = d.val
                e.wait_ge(d.sem, d.val)
                nwait += 1
            r = op.fn(e)
            if op.isdma:
                lst = r if isinstance(r, (list, tuple)) else [r]
                for ins in lst:
                    ins.then_inc(op.sem, 16)
            elif op.flag:
                r.then_inc(op.sem, 1)
        if eng == "sp":
            for d in self.final_ops:
                e.wait_ge(d.sem, d.val)
        return nwait

    def run(self):
        nc = self.nc
        with nc.Block() as block:
            @block.sync
            def _(e):
                self.replay("sp", e)

            @block.scalar
            def _(e):
                self.replay("act", e)

            @block.vector
            def _(e):
                self.replay("dve", e)

            @block.gpsimd
            def _(e):
                self.replay("pool", e)

            @block.tensor
            def _(e):
                self.replay("pe", e)


def ap_of(t):
    return t if isinstance(t, bass.AP) else t[:]


def mk(base, offset_elems, dims):
    b = ap_of(base)
    pstep, pcnt = b.ap[0]
    return bass.AP(b.tensor, b.offset + offset_elems, [[pstep, pcnt]] + [list(d) for d in dims])

D = 1024
TOK = 2048
NPRE = 3
EPS = 1e-6
NEG = -1.0e30


def build_nc(npre=NPRE, dbg=False, do_peer=True, ngroups=16):
    nc = bass.Bass("TRN2", target_bir_lowering=False)

    def din(name, shape):
        return nc.dram_tensor(name, shape, F32, kind="ExternalInput").ap()

    x_d = din("x", [TOK, D])
    xpre_d = din("xpre", [max(npre, 1) * TOK, D])
    msk_d = din("msk", [128, 4])
    cT_d = din("cT", [128, 8])
    wada_d = din("wada", [128, 8, 6 * D])
    bada_d = din("bada", [1, 6 * D])
    n1w_d = din("n1w", [1, D])
    n2w_d = din("n2w", [1, D])
    fw_d = din("fw", [1, D])
    win_d = din("win", [128, 8, 3584])
    lbl_d = din("lbl", [128, 2, 4])
    onw_d = din("onw", [128, 4])
    cnw_d = din("cnw", [128, 4])
    cw_d = din("cw", [128, 3, 4])
    wout_d = din("wout", [128, 8, D])
    wq_d = din("wq", [16, 128, 8, 128])
    skt_d = din("skt", [128, 16, 128])
    ut_d = din("ut", [128, 128, 8, 128])
    v_d = din("v", [16384, D])
    out_d = nc.dram_tensor("out", [TOK, D], F32, kind="ExternalOutput").ap()
    x1s_d = nc.dram_tensor("x1s", [TOK, D], F32, kind=("ExternalOutput" if dbg else "Internal")).ap()
    uts_d = nc.dram_tensor("uts", [128, 128, 8, 128], BF16, kind="Internal").ap()
    vs_d = nc.dram_tensor("vs", [16384, D], BF16, kind="Internal").ap()
    wqs_d = nc.dram_tensor("wqs", [16, 128, 8, 128], BF16, kind="Internal").ap()

    with contextlib.ExitStack() as st:
        S = Sched(nc, st)

        def sb(name, shape, dt=F32):
            return st.enter_context(nc.sbuf_tensor("t_" + name, shape, dt))

        def ps(name, shape, dt=F32):
            return st.enter_context(nc.psum_tensor("t_" + name, shape, dt))

        def E(eng, fn, r=(), w=()):
            return S.emit(eng, fn, reads=r, writes=w)

        dumps = []

        def dump(name, ap, shape, buf, dt=F32):
            if not dbg:
                return
            dd = nc.dram_tensor("dbg_" + name, shape, dt, kind="ExternalOutput").ap()
            dumps.append(S.dma("sp", lambda e: e.dma_start(out=dd, in_=ap), buf, reads=[buf]))

        pT0 = ps("pT0", [128, 1024], BF16); bT0 = Buf("pT0")
        pT1 = ps("pT1", [128, 1024], BF16); bT1 = Buf("pT1")
        PA = ps("PA", [128, 1024], F32); bA = [Buf("PA0"), Buf("PA1")]
        PB = ps("PB", [128, 1024], F32); bB = [Buf("PB0"), Buf("PB1")]
        PC = ps("PC", [128, 1024], F32); bC = [Buf("PC0"), Buf("PC1")]

        def bank(P, i):
            return P[:, i * 512:(i + 1) * 512]

        identf = sb("identf", [128, 128]); ident = sb("ident", [128, 128], BF16); bId = Buf("ident")
        E("pool", lambda e: e.memset(identf[:], 1.0), w=[bId])
        E("pool", lambda e: e.affine_select(out=identf[:], in_=identf[:], pattern=[[-1, 128]], compare_op=ALU.is_equal, fill=0.0, base=0, channel_multiplier=1), r=[bId], w=[bId])
        E("pool", lambda e: e.tensor_copy(out=ident[:], in_=identf[:]), r=[bId], w=[bId])
        onesf = sb("onesf", [128, 128]); ones_bf = sb("ones_bf", [128, 128], BF16); bOn = Buf("ones")
        E("pool", lambda e: e.memset(onesf[:], 1.0), w=[bOn])
        E("pool", lambda e: e.tensor_copy(out=ones_bf[:], in_=onesf[:]), r=[bOn], w=[bOn])
        maskST = sb("maskST", [128, 128]); bMk = Buf("maskST")
        E("pool", lambda e: e.memset(maskST[:], 1.0), w=[bMk])
        E("pool", lambda e: e.affine_select(out=maskST[:], in_=maskST[:], pattern=[[1, 128]], compare_op=ALU.is_ge, fill=0.0, base=0, channel_multiplier=-1), r=[bMk], w=[bMk])
        maskI = sb("maskI", [128, 128], mybir.dt.int32)
        E("pool", lambda e: e.tensor_copy(out=maskI[:], in_=maskST[:]), r=[bMk], w=[bMk])

        small = {}

        def load_small(name, src, shape):
            t = sb(name, shape); b = Buf(name)
            S.dma("sp", lambda e: e.dma_start(out=t[:], in_=src), b, writes=[b])
            small[name] = (t, b)
            return t, b

        msk, bMsk = load_small("msk", msk_d, [128, 4])
        cT, bcT = load_small("cT", cT_d, [128, 8])
        lbl, bLbl = load_small("lbl", lbl_d, [128, 2, 4])
        onw, bOnw = load_small("onw", onw_d, [128, 4])
        cnw, bCnw = load_small("cnw", cnw_d, [128, 4])
        cw, bCw = load_small("cw", cw_d, [128, 3, 4])

        lb = sb("lb", [128, 4]); oml = sb("oml", [128, 4]); noml = sb("noml", [128, 4]); bLb = Buf("lb")
        E("dve", lambda e: e.tensor_tensor(out=lb[:], in0=lbl[:, 0, :], in1=lbl[:, 1, :], op=ALU.subtract), r=[bLbl], w=[bLb])
        E("act", lambda e: e.activation(out=lb[:], in_=lb[:], func=AF.Sigmoid), r=[bLb], w=[bLb])
        E("dve", lambda e: e.tensor_scalar(out=oml[:], in0=lb[:], scalar1=-1.0, scalar2=1.0, op0=ALU.mult, op1=ALU.add), r=[bLb], w=[bLb])
        E("dve", lambda e: e.tensor_scalar(out=noml[:], in0=lb[:], scalar1=-1.0, scalar2=None, op0=ALU.add), r=[bLb], w=[bLb])

        g2b = sb("g2b", [128, D]); fwb = sb("fwb", [128, D]); sh2b = sb("sh2b", [128, D]); gt2b = sb("gt2b", [128, D])
        bG2 = Buf("g2b"); bFw = Buf("fwb"); bSh2 = Buf("sh2b"); bGt2 = Buf("gt2b")
        junk = sb("junk", [128, D]); bJunk = Buf("junk")
        ssq = sb("ssq", [128, 1]); bSsq = Buf("ssq")
        hb = sb("hb", [128, D], BF16); bHb = Buf("hb")

        def norm_mod(xap, bX, gb, bG, shiftap, bShift):
            E("dve", lambda e: e.memset(ssq[:], 0.0), w=[bSsq])
            E("act", lambda e: e.activation(out=junk[:], in_=xap, func=AF.Square, accum_out=ssq[:, 0:1]), r=[bX, bSsq], w=[bJunk, bSsq])
            E("dve", lambda e: e.tensor_scalar(out=ssq[:], in0=ssq[:], scalar1=1.0 / D, scalar2=EPS, op0=ALU.mult, op1=ALU.add), r=[bSsq], w=[bSsq])
            E("act", lambda e: e.activation(out=ssq[:], in_=ssq[:], func=AF.Sqrt), r=[bSsq], w=[bSsq])
            E("dve", lambda e: e.reciprocal(out=ssq[:], in_=ssq[:]), r=[bSsq], w=[bSsq])
            E("dve", lambda e: e.scalar_tensor_tensor(out=junk[:], in0=xap, scalar=ssq[:, 0:1], in1=gb[:], op0=ALU.mult, op1=ALU.mult), r=[bX, bSsq, bG, bJunk], w=[bJunk])
            E("pool", lambda e: e.tensor_tensor(out=hb[:], in0=junk[:], in1=shiftap, op=ALU.add), r=[bJunk, bShift], w=[bHb])

        def transpose_hb(dst3, bDst):
            for c in range(8):
                E("pe", lambda e, c=c: e.transpose(out=pT0[:, c * 128:(c + 1) * 128], in_=hb[:, c * 128:(c + 1) * 128], identity=ident[:]), r=[bHb, bId], w=[bT0])
            E("act", lambda e: e.copy(out=dst3, in_=pT0[:].rearrange("p (c t) -> p c t", c=8)), r=[bT0], w=[bDst])

        with contextlib.ExitStack() as st2:
            def sb2(name, shape, dt=F32):
                return st2.enter_context(nc.sbuf_tensor("t_" + name, shape, dt))
            win = sb2("win", [128, 8, 3584], BF16); bWin = Buf("win")
            for c in range(8):
                S.dma("pool", lambda e, c=c: e.dma_start(out=win[:, c, :], in_=win_d[:, c, :]), bWin, writes=[bWin])
            wout = sb2("wout", [128, 8, D], BF16); bWout = Buf("wout")
            for c in range(0, 8, 4):
                S.dma("pool", lambda e, c=c: e.dma_start(out=wout[:, c:c + 4, :], in_=wout_d[:, c:c + 4, :]), bWout, writes=[bWout])
            bUs = [Buf("uts%d" % i) for i in range(16)]; bVs = [Buf("vs%d" % i) for i in range(16)]
            xt = sb2("xt", [128, 4, D]); bXt = [Buf("xt%d" % i) for i in range(4)]
            h1T = sb2("h1T", [128, 8, 512], BF16); bH1T = [Buf("h1T%d" % i) for i in range(4)]
            itok = sb2("itok", [128, 4, 512], BF16); bItok = [Buf("itok%d" % i) for i in range(4)]
            sg = sb2("sg", [128, 512]); bSg = Buf("sg")
            lf = sb2("lf", [128, 512]); bLf = Buf("lf")
            kk = sb2("kk", [128, 512]); bKk = Buf("kk")
            qs = sb2("qs", [128, 512]); bQs = Buf("qs")
            gs = sb2("gs", [128, 512]); bGs = Buf("gs")
            bt = sb2("bt", [128, 128]); bBt = Buf("bt")
            nbm = sb2("nbm", [128, 1]); bNbm = Buf("nbm")
            eb = sb2("eb", [128, 1]); bEb = Buf("eb")
            ex = [sb2("ex%d" % i, [128, 128]) for i in range(4)]; bEx = [Buf("ex%d" % i) for i in range(4)]
            qt = sb2("qt", [128, 128], BF16); bQt = Buf("qt")
            kt = sb2("kt", [128, 128], BF16); bKt = Buf("kt")
            qh = sb2("qh", [128, 128], BF16); bQh = Buf("qh")
            khT = sb2("khT", [128, 128], BF16); bKhT = Buf("khT")
            kh = sb2("kh", [128, 128], BF16); bKh = Buf("kh")
            scm = sb2("scm", [128, 128], BF16); bScm = Buf("scm")
            Sst = sb2("Sst", [128, 4, 128]); bS = [Buf("S%d" % i) for i in range(4)]
            Sbf = sb2("Sbf", [128, 4, 128], BF16); bSbf = [Buf("Sbf%d" % i) for i in range(4)]
            sq = sb2("sq", [128, 512], BF16); bSq = Buf("sq")
            rb = sb2("rb", [128, 512]); bRb = Buf("rb")
            yv = sb2("yv", [128, 512]); bYv = Buf("yv")
            ymT = sb2("ymT", [128, 8, 512], BF16); bYm = [Buf("ym%d" % i) for i in range(8)]
            u = sb2("u", [128, 4, 514]); bU = [Buf("u%d" % i) for i in range(4)]
            csb = sg; bCsb = bSg
            y0 = lf; bY0 = bLf
            y1 = kk; bY1 = bKk
            t1 = junk; bTt1 = bJunk

            modb = sb2("modb", [128, 6 * D]); bMod = Buf("modb")
            cact = sb2("cact", [128, 8]); crep = sb2("crep", [128, 8, 128]); bCrep = Buf("crep")
            E("act", lambda e: e.activation(out=cact[:], in_=cT[:], func=AF.Silu), r=[bcT], w=[bCrep])
            for c in range(8):
                E("dve", lambda e, c=c: e.tensor_scalar(out=crep[:, c, :], in0=onesf[:], scalar1=cact[:, c:c + 1], scalar2=None, op0=ALU.mult), r=[bOn, bCrep], w=[bCrep])
            S.dma("sp", lambda e: e.dma_start(out=modb[:], in_=bada_d.partition_broadcast(128)), bMod, writes=[bMod])
            wst = [xt[:, 0:2, :].rearrange("p a (b n) -> p (a b) n", n=256), xt[:, 2:4, :].rearrange("p a (b n) -> p (a b) n", n=256)]
            bWst = [[bXt[0], bXt[1]], [bXt[2], bXt[3]]]
            for g in range(24):
                k = g % 2
                S.dma("sp", lambda e, g=g, k=k: e.dma_start(out=wst[k], in_=wada_d[:, :, g * 256:(g + 1) * 256]), bWst[k][0], writes=bWst[k])
                for c in range(8):
                    E("pe", lambda e, c=c, k=k: e.matmul(bank(PA, k)[:, 0:256], lhsT=crep[:, c, :], rhs=wst[k][:, c, :], start=(c == 0), stop=(c == 7)), r=[bCrep] + bWst[k], w=[bA[k]])
                E("dve", lambda e, g=g, k=k: e.tensor_tensor(out=modb[:, g * 256:(g + 1) * 256], in0=bank(PA, k)[:, 0:256], in1=modb[:, g * 256:(g + 1) * 256], op=ALU.add), r=[bA[k], bMod], w=[bMod])
            g1b = sb2("g1b", [128, D]); bG1 = Buf("g1b")
            S.dma("sp", lambda e: e.dma_start(out=g1b[:], in_=n1w_d.partition_broadcast(128)), bG1, writes=[bG1])
            S.dma("sp", lambda e: e.dma_start(out=g2b[:], in_=n2w_d.partition_broadcast(128)), bG2, writes=[bG2])
            S.dma("sp", lambda e: e.dma_start(out=fwb[:], in_=fw_d.partition_broadcast(128)), bFw, writes=[bFw])
            E("dve", lambda e: e.scalar_tensor_tensor(out=g1b[:], in0=modb[:, D:2 * D], scalar=1.0, in1=g1b[:], op0=ALU.add, op1=ALU.mult), r=[bMod, bG1], w=[bG1])
            E("dve", lambda e: e.scalar_tensor_tensor(out=g2b[:], in0=modb[:, 4 * D:5 * D], scalar=1.0, in1=g2b[:], op0=ALU.add, op1=ALU.mult), r=[bMod, bG2], w=[bG2])
            shift1 = modb[:, 0:D]; gate1 = modb[:, 2 * D:3 * D]
            dump("modb", modb[:], [128, 6 * D], bMod)
            dump("g1b", g1b[:], [128, D], bG1)
            dump("lb", lb[:], [128, 4], bLb)
            E("dve", lambda e: e.tensor_copy(out=sh2b[:], in_=modb[:, 3 * D:4 * D]), r=[bMod], w=[bSh2])
            E("dve", lambda e: e.tensor_copy(out=gt2b[:], in_=modb[:, 5 * D:6 * D]), r=[bMod], w=[bGt2])

            bWqs = [Buf("wqs%d" % i) for i in range(2)]
            if do_peer:
                for i in range(2):
                    S.dma("pool", lambda e, i=i: e.dma_start(out=wqs_d[i * 8:(i + 1) * 8], in_=wq_d[i * 8:(i + 1) * 8]), bWqs[i], reads=[bSh2, bGt2, bG1], writes=[bWqs[i]])
                for i in range(16):
                    S.dma("pool", lambda e, i=i: e.dma_start(out=uts_d[i * 8:(i + 1) * 8], in_=ut_d[i * 8:(i + 1) * 8]), bUs[i], reads=[bSh2, bGt2, bG1], writes=[bUs[i]])
                    S.dma("pool", lambda e, i=i: e.dma_start(out=vs_d[i * 1024:(i + 1) * 1024, :], in_=v_d[i * 1024:(i + 1) * 1024, :]), bVs[i], reads=[bSh2, bGt2, bG1], writes=[bVs[i]])
            E("pool", lambda e: e.memset(scm[:], 0.0), w=[bScm])
            for h in range(4):
                E("dve", lambda e, h=h: e.memset(Sst[:, h, :], 0.0), w=[bS[h]])
                E("pool", lambda e, h=h: e.memset(Sbf[:, h, :], 0.0), w=[bSbf[h]])
                E("pool", lambda e, h=h: e.memset(u[:, h, 0:2], 0.0), w=[bU[h]])

            rot = [0]

            def projbank():
                k = rot[0] % 2
                rot[0] += 1
                return k

            def proj_fm(col0, n):
                k = projbank()
                for c in range(8):
                    E("pe", lambda e, c=c, k=k: e.matmul(bank(PA, k)[:, 0:n], lhsT=win[:, c, col0:col0 + 128], rhs=h1T[:, c, 0:n], start=(c == 0), stop=(c == 7)), r=[bWin] + bH1T, w=[bA[k]])
                return bank(PA, k)[:, 0:n], bA[k]

            def hgrn_head(h, ntile, mode):
                n = ntile * 128
                fp, bf_ = proj_fm(512 + h * 128, n)
                E("act", lambda e: e.activation(out=sg[:, 0:n], in_=fp, func=AF.Sigmoid), r=[bf_], w=[bSg])
                E("act", lambda e: e.activation(out=lf[:, 0:n], in_=sg[:, 0:n], func=AF.Ln, scale=oml[:, h:h + 1], bias=lb[:, h:h + 1]), r=[bSg, bLb], w=[bLf])
                E("dve", lambda e: e.tensor_scalar(out=kk[:, 0:n], in0=sg[:, 0:n], scalar1=noml[:, h:h + 1], scalar2=oml[:, h:h + 1], op0=ALU.mult, op1=ALU.add), r=[bSg, bLb], w=[bKk])
                if mode == "main":
                    qp, bq_ = proj_fm(h * 128, n)
                    E("act", lambda e: e.copy(out=qs[:, 0:n], in_=qp), r=[bq_], w=[bQs])
                    gp, bg_ = proj_fm(1536 + h * 128, n)
                    E("act", lambda e: e.activation(out=gs[:, 0:n], in_=gp, func=AF.Silu), r=[bg_], w=[bGs])
                def do_chunk(ck):
                    cs = slice(ck * 128, (ck + 1) * 128)
                    E("dve", lambda e: e.tensor_tensor_scan(out=bt[:], data0=onesf[:], data1=lf[:, cs], initial=0.0, op0=ALU.mult, op1=ALU.add), r=[bOn, bLf], w=[bBt])
                    E("act", lambda e: e.activation(out=ex[3][:], in_=bt[:], func=AF.Exp, scale=-1.0, bias=bt[:, 127:128]), r=[bBt], w=[bEx[3]])
                    E("dve", lambda e: e.tensor_tensor(out=khT[:], in0=kk[:, cs], in1=ex[3][:], op=ALU.mult), r=[bKk, bEx[3]], w=[bKhT])
                    E("pe", lambda e: e.transpose(out=pT1[:, 0:128], in_=khT[:], identity=ident[:]), r=[bKhT, bId], w=[bT1])
                    E("act", lambda e: e.copy(out=kh[:], in_=pT1[:, 0:128]), r=[bT1], w=[bKh])
                    isl = itok[:, ck, h * 128:(h + 1) * 128]
                    if mode == "main":
                        E("dve", lambda e: e.tensor_scalar(out=nbm[:], in0=bt[:, 63:64], scalar1=-1.0, scalar2=None, op0=ALU.mult), r=[bBt], w=[bNbm])
                        E("act", lambda e: e.activation(out=ex[0][:], in_=bt[:], func=AF.Exp, bias=nbm[:, 0:1]), r=[bBt, bNbm], w=[bEx[0]])
                        E("act", lambda e: e.activation(out=ex[1][:], in_=bt[:], func=AF.Exp, scale=-1.0, bias=bt[:, 63:64]), r=[bBt], w=[bEx[1]])
                        E("act", lambda e: e.activation(out=ex[2][:], in_=bt[:], func=AF.Exp), r=[bBt], w=[bEx[2]])
                        E("dve", lambda e: e.tensor_tensor(out=qt[:], in0=qs[:, cs], in1=ex[0][:], op=ALU.mult), r=[bQs, bEx[0]], w=[bQt])
                        E("pool", lambda e: e.tensor_tensor(out=kt[:], in0=kk[:, cs], in1=ex[1][:], op=ALU.mult), r=[bKk, bEx[1]], w=[bKt])
                        E("pool", lambda e: e.tensor_tensor(out=qh[:], in0=qs[:, cs], in1=ex[2][:], op=ALU.mult), r=[bQs, bEx[2]], w=[bQh])
                        E("pe", lambda e: e.matmul(bank(PB, 0)[:, 0:128], lhsT=kt[:], rhs=qt[:], start=True, stop=True), r=[bKt, bQt], w=[bB[0]])
                        E("dve", lambda e: e.copy_predicated(out=scm[:], mask=maskI[:], data=bank(PB, 0)[:, 0:128]), r=[bB[0], bMk, bScm], w=[bScm])
                        E("pe", lambda e: e.matmul(bank(PB, 1)[:, cs], lhsT=isl, rhs=scm[:], start=True, stop=False), r=[bItok[ck], bScm], w=[bB[1]])
                        E("pe", lambda e: e.matmul(bank(PB, 1)[:, cs], lhsT=Sbf[:, h, :], rhs=qh[:], start=False, stop=True), r=[bSbf[h], bQh], w=[bB[1]])
                    E("pe", lambda e: e.matmul(bank(PC, 0)[:, 0:128], lhsT=kh[:], rhs=isl, start=True, stop=True), r=[bKh, bItok[ck]], w=[bC[0]])
                    E("act", lambda e: e.activation(out=eb[:], in_=bt[:, 127:128], func=AF.Exp), r=[bBt], w=[bEb])
                    E("dve", lambda e: e.scalar_tensor_tensor(out=Sst[:, h, :], in0=Sst[:, h, :], scalar=eb[:, 0:1], in1=bank(PC, 0)[:, 0:128], op0=ALU.mult, op1=ALU.add), r=[bS[h], bEb, bC[0]], w=[bS[h]])
                    if mode == "main":
                        E("pool", lambda e: e.tensor_copy(out=Sbf[:, h, :], in_=Sst[:, h, :]), r=[bS[h]], w=[bSbf[h]])
                for ck in range(ntile):
                    do_chunk(ck)
                if mode == "main":
                    groupnorm_out(bank(PB, 1), bB[1], onw[:, h:h + 1], bOnw, h, mulgs=True)

            def groupnorm_out(src, bSrc, wcol, bW, slot, mulgs):
                E("act", lambda e: e.activation(out=sq[:], in_=src, func=AF.Square), r=[bSrc], w=[bSq])
                E("pe", lambda e: e.matmul(bank(PC, 1), lhsT=ones_bf[:], rhs=sq[:], start=True, stop=True), r=[bOn, bSq], w=[bC[1]])
                E("dve", lambda e: e.tensor_scalar(out=rb[:], in0=bank(PC, 1), scalar1=1.0 / 128, scalar2=EPS, op0=ALU.mult, op1=ALU.add), r=[bC[1]], w=[bRb])
                E("act", lambda e: e.activation(out=rb[:], in_=rb[:], func=AF.Sqrt), r=[bRb], w=[bRb])
                E("dve", lambda e: e.reciprocal(out=rb[:], in_=rb[:]), r=[bRb], w=[bRb])
                if mulgs:
                    E("dve", lambda e: e.scalar_tensor_tensor(out=yv[:], in0=src, scalar=wcol, in1=rb[:], op0=ALU.mult, op1=ALU.mult), r=[bSrc, bW, bRb], w=[bYv])
                    E("pool", lambda e: e.tensor_tensor(out=ymT[:, slot, :], in0=yv[:], in1=gs[:], op=ALU.mult), r=[bYv, bGs], w=[bYm[slot]])
                else:
                    E("dve", lambda e: e.scalar_tensor_tensor(out=ymT[:, slot, :], in0=src, scalar=wcol, in1=rb[:], op0=ALU.mult, op1=ALU.mult), r=[bSrc, bW, bRb], w=[bYm[slot]])

            def conv_group(g, mode):
                if mode == "halo":
                    cp, bc_ = proj_fm(2560 + g * 128, 128)
                    E("act", lambda e: e.copy(out=csb[:, 0:128], in_=cp), r=[bc_], w=[bCsb])
                    xp, bx_ = proj_fm(3072 + g * 128, 128)
                    E("dve", lambda e: e.tensor_tensor(out=y0[:, 0:128], in0=csb[:, 0:128], in1=xp, op=ALU.mult), r=[bCsb, bx_], w=[bY0])
                    E("dve", lambda e: e.tensor_scalar(out=u[:, g, 0:2], in0=y0[:, 126:128], scalar1=msk[:, 3:4], scalar2=None, op0=ALU.mult), r=[bY0, bMsk], w=[bU[g]])
                    return
                cp, bc_ = proj_fm(2560 + g * 128, 512)
                E("act", lambda e: e.copy(out=csb[:], in_=cp), r=[bc_], w=[bCsb])
                xp, bx_ = proj_fm(3072 + g * 128, 512)
                E("dve", lambda e: e.tensor_tensor(out=u[:, g, 2:514], in0=csb[:], in1=xp, op=ALU.mult), r=[bCsb, bx_], w=[bU[g]])
                bp, bb_ = proj_fm(2048 + g * 128, 512)
                E("dve", lambda e: e.tensor_scalar(out=y0[:], in0=u[:, g, 2:514], scalar1=cw[:, 2, g:g + 1], scalar2=None, op0=ALU.mult), r=[bU[g], bCw], w=[bY0])
                E("dve", lambda e: e.scalar_tensor_tensor(out=y1[:], in0=u[:, g, 1:513], scalar=cw[:, 1, g:g + 1], in1=y0[:], op0=ALU.mult, op1=ALU.add), r=[bU[g], bCw, bY0], w=[bY1])
                E("dve", lambda e: e.scalar_tensor_tensor(out=y0[:], in0=u[:, g, 0:512], scalar=cw[:, 0, g:g + 1], in1=y1[:], op0=ALU.mult, op1=ALU.add), r=[bU[g], bCw, bY1], w=[bY0])
                E("dve", lambda e: e.tensor_tensor(out=y1[:], in0=y0[:], in1=bp, op=ALU.mult), r=[bY0, bb_], w=[bY1])
                E("pool", lambda e: e.tensor_copy(out=u[:, g, 0:2], in_=u[:, g, 512:514]), r=[bU[g]], w=[bU[g]])
                groupnorm_out(y1[:], bY1, cnw[:, g:g + 1], bCnw, 4 + g, mulgs=False)

            def mixer_macro(src_d, row0, ntile, mode):
                n = ntile * 128
                for tt in range(ntile):
                    S.dma("sp", lambda e, tt=tt: e.dma_start(out=xt[:, tt, :], in_=src_d[row0 + tt * 128: row0 + (tt + 1) * 128, :]), bXt[tt], writes=[bXt[tt]])
                    norm_mod(xt[:, tt, :], bXt[tt], g1b, bG1, shift1, bMod)
                    transpose_hb(h1T[:, :, tt * 128:(tt + 1) * 128], bH1T[tt])
                if mode == "halo":
                    for g in range(4):
                        conv_group(g, "halo")
                    return
                for tt in range(ntile):
                    k = projbank()
                    for c in range(8):
                        E("pe", lambda e, c=c, k=k, tt=tt: e.matmul(bank(PA, k), lhsT=h1T[:, c, tt * 128:(tt + 1) * 128], rhs=win[:, c, 1024:1536], start=(c == 0), stop=(c == 7)), r=[bWin, bH1T[tt]], w=[bA[k]])
                    E("act", lambda e, k=k, tt=tt: e.copy(out=itok[:, tt, :], in_=bank(PA, k)), r=[bA[k]], w=[bItok[tt]])
                for h in range(4):
                    hgrn_head(h, ntile, mode)
                if mode != "main":
                    return
                for g in range(4):
                    conv_group(g, "main")
                for tt in range(ntile):
                    for half in range(2):
                        for cc in range(8):
                            E("pe", lambda e, cc=cc, half=half, tt=tt: e.matmul(bank(PA, half), lhsT=ymT[:, cc, tt * 128:(tt + 1) * 128], rhs=wout[:, cc, half * 512:(half + 1) * 512], start=(cc == 0), stop=(cc == 7)), r=[bYm[cc], bWout], w=[bA[half]])
                    for half in range(2):
                        hs = slice(half * 512, (half + 1) * 512)
                        E("dve", lambda e, half=half, hs=hs: e.tensor_tensor(out=t1[:, hs], in0=bank(PA, half), in1=gate1[:, hs], op=ALU.mult), r=[bA[half], bMod, bTt1], w=[bTt1])
                    E("pool", lambda e, tt=tt: e.tensor_tensor(out=xt[:, tt, :], in0=t1[:], in1=xt[:, tt, :], op=ALU.add), r=[bTt1, bXt[tt]], w=[bXt[tt]])
                    S.dma("sp", lambda e, tt=tt: e.dma_start(out=x1s_d[row0 + tt * 128: row0 + (tt + 1) * 128, :], in_=xt[:, tt, :]), bXt[tt], reads=[bXt[tt]], writes=[bX1s[(row0 + tt * 128) // 128]])

            bX1s = [Buf("x1s%d" % i) for i in range(16)]
            for seg in range(npre):
                for m in range(4):
                    mixer_macro(xpre_d, seg * TOK + m * 512, 4, "pre")
                for h in range(4):
                    E("dve", lambda e, h=h, seg=seg: e.tensor_scalar(out=Sst[:, h, :], in0=Sst[:, h, :], scalar1=msk[:, seg:seg + 1], scalar2=None, op0=ALU.mult), r=[bS[h], bMsk], w=[bS[h]])
            if npre > 0:
                mixer_macro(xpre_d, npre * TOK - 128, 1, "halo")
            for h in range(4):
                E("pool", lambda e, h=h: e.tensor_copy(out=Sbf[:, h, :], in_=Sst[:, h, :]), r=[bS[h]], w=[bSbf[h]])
            for m in range(4):
                mixer_macro(x_d, m * 512, 4, "main")
                if m == 0:
                    dump("ymT", ymT[:], [128, 8, 512], bYm[7], BF16)
                    dump("h1T", h1T[:], [128, 8, 512], bH1T[3], BF16)
                    dump("itok", itok[:], [128, 4, 512], bItok[3], BF16)
                    dump("Sst", Sst[:], [128, 4, 128], bS[3])
                    dump("u", u[:], [128, 4, 514], bU[3])
            mixer_tail_bufs = [bWin, bWout] + bXt + bH1T + bItok + [bSg, bLf, bKk, bQs, bGs, bBt, bNbm, bEb] + bEx + [bQt, bKt, bQh, bKhT, bKh, bScm] + bS + bSbf + [bSq, bRb, bYv] + bYm + bU + [bMod, bG1, bCrep]

        final_ops = []
        with contextlib.ExitStack() as st3:
            def sb3(name, shape, dt=F32):
                return st3.enter_context(nc.sbuf_tensor("t_" + name, shape, dt))
            bFence = Buf("fence")
            E("dve", lambda e: e.memset(ssq[:], 0.0), w=mixer_tail_bufs + [bFence, bSsq])
            E("act", lambda e: e.copy(out=junk[:, 0:1], in_=ssq[:]), r=[bFence, bSsq], w=[bFence, bJunk])
            E("pool", lambda e: e.tensor_copy(out=junk[:, 1:2], in_=ssq[:]), r=[bFence, bSsq], w=[bFence, bJunk])
            E("pe", lambda e: e.transpose(out=pT0[:, 0:128], in_=ident[:], identity=ident[:]), r=[bFence, bId], w=[bFence, bT0])
            S.dma("sp", lambda e: e.dma_start(out=msk[:], in_=msk_d), bMsk, reads=[bFence], writes=[bFence, bMsk])
            S.dma("pool", lambda e: e.dma_start(out=msk[:], in_=msk_d), bMsk, reads=[bFence], writes=[bFence, bMsk])

            skt = sb3("skt", [128, 16, 128], BF16); bSkt = Buf("skt")
            S.dma("pool", lambda e: e.dma_start(out=skt[:], in_=skt_d), bSkt, reads=[bFence], writes=[bSkt])
            wqb = [sb3("wqb%d" % i, [128, 8, 128], BF16) for i in range(2)]; bWq = [Buf("wqb%d" % i) for i in range(2)]
            x1t = sb3("x1t", [128, D]); bX1 = Buf("x1t")
            h2T = sb3("h2T", [128, 8, 128], BF16); bH2T = Buf("h2T")
            qT = sb3("qT", [128, 16, 128], BF16); bQT = Buf("qT")
            ssb = sb3("ssb", [128, 16, 128]); bSs = Buf("ssb")
            stmp = sb3("stmp", [128, 16, 128]); bStmp = Buf("stmp")
            tv = sb3("tv", [128, 16, 16]); bTv = Buf("tv")
            c8 = sb3("c8", [128, 8, 16]); bC8 = Buf("c8")
            negm = sb3("negm", [128, 8]); bNegm = Buf("negm")
            Zs = sb3("Zs", [128, 8]); bZs = Buf("Zs")
            rZ = sb3("rZ", [128, 8]); bRZ = Buf("rZ")
            ez = sb3("ez", [128, 16, 128]); bEz = Buf("ez")
            cand = stmp[:].rearrange("p (h a) b -> p h (a b)", a=2); bCand = bStmp
            cand2 = ez[:].rearrange("p (h a) b -> p h (a b)", a=2); bCand2 = bEz
            bigA = sb3("bigA", [128, 128, 128], BF16); bBigAh = [Buf("bigA%d" % i) for i in range(8)]
            XT = sb3("XT", [128, 128, 128], BF16); bXT = Buf("XT")
            YT = sb3("YT", [128, 128, 128], BF16); bYT = Buf("YT")
            NUV = 2
            utb = [sb3("utb%d" % i, [128, 4, 8, 128], BF16) for i in range(NUV)]; bUt = [Buf("utb%d" % i) for i in range(NUV)]
            vb = [sb3("vb%d" % i, [128, 4, D], BF16) for i in range(NUV)]; bVb = [Buf("vb%d" % i) for i in range(NUV)]
            ga = [sb3("ga%d" % i, [128, 512], BF16) for i in range(2)]; bGa = [Buf("ga%d" % i) for i in range(2)]
            wT = [sb3("wT%d" % i, [128, 4, 128], BF16) for i in range(2)]; bWT = [Buf("wT%d" % i) for i in range(2)]

            x1ts = [x1t, sb3("x1tB", [128, D])]; bX1b = [bX1, Buf("x1tB")]
            h2Ts = [h2T, sb3("h2TB", [128, 8, 128], BF16)]; bH2Tb = [bH2T, Buf("h2TB")]
            bYTh = [Buf("YTh%d" % i) for i in range(8)]
            zbufs = [(stmp, bStmp), (ez, bEz)]

            def transposes(srcTok, bSrc, dstT, bDstW):
                for i8 in range(16):
                    pt, bpt = (pT0, bT0) if i8 % 2 == 0 else (pT1, bT1)
                    for j in range(8):
                        i = i8 * 8 + j
                        E("pe", lambda e, i=i, j=j, pt=pt: e.transpose(out=pt[:, j * 128:(j + 1) * 128], in_=srcTok[:, :, i], identity=ident[:]), r=bSrc + [bId], w=[bpt])
                    src = pt[:]
                    dst = dstT[:, i8 * 8:(i8 + 1) * 8, :].rearrange("p a b -> p (a b)")
                    if i8 % 2 == 0:
                        E("act", lambda e, src=src, dst=dst: e.copy(out=dst, in_=src), r=[bpt], w=bDstW)
                    else:
                        E("dve", lambda e, src=src, dst=dst: e.tensor_copy(out=dst, in_=src), r=[bpt], w=bDstW)
                    yield

            def front(gi):
                r0 = gi * 128
                xt_, bx_ = x1ts[gi % 2], bX1b[gi % 2]
                hT_, bh_ = h2Ts[gi % 2], bH2Tb[gi % 2]
                S.dma("act", lambda e: e.dma_start(out=xt_[:], in_=x1s_d[r0:r0 + 128, :]), bx_, reads=[bX1s[gi]], writes=[bx_])
                norm_mod(xt_[:], bx_, g2b, bG2, sh2b[:], bSh2)
                transpose_hb(hT_[:], bh_)
                yield
                S.dma("act", lambda e: e.dma_start(out=wqb[0][:], in_=wqs_d[0]), bWq[0], reads=[bWqs[0]], writes=[bWq[0]])
                for hp in range(16):
                    k = hp % 2
                    if hp + 1 < 16:
                        S.dma("act", lambda e, hp=hp: e.dma_start(out=wqb[(hp + 1) % 2][:], in_=wqs_d[hp + 1]), bWq[(hp + 1) % 2], reads=[bWqs[(hp + 1) // 8]], writes=[bWq[(hp + 1) % 2]])
                    bk = bB[(hp // 4) % 2]
                    dst = bank(PB, (hp // 4) % 2)[:, (hp % 4) * 128:(hp % 4 + 1) * 128]
                    for c in range(8):
                        E("pe", lambda e, c=c, k=k, dst=dst: e.matmul(dst, lhsT=wqb[k][:, c, :], rhs=hT_[:, c, :], start=(c == 0), stop=(c == 7)), r=[bWq[k], bh_], w=[bk])
                    if hp % 4 == 3:
                        q4 = hp // 4
                        E("act", lambda e, q4=q4: e.copy(out=qT[:, q4 * 4:(q4 + 1) * 4, :], in_=bank(PB, q4 % 2).rearrange("p (a t) -> p a t", a=4)), r=[bk], w=[bQT])
                    yield
                for hp in range(16):
                    bk = bB[(hp // 4) % 2]
                    dst = bank(PB, (hp // 4) % 2)[:, (hp % 4) * 128:(hp % 4 + 1) * 128]
                    E("pe", lambda e, hp=hp, dst=dst: e.matmul(dst, lhsT=qT[:, hp, :], rhs=skt[:, hp, :], start=True, stop=True), r=[bQT, bSkt], w=[bk])
                    if hp % 4 == 3:
                        q4 = hp // 4
                        E("act", lambda e, q4=q4: e.copy(out=ssb[:, q4 * 4:(q4 + 1) * 4, :], in_=bank(PB, q4 % 2).rearrange("p (a t) -> p a t", a=4)), r=[bk], w=[bSs])
                        yield
                bTvh = [Buf("tvh%d" % i) for i in range(16)]; bStmph = [Buf("stmph%d" % i) for i in range(16)]
                for hp in range(16):
                    E("dve", lambda e, hp=hp: e.max(out=tv[:, hp, 0:8], in_=ssb[:, hp, :]), r=[bSs, bTv], w=[bTvh[hp]])
                    if hp % 4 == 3:
                        yield
                for hp in range(16):
                    E("dve", lambda e, hp=hp: e.match_replace(out=stmp[:, hp, :], in_to_replace=tv[:, hp, 0:8], in_values=ssb[:, hp, :], imm_value=NEG), r=[bSs, bTvh[hp], bStmp], w=[bStmph[hp]])
                    if hp % 4 == 3:
                        yield
                for hp in range(16):
                    E("dve", lambda e, hp=hp: e.max(out=tv[:, hp, 8:16], in_=stmp[:, hp, :]), r=[bStmph[hp]], w=[bTvh[hp]])
                    if hp % 4 == 3:
                        yield
                E("dve", lambda e: e.memset(ssq[:], 0.0), r=bTvh + bStmph, w=[bTv, bStmp, bSsq])
                in0 = mk(tv, 0, [[32, 8], [1, 16], [0, 16]])
                in1 = mk(tv, 16, [[32, 8], [0, 16], [1, 16]])
                E("dve", lambda e: e.tensor_tensor(out=cand.rearrange("p h (a b) -> p h a b", a=16), in0=in0, in1=in1, op=ALU.add), r=[bTv], w=[bCand])
                yield
                bC8h = [Buf("c8h%d" % i) for i in range(8)]; bCand2h = [Buf("cand2h%d" % i) for i in range(8)]
                for h in range(8):
                    E("dve", lambda e, h=h: e.max(out=c8[:, h, 0:8], in_=cand[:, h, :]), r=[bCand, bC8], w=[bC8h[h]])
                yield
                for h in range(8):
                    E("dve", lambda e, h=h: e.match_replace(out=cand2[:, h, :], in_to_replace=c8[:, h, 0:8], in_values=cand[:, h, :], imm_value=NEG), r=[bCand, bC8h[h], bCand2], w=[bCand2h[h]])
                yield
                for h in range(8):
                    E("dve", lambda e, h=h: e.max(out=c8[:, h, 8:16], in_=cand2[:, h, :]), r=[bCand2h[h]], w=[bC8h[h]])
                E("dve", lambda e: e.memset(ssq[:], 0.0), r=bC8h + bCand2h, w=[bC8, bCand2, bSsq])
                E("dve", lambda e: e.tensor_scalar(out=negm[:], in0=c8[:, :, 0], scalar1=-1.0, scalar2=None, op0=ALU.mult), r=[bC8], w=[bNegm])
                E("dve", lambda e: e.memset(Zs[:], 0.0), w=[bZs])
                yield
                for h in range(8):
                    zb, bZb = zbufs[h % 2]
                    zin0 = mk(ssb, (2 * h + 1) * 128, [[0, 16], [1, 128]])
                    zin1 = mk(tv, (2 * h) * 16, [[1, 16], [0, 128]])
                    slot = YT[:, h * 16:(h + 1) * 16, :].rearrange("p a b -> p (a b)")
                    E("pool", lambda e, zin0=zin0, zin1=zin1, zb=zb: e.tensor_tensor(out=zb[:], in0=zin0, in1=zin1, op=ALU.add), r=[bSs, bTv], w=[bZb])
                    E("act", lambda e, h=h, zb=zb, slot=slot: e.activation(out=slot, in_=zb[:].rearrange("p a b -> p (a b)"), func=AF.Exp, bias=negm[:, h:h + 1]), r=[bZb, bNegm], w=[bYTh[h]])
                    E("dve", lambda e, h=h, zb=zb, slot=slot: e.scalar_tensor_tensor(out=slot, in0=zb[:].rearrange("p a b -> p (a b)"), scalar=c8[:, h, 15:16], in1=slot, op0=ALU.is_ge, op1=ALU.mult, accum_out=Zs[:, h:h + 1]), r=[bZb, bC8, bYTh[h], bZs], w=[bYTh[h], bZs])
                    yield
                E("dve", lambda e: e.reciprocal(out=rZ[:], in_=Zs[:]), r=[bZs], w=[bRZ])
                yield from transposes(YT, bYTh, XT, [bXT])

            def tail(gi):
                for h in range(8):
                    zb, bZb = zbufs[h % 2]
                    yin0 = mk(ssb, (2 * h) * 128, [[0, 16], [1, 128]])
                    yin1 = mk(tv, (2 * h) * 16, [[1, 16], [0, 128]])
                    E("dve", lambda e, yin0=yin0, yin1=yin1, zb=zb: e.tensor_tensor(out=zb[:], in0=yin0, in1=yin1, op=ALU.is_equal), r=[bSs, bTv], w=[bZb])
                    E("dve", lambda e, h=h, zb=zb: e.tensor_scalar(out=bigA[:, h * 16:(h + 1) * 16, :].rearrange("p a b -> p (a b)"), in0=zb[:].rearrange("p a b -> p (a b)"), scalar1=rZ[:, h:h + 1], scalar2=None, op0=ALU.mult), r=[bZb, bRZ], w=[bBigAh[h]])
                for _ in transposes(bigA, bBigAh, YT, bYTh):
                    pass
                for t4 in range(32):
                    P, bP = (PA, bA) if (t4 // 2) % 2 == 0 else (PB, bB)
                    half = t4 % 2
                    for j in range(4):
                        t = t4 * 4 + j
                        E("pe", lambda e, t=t, j=j, P=P, half=half: e.matmul(bank(P, half)[:, j * 128:(j + 1) * 128], lhsT=XT[:, :, t], rhs=YT[:, :, t], start=True, stop=True), r=[bXT] + bYTh, w=[bP[half]])
                    src = bank(P, half)
                    dst = bigA[:, t4 * 4:(t4 + 1) * 4, :].rearrange("p a b -> p (a b)")
                    if t4 % 2 == 0:
                        E("act", lambda e, src=src, dst=dst: e.copy(out=dst, in_=src), r=[bP[half]], w=bBigAh)
                    else:
                        E("dve", lambda e, src=src, dst=dst: e.tensor_copy(out=dst, in_=src), r=[bP[half]], w=bBigAh)
                if gi == 0:
                    dump("ssb", ssb[:], [128, 16, 128], bSs)
                    dump("tv", tv[:], [128, 16, 16], bTv)
                    dump("c8", c8[:], [128, 8, 16], bC8)
                    dump("Zs", Zs[:], [128, 8], bZs)
                    dump("GT", bigA[:], [128, 128, 128], bBigAh[0], BF16)

            def mainloop(gi):
                hT_, bh_ = h2Ts[gi % 2], bH2Tb[gi % 2]
                for j4 in range(32):
                    k = j4 % NUV
                    k2 = j4 % 2
                    S.dma("sp", lambda e, j4=j4, k=k: e.dma_start(out=utb[k][:], in_=uts_d[j4 * 4:(j4 + 1) * 4].rearrange("j p c e -> p j c e")), bUt[k], reads=[bUs[j4 // 2]], writes=[bUt[k]])
                    S.dma("sp", lambda e, j4=j4, k=k: e.dma_start(out=vb[k][:], in_=vs_d[j4 * 512:(j4 + 1) * 512, :].rearrange("(j p) d -> p j d", p=128)), bVb[k], reads=[bVs[j4 // 2]], writes=[bVb[k]])
                    for jj in range(4):
                        for c in range(8):
                            E("pe", lambda e, c=c, k=k, k2=k2, jj=jj: e.matmul(bank(PA, k2)[:, jj * 128:(jj + 1) * 128], lhsT=utb[k][:, jj, c, :], rhs=hT_[:, c, :], start=(c == 0), stop=(c == 7)), r=[bUt[k], bh_], w=[bA[k2]])
                    E("act", lambda e, k2=k2: e.activation(out=ga[k2][:], in_=bank(PA, k2), func=AF.Gelu), r=[bA[k2]], w=[bGa[k2]])
                    E("dve", lambda e, k2=k2, j4=j4: e.tensor_tensor(out=wT[k2][:], in0=ga[k2][:].rearrange("p (a b) -> p a b", a=4), in1=mk(bigA, j4 * 4, [[1, 4], [128, 128]]), op=ALU.mult), r=[bGa[k2]] + bBigAh, w=[bWT[k2]])
                    for jj in range(4):
                        j = j4 * 4 + jj
                        for half in range(2):
                            E("pe", lambda e, half=half, k=k, k2=k2, j=j, jj=jj: e.matmul(bank(PC, half), lhsT=wT[k2][:, jj, :], rhs=vb[k][:, jj, half * 512:(half + 1) * 512], start=(j == 0), stop=(j == 127)), r=[bWT[k2], bVb[k]], w=[bC[half]])
                    yield

            def epilogue(gi):
                r0 = gi * 128
                ot_, bo_ = x1ts[gi % 2], bX1b[gi % 2]
                if do_peer:
                    for half in range(2):
                        hs = slice(half * 512, (half + 1) * 512)
                        E("dve", lambda e, half=half, hs=hs: e.tensor_tensor(out=junk[:, hs], in0=bank(PC, half), in1=gt2b[:, hs], op=ALU.mult), r=[bC[half], bGt2, bJunk], w=[bJunk])
                    E("pool", lambda e: e.tensor_tensor(out=ot_[:], in0=junk[:], in1=ot_[:], op=ALU.add), r=[bJunk, bo_], w=[bo_])
                E("dve", lambda e: e.memset(ssq[:], 0.0), w=[bSsq])
                E("act", lambda e: e.activation(out=junk[:], in_=ot_[:], func=AF.Square, accum_out=ssq[:, 0:1]), r=[bo_, bSsq], w=[bJunk, bSsq])
                E("dve", lambda e: e.tensor_scalar(out=ssq[:], in0=ssq[:], scalar1=1.0 / D, scalar2=EPS, op0=ALU.mult, op1=ALU.add), r=[bSsq], w=[bSsq])
                E("act", lambda e: e.activation(out=ssq[:], in_=ssq[:], func=AF.Sqrt), r=[bSsq], w=[bSsq])
                E("dve", lambda e: e.reciprocal(out=ssq[:], in_=ssq[:]), r=[bSsq], w=[bSsq])
                E("dve", lambda e: e.scalar_tensor_tensor(out=ot_[:], in0=ot_[:], scalar=ssq[:, 0:1], in1=fwb[:], op0=ALU.mult, op1=ALU.mult), r=[bo_, bSsq, bFw], w=[bo_])
                final_ops.append(S.dma("sp", lambda e: e.dma_start(out=out_d[r0:r0 + 128, :], in_=ot_[:]), bo_, reads=[bo_]))

            def run_all(g):
                for _ in g:
                    pass

            if not do_peer:
                for gi in range(ngroups):
                    r0 = gi * 128
                    S.dma("sp", lambda e, r0=r0, gi=gi: e.dma_start(out=x1ts[gi % 2][:], in_=x1s_d[r0:r0 + 128, :]), bX1b[gi % 2], reads=[bX1s[gi]], writes=[bX1b[gi % 2]])
                    epilogue(gi)
            else:
                run_all(front(0))
                tail(0)
                if dbg:
                    dump("h2T", h2Ts[0][:], [128, 8, 128], bH2Tb[0], BF16)
                for gi in range(ngroups):
                    nxt = front(gi + 1) if gi + 1 < ngroups else iter(())
                    for it, _ in enumerate(mainloop(gi)):
                        for _k in range(1 if it < 17 else 4):
                            next(nxt, None)
                    run_all(nxt)
                    epilogue(gi)
                    if gi + 1 < ngroups:
                        tail(gi + 1)

            S.finalize(final_ops + dumps)
            S.run()
    return nc


def _prep_shared(w_ada, b_ada, norm1_w, w_in, hgrn_lb_logits, hgrn_onorm_w, conv_w, conv_onorm_w, w_out,
                 norm2_w, peer_w_query, peer_sub_keys, peer_u, peer_v, final_norm_w):
    f = np.float32
    c = np.ascontiguousarray

    def pm(w):
        return c(np.asarray(w, f).reshape(8, 128, -1).transpose(1, 0, 2))
    sh = {}
    sh["wada"] = pm(w_ada[0])
    sh["bada"] = c(np.asarray(b_ada, f).reshape(1, -1))
    sh["n1w"] = c(np.asarray(norm1_w, f).reshape(1, -1))
    sh["n2w"] = c(np.asarray(norm2_w, f).reshape(1, -1))
    sh["fw"] = c(np.asarray(final_norm_w, f).reshape(1, -1))
    sh["win"] = pm(w_in[0])
    sh["lbl"] = c(np.asarray(hgrn_lb_logits, f).reshape(2, 4, 128).transpose(2, 0, 1))
    sh["onw"] = c(np.asarray(hgrn_onorm_w, f).reshape(4, 128).T)
    sh["cnw"] = c(np.asarray(conv_onorm_w, f).reshape(4, 128).T)
    sh["cw"] = c(np.asarray(conv_w, f).reshape(3, 4, 128).transpose(2, 0, 1))
    sh["wout"] = pm(w_out[0])
    sh["wq"] = c(np.asarray(peer_w_query[0], f).reshape(8, 128, 16, 128).transpose(2, 1, 0, 3))
    sh["skt"] = c(np.asarray(peer_sub_keys, f).reshape(16, 128, 128).transpose(2, 0, 1))
    sh["ut"] = c(np.asarray(peer_u, f).reshape(128, 128, 8, 128).transpose(0, 3, 2, 1))
    sh["v"] = c(np.asarray(peer_v, f).reshape(16384, 1024))
    return sh


def _in_maps(x, c, sh, npre):
    f = np.float32
    x = np.asarray(x, f); cc = np.asarray(c, f)
    maps = []
    for core in range(8):
        b, jj = core // 4, core % 4
        m = dict(sh)
        m["x"] = np.ascontiguousarray(x[b, jj * TOK:(jj + 1) * TOK])
        xpre = np.zeros((max(npre, 1) * TOK, D), f)
        msk = np.zeros((128, 4), f)
        for s in range(npre):
            src = jj - npre + s
            if src >= 0:
                xpre[s * TOK:(s + 1) * TOK] = x[b, src * TOK:(src + 1) * TOK]
                msk[:, s] = 1.0
        msk[:, 3] = 1.0 if (jj >= 1 and npre >= 1) else 0.0
        m["xpre"] = xpre
        m["msk"] = msk
        m["cT"] = np.ascontiguousarray(cc[b].reshape(8, 128).T)
        maps.append(m)
    return maps


_NC_CACHE = {}


def kernel(x, c, w_ada, b_ada, norm1_w, w_in, hgrn_lb_logits, hgrn_onorm_w, conv_w, conv_onorm_w, w_out,
           norm2_w, peer_w_query, peer_sub_keys, peer_u, peer_v, final_norm_w):
    sh = _prep_shared(w_ada, b_ada, norm1_w, w_in, hgrn_lb_logits, hgrn_onorm_w, conv_w, conv_onorm_w, w_out,
                      norm2_w, peer_w_query, peer_sub_keys, peer_u, peer_v, final_norm_w)
    maps = _in_maps(x, c, sh, NPRE)
    nc = build_nc(NPRE)
    res = run_bass_kernel_spmd(nc, maps, core_ids=list(range(8)))
    out = np.zeros((2, 8192, D), np.float32)
    for core in range(8):
        b, jj = core // 4, core % 4
        out[b, jj * TOK:(jj + 1) * TOK] = res.results[core]["out"]
    return out
```

```python
import contextlib
import numpy as np
import concourse.bass as bass
import concourse.mybir as mybir
from concourse.bass_utils import run_bass_kernel_spmd

F32 = mybir.dt.float32
BF16 = mybir.dt.bfloat16
ALU = mybir.AluOpType
AF = mybir.ActivationFunctionType
STRICT_SAME_ENGINE = True


class Buf:
    __slots__ = ("name", "w", "r", "sem", "cnt")

    def __init__(self, name):
        self.name = name
        self.w = None
        self.r = {}
        self.sem = None
        self.cnt = 0


class Op:
    __slots__ = ("eng", "fn", "deps", "flag", "val", "sem", "isdma", "name")

    def __init__(self, eng, fn, isdma=False, name=""):
        self.eng = eng
        self.fn = fn
        self.deps = []
        self.flag = False
        self.val = None
        self.sem = None
        self.isdma = isdma
        self.name = name


class Sched:
    ENGS = ("pe", "act", "dve", "pool", "sp")

    def __init__(self, nc, stack):
        self.nc = nc
        self.stack = stack
        self.q = {e: [] for e in self.ENGS}
        self.esem = {e: stack.enter_context(nc.semaphore("s_" + e)) for e in self.ENGS}
        self.nsem = len(self.ENGS)
        self.ndma = 0

    def newsem(self, name):
        self.nsem += 1
        return self.stack.enter_context(self.nc.semaphore(name))

    def _deps(self, op, reads, writes):
        deps = []
        for b in reads:
            if b.w is not None:
                deps.append(b.w)
        for b in writes:
            if b.w is not None:
                deps.append(b.w)
            deps.extend(b.r.values())
        seen = set()
        for d in deps:
            if d is op or id(d) in seen:
                continue
            seen.add(id(d))
            if (not d.isdma) and d.eng == op.eng and (op.eng == "pe" or not STRICT_SAME_ENGINE):
                continue
            op.deps.append(d)
        for b in reads:
            key = ("dma", id(op)) if op.isdma else op.eng
            b.r[key] = op
        for b in writes:
            b.w = op
            b.r = {}

    def emit(self, eng, fn, reads=(), writes=(), name=""):
        op = Op(eng, fn, name=name)
        self._deps(op, reads, writes)
        self.q[eng].append(op)
        return op

    def dma(self, eng, fn, sbuf, reads=(), writes=(), nparts=1, name=""):
        op = Op(eng, fn, isdma=True, name=name)
        self._deps(op, reads, writes)
        if sbuf.sem is None:
            sbuf.sem = self.newsem("d_" + sbuf.name)
        sbuf.cnt += 16 * nparts
        op.sem = sbuf.sem
        op.val = sbuf.cnt
        self.q[eng].append(op)
        self.ndma += nparts
        return op

    def finalize(self, final_ops):
        for e in self.ENGS:
            for op in self.q[e]:
                for d in op.deps:
                    if not d.isdma:
                        d.flag = True
        for e in self.ENGS:
            c = 0
            for op in self.q[e]:
                if not op.isdma and op.flag:
                    c += 1
                    op.val = c
                    op.sem = self.esem[e]
        self.final_ops = final_ops

    def replay(self, eng, e):
        seen = {}
        nwait = 0
        for op in self.q[eng]:
            for d in op.deps:
                k = id(d.sem)
                if seen.get(k, 0) >= d.val:
                    continue
                seen[k] = d.val
                e.wait_ge(d.sem, d.val)
                nwait += 1
            r = op.fn(e)
            if op.isdma:
                lst = r if isinstance(r, (list, tuple)) else [r]
                for ins in lst:
                    ins.then_inc(op.sem, 16)
            elif op.flag:
                r.then_inc(op.sem, 1)
        if eng == "sp":
            for d in self.final_ops:
                e.wait_ge(d.sem, d.val)
        return nwait

    def run(self):
        nc = self.nc
        with nc.Block() as block:
            @block.sync
            def _(e):
                self.replay("sp", e)

            @block.scalar
            def _(e):
                self.replay("act", e)

            @block.vector
            def _(e):
                self.replay("dve", e)

            @block.gpsimd
            def _(e):
                self.replay("pool", e)

            @block.tensor
            def _(e):
                self.replay("pe", e)


def ap_of(t):
    return t if isinstance(t, bass.AP) else t[:]


def mk(base, offset_elems, dims):
    b = ap_of(base)
    pstep, pcnt = b.ap[0]
    return bass.AP(b.tensor, b.offset + offset_elems, [[pstep, pcnt]] + [list(d) for d in dims])

D = 1024
TOK = 2048
NPRE = 3
EPS = 1e-6
NEG = -1.0e30


def build_nc(npre=NPRE, dbg=False, do_peer=True, ngroups=16):
    nc = bass.Bass("TRN2", target_bir_lowering=False)

    def din(name, shape):
        return nc.dram_tensor(name, shape, F32, kind="ExternalInput").ap()

    x_d = din("x", [TOK, D])
    xpre_d = din("xpre", [max(npre, 1) * TOK, D])
    msk_d = din("msk", [128, 4])
    cT_d = din("cT", [128, 8])
    wada_d = din("wada", [128, 8, 6 * D])
    bada_d = din("bada", [1, 6 * D])
    n1w_d = din("n1w", [1, D])
    n2w_d = din("n2w", [1, D])
    fw_d = din("fw", [1, D])
    win_d = din("win", [128, 8, 3584])
    lbl_d = din("lbl", [128, 2, 4])
    onw_d = din("onw", [128, 4])
    cnw_d = din("cnw", [128, 4])
    cw_d = din("cw", [128, 3, 4])
    wout_d = din("wout", [128, 8, D])
    wq_d = din("wq", [16, 128, 8, 128])
    skt_d = din("skt", [128, 16, 128])
    ut_d = din("ut", [128, 128, 8, 128])
    v_d = din("v", [16384, D])
    out_d = nc.dram_tensor("out", [TOK, D], F32, kind="ExternalOutput").ap()
    x1s_d = nc.dram_tensor("x1s", [TOK, D], F32, kind=("ExternalOutput" if dbg else "Internal")).ap()
    uts_d = nc.dram_tensor("uts", [128, 128, 8, 128], BF16, kind="Internal").ap()
    vs_d = nc.dram_tensor("vs", [16384, D], BF16, kind="Internal").ap()
    wqs_d = nc.dram_tensor("wqs", [16, 128, 8, 128], BF16, kind="Internal").ap()

    with contextlib.ExitStack() as st:
        S = Sched(nc, st)

        def sb(name, shape, dt=F32):
            return st.enter_context(nc.sbuf_tensor("t_" + name, shape, dt))

        def ps(name, shape, dt=F32):
            return st.enter_context(nc.psum_tensor("t_" + name, shape, dt))

        def E(eng, fn, r=(), w=()):
            return S.emit(eng, fn, reads=r, writes=w)

        dumps = []

        def dump(name, ap, shape, buf, dt=F32):
            if not dbg:
                return
            dd = nc.dram_tensor("dbg_" + name, shape, dt, kind="ExternalOutput").ap()
            dumps.append(S.dma("sp", lambda e: e.dma_start(out=dd, in_=ap), buf, reads=[buf]))

        pT0 = ps("pT0", [128, 1024], BF16); bT0 = Buf("pT0")
        pT1 = ps("pT1", [128, 1024], BF16); bT1 = Buf("pT1")
        PA = ps("PA", [128, 1024], F32); bA = [Buf("PA0"), Buf("PA1")]
        PB = ps("PB", [128, 1024], F32); bB = [Buf("PB0"), Buf("PB1")]
        PC = ps("PC", [128, 1024], F32); bC = [Buf("PC0"), Buf("PC1")]

        def bank(P, i):
            return P[:, i * 512:(i + 1) * 512]

        identf = sb("identf", [128, 128]); ident = sb("ident", [128, 128], BF16); bId = Buf("ident")
        E("pool", lambda e: e.memset(identf[:], 1.0), w=[bId])
        E("pool", lambda e: e.affine_select(out=identf[:], in_=identf[:], pattern=[[-1, 128]], compare_op=ALU.is_equal, fill=0.0, base=0, channel_multiplier=1), r=[bId], w=[bId])
        E("pool", lambda e: e.tensor_copy(out=ident[:], in_=identf[:]), r=[bId], w=[bId])
        onesf = sb("onesf", [128, 128]); ones_bf = sb("ones_bf", [128, 128], BF16); bOn = Buf("ones")
        E("pool", lambda e: e.memset(onesf[:], 1.0), w=[bOn])
        E("pool", lambda e: e.tensor_copy(out=ones_bf[:], in_=onesf[:]), r=[bOn], w=[bOn])
        maskST = sb("maskST", [128, 128]); bMk = Buf("maskST")
        E("pool", lambda e: e.memset(maskST[:], 1.0), w=[bMk])
        E("pool", lambda e: e.affine_select(out=maskST[:], in_=maskST[:], pattern=[[1, 128]], compare_op=ALU.is_ge, fill=0.0, base=0, channel_multiplier=-1), r=[bMk], w=[bMk])
        maskI = sb("maskI", [128, 128], mybir.dt.int32)
        E("pool", lambda e: e.tensor_copy(out=maskI[:], in_=maskST[:]), r=[bMk], w=[bMk])

        small = {}

        def load_small(name, src, shape):
            t = sb(name, shape); b = Buf(name)
            S.dma("sp", lambda e: e.dma_start(out=t[:], in_=src), b, writes=[b])
            small[name] = (t, b)
            return t, b

        msk, bMsk = load_small("msk", msk_d, [128, 4])
        cT, bcT = load_small("cT", cT_d, [128, 8])
        lbl, bLbl = load_small("lbl", lbl_d, [128, 2, 4])
        onw, bOnw = load_small("onw", onw_d, [128, 4])
        cnw, bCnw = load_small("cnw", cnw_d, [128, 4])
        cw, bCw = load_small("cw", cw_d, [128, 3, 4])

        lb = sb("lb", [128, 4]); oml = sb("oml", [128, 4]); noml = sb("noml", [128, 4]); bLb = Buf("lb")
        E("dve", lambda e: e.tensor_tensor(out=lb[:], in0=lbl[:, 0, :], in1=lbl[:, 1, :], op=ALU.subtract), r=[bLbl], w=[bLb])
        E("act", lambda e: e.activation(out=lb[:], in_=lb[:], func=AF.Sigmoid), r=[bLb], w=[bLb])
        E("dve", lambda e: e.tensor_scalar(out=oml[:], in0=lb[:], scalar1=-1.0, scalar2=1.0, op0=ALU.mult, op1=ALU.add), r=[bLb], w=[bLb])
        E("dve", lambda e: e.tensor_scalar(out=noml[:], in0=lb[:], scalar1=-1.0, scalar2=None, op0=ALU.add), r=[bLb], w=[bLb])

        g2b = sb("g2b", [128, D]); fwb = sb("fwb", [128, D]); sh2b = sb("sh2b", [128, D]); gt2b = sb("gt2b", [128, D])
        bG2 = Buf("g2b"); bFw = Buf("fwb"); bSh2 = Buf("sh2b"); bGt2 = Buf("gt2b")
        junk = sb("junk", [128, D]); bJunk = Buf("junk")
        ssq = sb("ssq", [128, 1]); bSsq = Buf("ssq")
        hb = sb("hb", [128, D], BF16); bHb = Buf("hb")

        def norm_mod(xap, bX, gb, bG, shiftap, bShift):
            E("dve", lambda e: e.memset(ssq[:], 0.0), w=[bSsq])
            E("act", lambda e: e.activation(out=junk[:], in_=xap, func=AF.Square, accum_out=ssq[:, 0:1]), r=[bX, bSsq], w=[bJunk, bSsq])
            E("dve", lambda e: e.tensor_scalar(out=ssq[:], in0=ssq[:], scalar1=1.0 / D, scalar2=EPS, op0=ALU.mult, op1=ALU.add), r=[bSsq], w=[bSsq])
            E("act", lambda e: e.activation(out=ssq[:], in_=ssq[:], func=AF.Sqrt), r=[bSsq], w=[bSsq])
            E("dve", lambda e: e.reciprocal(out=ssq[:], in_=ssq[:]), r=[bSsq], w=[bSsq])
            E("dve", lambda e: e.scalar_tensor_tensor(out=junk[:], in0=xap, scalar=ssq[:, 0:1], in1=gb[:], op0=ALU.mult, op1=ALU.mult), r=[bX, bSsq, bG, bJunk], w=[bJunk])
            E("pool", lambda e: e.tensor_tensor(out=hb[:], in0=junk[:], in1=shiftap, op=ALU.add), r=[bJunk, bShift], w=[bHb])

        def transpose_hb(dst3, bDst):
            for c in range(8):
                E("pe", lambda e, c=c: e.transpose(out=pT0[:, c * 128:(c + 1) * 128], in_=hb[:, c * 128:(c + 1) * 128], identity=ident[:]), r=[bHb, bId], w=[bT0])
            E("act", lambda e: e.copy(out=dst3, in_=pT0[:].rearrange("p (c t) -> p c t", c=8)), r=[bT0], w=[bDst])

        with contextlib.ExitStack() as st2:
            def sb2(name, shape, dt=F32):
                return st2.enter_context(nc.sbuf_tensor("t_" + name, shape, dt))
            win = sb2("win", [128, 8, 3584], BF16); bWin = Buf("win")
            for c in range(8):
                S.dma("pool", lambda e, c=c: e.dma_start(out=win[:, c, :], in_=win_d[:, c, :]), bWin, writes=[bWin])
            wout = sb2("wout", [128, 8, D], BF16); bWout = Buf("wout")
            for c in range(0, 8, 4):
                S.dma("pool", lambda e, c=c: e.dma_start(out=wout[:, c:c + 4, :], in_=wout_d[:, c:c + 4, :]), bWout, writes=[bWout])
            bUs = [Buf("uts%d" % i) for i in range(16)]; bVs = [Buf("vs%d" % i) for i in range(16)]
            xt = sb2("xt", [128, 4, D]); bXt = [Buf("xt%d" % i) for i in range(4)]
            h1T = sb2("h1T", [128, 8, 512], BF16); bH1T = [Buf("h1T%d" % i) for i in range(4)]
            itok = sb2("itok", [128, 4, 512], BF16); bItok = [Buf("itok%d" % i) for i in range(4)]
            sg = sb2("sg", [128, 512]); bSg = Buf("sg")
            lf = sb2("lf", [128, 512]); bLf = Buf("lf")
            kk = sb2("kk", [128, 512]); bKk = Buf("kk")
            qs = sb2("qs", [128, 512]); bQs = Buf("qs")
            gs = sb2("gs", [128, 512]); bGs = Buf("gs")
            btc = [sb2("btc%d" % i, [128, 128]) for i in range(4)]; bBtc = [Buf("btc%d" % i) for i in range(4)]
            exc = [sb2("exc%d" % i, [128, 128]) for i in range(4)]; bExc = [Buf("exc%d" % i) for i in range(4)]
            khTc = [sb2("khTc%d" % i, [128, 128], BF16) for i in range(4)]; bKhTc = [Buf("khTc%d" % i) for i in range(4)]
            khc = [sb2("khc%d" % i, [128, 128], BF16) for i in range(4)]; bKhc = [Buf("khc%d" % i) for i in range(4)]
            qtc = [sb2("qtc%d" % i, [128, 128], BF16) for i in range(4)]; bQtc = [Buf("qtc%d" % i) for i in range(4)]
            ktc = [sb2("ktc%d" % i, [128, 128], BF16) for i in range(4)]; bKtc = [Buf("ktc%d" % i) for i in range(4)]
            qhc = [sb2("qhc%d" % i, [128, 128], BF16) for i in range(4)]; bQhc = [Buf("qhc%d" % i) for i in range(4)]
            scmc = [sb2("scmc%d" % i, [128, 128], BF16) for i in range(4)]; bScmc = [Buf("scmc%d" % i) for i in range(4)]
            ebc = sb2("ebc", [128, 4]); bEbc = [Buf("ebc%d" % i) for i in range(4)]
            nbmc = sb2("nbmc", [128, 4]); bNbmc = [Buf("nbmc%d" % i) for i in range(4)]
            bT1s = [Buf("pT1s%d" % i) for i in range(4)]; bB0s = [Buf("PB0s%d" % i) for i in range(4)]; bC0s = [Buf("PC0s%d" % i) for i in range(4)]
            bt = btc[0]; bBt = bBtc[0]
            Sst = sb2("Sst", [128, 4, 128]); bS = [Buf("S%d" % i) for i in range(4)]
            Sbf = sb2("Sbf", [128, 4, 128], BF16); bSbf = [Buf("Sbf%d" % i) for i in range(4)]
            sq = sb2("sq", [128, 512], BF16); bSq = Buf("sq")
            rb = sb2("rb", [128, 512]); bRb = Buf("rb")
            yv = sb2("yv", [128, 512]); bYv = Buf("yv")
            ymT = sb2("ymT", [128, 8, 512], BF16); bYm = [Buf("ym%d" % i) for i in range(8)]
            u = sb2("u", [128, 4, 514]); bU = [Buf("u%d" % i) for i in range(4)]
            csb = sg; bCsb = bSg
            y0 = lf; bY0 = bLf
            y1 = kk; bY1 = bKk
            t1 = junk; bTt1 = bJunk

            modb = sb2("modb", [128, 6 * D]); bMod = Buf("modb")
            cact = sb2("cact", [128, 8]); crep = sb2("crep", [128, 8, 128]); bCrep = Buf("crep")
            E("act", lambda e: e.activation(out=cact[:], in_=cT[:], func=AF.Silu), r=[bcT], w=[bCrep])
            for c in range(8):
                E("dve", lambda e, c=c: e.tensor_scalar(out=crep[:, c, :], in0=onesf[:], scalar1=cact[:, c:c + 1], scalar2=None, op0=ALU.mult), r=[bOn, bCrep], w=[bCrep])
            S.dma("sp", lambda e: e.dma_start(out=modb[:], in_=bada_d.partition_broadcast(128)), bMod, writes=[bMod])
            wst = [xt[:, 0:2, :].rearrange("p a (b n) -> p (a b) n", n=256), xt[:, 2:4, :].rearrange("p a (b n) -> p (a b) n", n=256)]
            bWst = [[bXt[0], bXt[1]], [bXt[2], bXt[3]]]
            for g in range(24):
                k = g % 2
                S.dma("sp", lambda e, g=g, k=k: e.dma_start(out=wst[k], in_=wada_d[:, :, g * 256:(g + 1) * 256]), bWst[k][0], writes=bWst[k])
                for c in range(8):
                    E("pe", lambda e, c=c, k=k: e.matmul(bank(PA, k)[:, 0:256], lhsT=crep[:, c, :], rhs=wst[k][:, c, :], start=(c == 0), stop=(c == 7)), r=[bCrep] + bWst[k], w=[bA[k]])
                E("dve", lambda e, g=g, k=k: e.tensor_tensor(out=modb[:, g * 256:(g + 1) * 256], in0=bank(PA, k)[:, 0:256], in1=modb[:, g * 256:(g + 1) * 256], op=ALU.add), r=[bA[k], bMod], w=[bMod])
            g1b = sb2("g1b", [128, D]); bG1 = Buf("g1b")
            S.dma("sp", lambda e: e.dma_start(out=g1b[:], in_=n1w_d.partition_broadcast(128)), bG1, writes=[bG1])
            S.dma("sp", lambda e: e.dma_start(out=g2b[:], in_=n2w_d.partition_broadcast(128)), bG2, writes=[bG2])
            S.dma("sp", lambda e: e.dma_start(out=fwb[:], in_=fw_d.partition_broadcast(128)), bFw, writes=[bFw])
            E("dve", lambda e: e.scalar_tensor_tensor(out=g1b[:], in0=modb[:, D:2 * D], scalar=1.0, in1=g1b[:], op0=ALU.add, op1=ALU.mult), r=[bMod, bG1], w=[bG1])
            E("dve", lambda e: e.scalar_tensor_tensor(out=g2b[:], in0=modb[:, 4 * D:5 * D], scalar=1.0, in1=g2b[:], op0=ALU.add, op1=ALU.mult), r=[bMod, bG2], w=[bG2])
            shift1 = modb[:, 0:D]; gate1 = modb[:, 2 * D:3 * D]
            dump("modb", modb[:], [128, 6 * D], bMod)
            dump("g1b", g1b[:], [128, D], bG1)
            dump("lb", lb[:], [128, 4], bLb)
            E("dve", lambda e: e.tensor_copy(out=sh2b[:], in_=modb[:, 3 * D:4 * D]), r=[bMod], w=[bSh2])
            E("dve", lambda e: e.tensor_copy(out=gt2b[:], in_=modb[:, 5 * D:6 * D]), r=[bMod], w=[bGt2])

            bWqs = [Buf("wqs%d" % i) for i in range(2)]
            if do_peer:
                for i in range(2):
                    S.dma("pool", lambda e, i=i: e.dma_start(out=wqs_d[i * 8:(i + 1) * 8], in_=wq_d[i * 8:(i + 1) * 8]), bWqs[i], reads=[bSh2, bGt2, bG1], writes=[bWqs[i]])
                for i in range(16):
                    S.dma("pool", lambda e, i=i: e.dma_start(out=uts_d[i * 8:(i + 1) * 8], in_=ut_d[i * 8:(i + 1) * 8]), bUs[i], reads=[bSh2, bGt2, bG1], writes=[bUs[i]])
                    S.dma("pool", lambda e, i=i: e.dma_start(out=vs_d[i * 1024:(i + 1) * 1024, :], in_=v_d[i * 1024:(i + 1) * 1024, :]), bVs[i], reads=[bSh2, bGt2, bG1], writes=[bVs[i]])
            for ck in range(4):
                E("pool", lambda e, ck=ck: e.memset(scmc[ck][:], 0.0), w=[bScmc[ck]])
            for h in range(4):
                E("dve", lambda e, h=h: e.memset(Sst[:, h, :], 0.0), w=[bS[h]])
                E("pool", lambda e, h=h: e.memset(Sbf[:, h, :], 0.0), w=[bSbf[h]])
                E("pool", lambda e, h=h: e.memset(u[:, h, 0:2], 0.0), w=[bU[h]])

            rot = [0]

            def projbank():
                k = rot[0] % 2
                rot[0] += 1
                return k

            def proj_fm(col0, n):
                k = projbank()
                for c in range(8):
                    E("pe", lambda e, c=c, k=k: e.matmul(bank(PA, k)[:, 0:n], lhsT=win[:, c, col0:col0 + 128], rhs=h1T[:, c, 0:n], start=(c == 0), stop=(c == 7)), r=[bWin] + bH1T, w=[bA[k]])
                return bank(PA, k)[:, 0:n], bA[k]

            def hgrn_head(h, ntile, mode):
                n = ntile * 128
                fp, bf_ = proj_fm(512 + h * 128, n)
                E("act", lambda e: e.activation(out=sg[:, 0:n], in_=fp, func=AF.Sigmoid), r=[bf_], w=[bSg])
                E("act", lambda e: e.activation(out=lf[:, 0:n], in_=sg[:, 0:n], func=AF.Ln, scale=oml[:, h:h + 1], bias=lb[:, h:h + 1]), r=[bSg, bLb], w=[bLf])
                E("dve", lambda e: e.tensor_scalar(out=kk[:, 0:n], in0=sg[:, 0:n], scalar1=noml[:, h:h + 1], scalar2=oml[:, h:h + 1], op0=ALU.mult, op1=ALU.add), r=[bSg, bLb], w=[bKk])
                if mode == "main":
                    qp, bq_ = proj_fm(h * 128, n)
                    E("act", lambda e: e.copy(out=qs[:, 0:n], in_=qp), r=[bq_], w=[bQs])
                    gp, bg_ = proj_fm(1536 + h * 128, n)
                    E("act", lambda e: e.activation(out=gs[:, 0:n], in_=gp, func=AF.Silu), r=[bg_], w=[bGs])
                NT = ntile
                css = [slice(ck * 128, (ck + 1) * 128) for ck in range(NT)]
                isls = [itok[:, ck, h * 128:(h + 1) * 128] for ck in range(NT)]
                for ck in range(NT):
                    E("dve", lambda e, ck=ck: e.tensor_tensor_scan(out=btc[ck][:], data0=onesf[:], data1=lf[:, css[ck]], initial=0.0, op0=ALU.mult, op1=ALU.add), r=[bOn, bLf], w=[bBtc[ck]])
                for ck in range(NT):
                    E("act", lambda e, ck=ck: e.activation(out=exc[ck][:], in_=btc[ck][:], func=AF.Exp, scale=-1.0, bias=btc[ck][:, 127:128]), r=[bBtc[ck]], w=[bExc[ck]])
                for ck in range(NT):
                    E("dve", lambda e, ck=ck: e.tensor_tensor(out=khTc[ck][:], in0=kk[:, css[ck]], in1=exc[ck][:], op=ALU.mult), r=[bKk, bExc[ck]], w=[bKhTc[ck]])
                for ck in range(NT):
                    E("pe", lambda e, ck=ck: e.transpose(out=pT1[:, ck * 128:(ck + 1) * 128], in_=khTc[ck][:], identity=ident[:]), r=[bKhTc[ck], bId], w=[bT1])
                for ck in range(NT):
                    E("act", lambda e, ck=ck: e.copy(out=khc[ck][:], in_=pT1[:, ck * 128:(ck + 1) * 128]), r=[bT1], w=[bKhc[ck]])
                for ck in range(NT):
                    E("pe", lambda e, ck=ck: e.matmul(bank(PC, 0)[:, css[ck]], lhsT=khc[ck][:], rhs=isls[ck], start=True, stop=True), r=[bKhc[ck], bItok[ck]], w=[bC[0]])
                for ck in range(NT):
                    E("act", lambda e, ck=ck: e.activation(out=ebc[:, ck:ck + 1], in_=btc[ck][:, 127:128], func=AF.Exp), r=[bBtc[ck]], w=[bEbc[ck]])
                if mode == "main":
                    for ck in range(NT):
                        E("dve", lambda e, ck=ck: e.tensor_scalar(out=nbmc[:, ck:ck + 1], in0=btc[ck][:, 63:64], scalar1=-1.0, scalar2=None, op0=ALU.mult), r=[bBtc[ck]], w=[bNbmc[ck]])
                    for ck in range(NT):
                        E("act", lambda e, ck=ck: e.activation(out=exc[ck][:], in_=btc[ck][:], func=AF.Exp, bias=nbmc[:, ck:ck + 1]), r=[bBtc[ck], bNbmc[ck], bExc[ck]], w=[bExc[ck]])
                    for ck in range(NT):
                        E("dve", lambda e, ck=ck: e.tensor_tensor(out=qtc[ck][:], in0=qs[:, css[ck]], in1=exc[ck][:], op=ALU.mult), r=[bQs, bExc[ck]], w=[bQtc[ck]])
                    for ck in range(NT):
                        E("act", lambda e, ck=ck: e.activation(out=exc[ck][:], in_=btc[ck][:], func=AF.Exp, scale=-1.0, bias=btc[ck][:, 63:64]), r=[bBtc[ck], bExc[ck]], w=[bExc[ck]])
                    for ck in range(NT):
                        E("pool", lambda e, ck=ck: e.tensor_tensor(out=ktc[ck][:], in0=kk[:, css[ck]], in1=exc[ck][:], op=ALU.mult), r=[bKk, bExc[ck]], w=[bKtc[ck]])
                    for ck in range(NT):
                        E("act", lambda e, ck=ck: e.activation(out=exc[ck][:], in_=btc[ck][:], func=AF.Exp), r=[bBtc[ck], bExc[ck]], w=[bExc[ck]])
                    for ck in range(NT):
                        E("dve", lambda e, ck=ck: e.tensor_tensor(out=qhc[ck][:], in0=qs[:, css[ck]], in1=exc[ck][:], op=ALU.mult), r=[bQs, bExc[ck]], w=[bQhc[ck]])
                    for ck in range(NT):
                        E("pe", lambda e, ck=ck: e.matmul(bank(PB, 0)[:, css[ck]], lhsT=ktc[ck][:], rhs=qtc[ck][:], start=True, stop=True), r=[bKtc[ck], bQtc[ck]], w=[bB[0]])
                    for ck in range(NT):
                        E("dve", lambda e, ck=ck: e.copy_predicated(out=scmc[ck][:], mask=maskI[:], data=bank(PB, 0)[:, css[ck]]), r=[bB[0], bMk, bScmc[ck]], w=[bScmc[ck]])
                for ck in range(NT):
                    if mode == "main":
                        E("pe", lambda e, ck=ck: e.matmul(bank(PB, 1)[:, css[ck]], lhsT=isls[ck], rhs=scmc[ck][:], start=True, stop=False), r=[bItok[ck], bScmc[ck]], w=[bB[1]])
                        E("pe", lambda e, ck=ck: e.matmul(bank(PB, 1)[:, css[ck]], lhsT=Sbf[:, h, :], rhs=qhc[ck][:], start=False, stop=True), r=[bSbf[h], bQhc[ck]], w=[bB[1]])
                    E("dve", lambda e, ck=ck: e.scalar_tensor_tensor(out=Sst[:, h, :], in0=Sst[:, h, :], scalar=ebc[:, ck:ck + 1], in1=bank(PC, 0)[:, css[ck]], op0=ALU.mult, op1=ALU.add), r=[bS[h], bEbc[ck], bC[0]], w=[bS[h]])
                    if mode == "main":
                        E("pool", lambda e: e.tensor_copy(out=Sbf[:, h, :], in_=Sst[:, h, :]), r=[bS[h]], w=[bSbf[h]])
                if mode == "main":
                    groupnorm_out(bank(PB, 1), bB[1], onw[:, h:h + 1], bOnw, h, mulgs=True)

            def groupnorm_out(src, bSrc, wcol, bW, slot, mulgs):
                E("act", lambda e: e.activation(out=sq[:], in_=src, func=AF.Square), r=[bSrc], w=[bSq])
                E("pe", lambda e: e.matmul(bank(PC, 1), lhsT=ones_bf[:], rhs=sq[:], start=True, stop=True), r=[bOn, bSq], w=[bC[1]])
                E("dve", lambda e: e.tensor_scalar(out=rb[:], in0=bank(PC, 1), scalar1=1.0 / 128, scalar2=EPS, op0=ALU.mult, op1=ALU.add), r=[bC[1]], w=[bRb])
                E("act", lambda e: e.activation(out=rb[:], in_=rb[:], func=AF.Sqrt), r=[bRb], w=[bRb])
                E("dve", lambda e: e.reciprocal(out=rb[:], in_=rb[:]), r=[bRb], w=[bRb])
                if mulgs:
                    E("dve", lambda e: e.scalar_tensor_tensor(out=yv[:], in0=src, scalar=wcol, in1=rb[:], op0=ALU.mult, op1=ALU.mult), r=[bSrc, bW, bRb], w=[bYv])
                    E("pool", lambda e: e.tensor_tensor(out=ymT[:, slot, :], in0=yv[:], in1=gs[:], op=ALU.mult), r=[bYv, bGs], w=[bYm[slot]])
                else:
                    E("dve", lambda e: e.scalar_tensor_tensor(out=ymT[:, slot, :], in0=src, scalar=wcol, in1=rb[:], op0=ALU.mult, op1=ALU.mult), r=[bSrc, bW, bRb], w=[bYm[slot]])

            def conv_group(g, mode):
                if mode == "halo":
                    cp, bc_ = proj_fm(2560 + g * 128, 128)
                    E("act", lambda e: e.copy(out=csb[:, 0:128], in_=cp), r=[bc_], w=[bCsb])
                    xp, bx_ = proj_fm(3072 + g * 128, 128)
                    E("dve", lambda e: e.tensor_tensor(out=y0[:, 0:128], in0=csb[:, 0:128], in1=xp, op=ALU.mult), r=[bCsb, bx_], w=[bY0])
                    E("dve", lambda e: e.tensor_scalar(out=u[:, g, 0:2], in0=y0[:, 126:128], scalar1=msk[:, 3:4], scalar2=None, op0=ALU.mult), r=[bY0, bMsk], w=[bU[g]])
                    return
                cp, bc_ = proj_fm(2560 + g * 128, 512)
                E("act", lambda e: e.copy(out=csb[:], in_=cp), r=[bc_], w=[bCsb])
                xp, bx_ = proj_fm(3072 + g * 128, 512)
                E("dve", lambda e: e.tensor_tensor(out=u[:, g, 2:514], in0=csb[:], in1=xp, op=ALU.mult), r=[bCsb, bx_], w=[bU[g]])
                bp, bb_ = proj_fm(2048 + g * 128, 512)
                E("dve", lambda e: e.tensor_scalar(out=y0[:], in0=u[:, g, 2:514], scalar1=cw[:, 2, g:g + 1], scalar2=None, op0=ALU.mult), r=[bU[g], bCw], w=[bY0])
                E("dve", lambda e: e.scalar_tensor_tensor(out=y1[:], in0=u[:, g, 1:513], scalar=cw[:, 1, g:g + 1], in1=y0[:], op0=ALU.mult, op1=ALU.add), r=[bU[g], bCw, bY0], w=[bY1])
                E("dve", lambda e: e.scalar_tensor_tensor(out=y0[:], in0=u[:, g, 0:512], scalar=cw[:, 0, g:g + 1], in1=y1[:], op0=ALU.mult, op1=ALU.add), r=[bU[g], bCw, bY1], w=[bY0])
                E("dve", lambda e: e.tensor_tensor(out=y1[:], in0=y0[:], in1=bp, op=ALU.mult), r=[bY0, bb_], w=[bY1])
                E("pool", lambda e: e.tensor_copy(out=u[:, g, 0:2], in_=u[:, g, 512:514]), r=[bU[g]], w=[bU[g]])
                groupnorm_out(y1[:], bY1, cnw[:, g:g + 1], bCnw, 4 + g, mulgs=False)

            def mixer_macro(src_d, row0, ntile, mode):
                n = ntile * 128
                for tt in range(ntile):
                    S.dma("sp", lambda e, tt=tt: e.dma_start(out=xt[:, tt, :], in_=src_d[row0 + tt * 128: row0 + (tt + 1) * 128, :]), bXt[tt], writes=[bXt[tt]])
                    norm_mod(xt[:, tt, :], bXt[tt], g1b, bG1, shift1, bMod)
                    transpose_hb(h1T[:, :, tt * 128:(tt + 1) * 128], bH1T[tt])
                if mode == "halo":
                    for g in range(4):
                        conv_group(g, "halo")
                    return
                for tt in range(ntile):
                    k = projbank()
                    for c in range(8):
                        E("pe", lambda e, c=c, k=k, tt=tt: e.matmul(bank(PA, k), lhsT=h1T[:, c, tt * 128:(tt + 1) * 128], rhs=win[:, c, 1024:1536], start=(c == 0), stop=(c == 7)), r=[bWin, bH1T[tt]], w=[bA[k]])
                    E("act", lambda e, k=k, tt=tt: e.copy(out=itok[:, tt, :], in_=bank(PA, k)), r=[bA[k]], w=[bItok[tt]])
                for h in range(4):
                    hgrn_head(h, ntile, mode)
                if mode != "main":
                    return
                for g in range(4):
                    conv_group(g, "main")
                for tt in range(ntile):
                    for half in range(2):
                        for cc in range(8):
                            E("pe", lambda e, cc=cc, half=half, tt=tt: e.matmul(bank(PA, half), lhsT=ymT[:, cc, tt * 128:(tt + 1) * 128], rhs=wout[:, cc, half * 512:(half + 1) * 512], start=(cc == 0), stop=(cc == 7)), r=[bYm[cc], bWout], w=[bA[half]])
                    for half in range(2):
                        hs = slice(half * 512, (half + 1) * 512)
                        E("dve", lambda e, half=half, hs=hs: e.tensor_tensor(out=t1[:, hs], in0=bank(PA, half), in1=gate1[:, hs], op=ALU.mult), r=[bA[half], bMod, bTt1], w=[bTt1])
                    E("pool", lambda e, tt=tt: e.tensor_tensor(out=xt[:, tt, :], in0=t1[:], in1=xt[:, tt, :], op=ALU.add), r=[bTt1, bXt[tt]], w=[bXt[tt]])
                    S.dma("sp", lambda e, tt=tt: e.dma_start(out=x1s_d[row0 + tt * 128: row0 + (tt + 1) * 128, :], in_=xt[:, tt, :]), bXt[tt], reads=[bXt[tt]], writes=[bX1s[(row0 + tt * 128) // 128]])

            bX1s = [Buf("x1s%d" % i) for i in range(16)]
            for seg in range(npre):
                for m in range(4):
                    mixer_macro(xpre_d, seg * TOK + m * 512, 4, "pre")
                for h in range(4):
                    E("dve", lambda e, h=h, seg=seg: e.tensor_scalar(out=Sst[:, h, :], in0=Sst[:, h, :], scalar1=msk[:, seg:seg + 1], scalar2=None, op0=ALU.mult), r=[bS[h], bMsk], w=[bS[h]])
            if npre > 0:
                mixer_macro(xpre_d, npre * TOK - 128, 1, "halo")
            for h in range(4):
                E("pool", lambda e, h=h: e.tensor_copy(out=Sbf[:, h, :], in_=Sst[:, h, :]), r=[bS[h]], w=[bSbf[h]])
            for m in range(4):
                mixer_macro(x_d, m * 512, 4, "main")
                if m == 0:
                    dump("ymT", ymT[:], [128, 8, 512], bYm[7], BF16)
                    dump("h1T", h1T[:], [128, 8, 512], bH1T[3], BF16)
                    dump("itok", itok[:], [128, 4, 512], bItok[3], BF16)
                    dump("Sst", Sst[:], [128, 4, 128], bS[3])
                    dump("u", u[:], [128, 4, 514], bU[3])
            mixer_tail_bufs = [bWin, bWout] + bXt + bH1T + bItok + [bSg, bLf, bKk, bQs, bGs] + bBtc + bExc + bKhTc + bKhc + bQtc + bKtc + bQhc + bScmc + bEbc + bNbmc + bT1s + bB0s + bC0s + bS + bSbf + [bSq, bRb, bYv] + bYm + bU + [bMod, bG1, bCrep]

        final_ops = []
        with contextlib.ExitStack() as st3:
            def sb3(name, shape, dt=F32):
                return st3.enter_context(nc.sbuf_tensor("t_" + name, shape, dt))
            bFence = Buf("fence")
            E("dve", lambda e: e.memset(ssq[:], 0.0), w=mixer_tail_bufs + [bFence, bSsq])
            E("act", lambda e: e.copy(out=junk[:, 0:1], in_=ssq[:]), r=[bFence, bSsq], w=[bFence, bJunk])
            E("pool", lambda e: e.tensor_copy(out=junk[:, 1:2], in_=ssq[:]), r=[bFence, bSsq], w=[bFence, bJunk])
            E("pe", lambda e: e.transpose(out=pT0[:, 0:128], in_=ident[:], identity=ident[:]), r=[bFence, bId], w=[bFence, bT0])
            S.dma("sp", lambda e: e.dma_start(out=msk[:], in_=msk_d), bMsk, reads=[bFence], writes=[bFence, bMsk])
            S.dma("pool", lambda e: e.dma_start(out=msk[:], in_=msk_d), bMsk, reads=[bFence], writes=[bFence, bMsk])

            skt = sb3("skt", [128, 16, 128], BF16); bSkt = Buf("skt")
            S.dma("pool", lambda e: e.dma_start(out=skt[:], in_=skt_d), bSkt, reads=[bFence], writes=[bSkt])
            wqb = [sb3("wqb%d" % i, [128, 8, 128], BF16) for i in range(2)]; bWq = [Buf("wqb%d" % i) for i in range(2)]
            x1t = sb3("x1t", [128, D]); bX1 = Buf("x1t")
            h2T = sb3("h2T", [128, 8, 128], BF16); bH2T = Buf("h2T")
            qT = sb3("qT", [128, 16, 128], BF16); bQT = Buf("qT")
            ssb = sb3("ssb", [128, 16, 128]); bSs = Buf("ssb")
            stmp = sb3("stmp", [128, 16, 128]); bStmp = Buf("stmp")
            tv = sb3("tv", [128, 16, 16]); bTv = Buf("tv")
            c8 = sb3("c8", [128, 8, 16]); bC8 = Buf("c8")
            negm = sb3("negm", [128, 8]); bNegm = Buf("negm")
            Zs = sb3("Zs", [128, 8]); bZs = Buf("Zs")
            rZ = sb3("rZ", [128, 8]); bRZ = Buf("rZ")
            ez = sb3("ez", [128, 16, 128]); bEz = Buf("ez")
            cand = stmp[:].rearrange("p (h a) b -> p h (a b)", a=2); bCand = bStmp
            cand2 = ez[:].rearrange("p (h a) b -> p h (a b)", a=2); bCand2 = bEz
            bigA = sb3("bigA", [128, 128, 128], BF16); bBigAh = [Buf("bigA%d" % i) for i in range(8)]
            XT = sb3("XT", [128, 128, 128], BF16); bXT = Buf("XT")
            YT = sb3("YT", [128, 128, 128], BF16); bYT = Buf("YT")
            NUV = 2
            utb = [sb3("utb%d" % i, [128, 4, 8, 128], BF16) for i in range(NUV)]; bUt = [Buf("utb%d" % i) for i in range(NUV)]
            vb = [sb3("vb%d" % i, [128, 4, D], BF16) for i in range(NUV)]; bVb = [Buf("vb%d" % i) for i in range(NUV)]
            ga = [sb3("ga%d" % i, [128, 512], BF16) for i in range(2)]; bGa = [Buf("ga%d" % i) for i in range(2)]
            wT = [sb3("wT%d" % i, [128, 4, 128], BF16) for i in range(2)]; bWT = [Buf("wT%d" % i) for i in range(2)]

            x1ts = [x1t, sb3("x1tB", [128, D])]; bX1b = [bX1, Buf("x1tB")]
            h2Ts = [h2T, sb3("h2TB", [128, 8, 128], BF16)]; bH2Tb = [bH2T, Buf("h2TB")]
            bYTh = [Buf("YTh%d" % i) for i in range(8)]
            zbufs = [(stmp, bStmp), (ez, bEz)]

            def transposes(srcTok, bSrc, dstT, bDstW):
                for i8 in range(16):
                    pt, bpt = (pT0, bT0) if i8 % 2 == 0 else (pT1, bT1)
                    for j in range(8):
                        i = i8 * 8 + j
                        E("pe", lambda e, i=i, j=j, pt=pt: e.transpose(out=pt[:, j * 128:(j + 1) * 128], in_=srcTok[:, :, i], identity=ident[:]), r=bSrc + [bId], w=[bpt])
                    src = pt[:]
                    dst = dstT[:, i8 * 8:(i8 + 1) * 8, :].rearrange("p a b -> p (a b)")
                    if i8 % 2 == 0:
                        E("act", lambda e, src=src, dst=dst: e.copy(out=dst, in_=src), r=[bpt], w=bDstW)
                    else:
                        E("dve", lambda e, src=src, dst=dst: e.tensor_copy(out=dst, in_=src), r=[bpt], w=bDstW)
                    yield

            def front(gi):
                r0 = gi * 128
                xt_, bx_ = x1ts[gi % 2], bX1b[gi % 2]
                hT_, bh_ = h2Ts[gi % 2], bH2Tb[gi % 2]
                S.dma("act", lambda e: e.dma_start(out=xt_[:], in_=x1s_d[r0:r0 + 128, :]), bx_, reads=[bX1s[gi]], writes=[bx_])
                norm_mod(xt_[:], bx_, g2b, bG2, sh2b[:], bSh2)
                transpose_hb(hT_[:], bh_)
                yield
                S.dma("act", lambda e: e.dma_start(out=wqb[0][:], in_=wqs_d[0]), bWq[0], reads=[bWqs[0]], writes=[bWq[0]])
                for hp in range(16):
                    k = hp % 2
                    if hp + 1 < 16:
                        S.dma("act", lambda e, hp=hp: e.dma_start(out=wqb[(hp + 1) % 2][:], in_=wqs_d[hp + 1]), bWq[(hp + 1) % 2], reads=[bWqs[(hp + 1) // 8]], writes=[bWq[(hp + 1) % 2]])
                    bk = bB[(hp // 4) % 2]
                    dst = bank(PB, (hp // 4) % 2)[:, (hp % 4) * 128:(hp % 4 + 1) * 128]
                    for c in range(8):
                        E("pe", lambda e, c=c, k=k, dst=dst: e.matmul(dst, lhsT=wqb[k][:, c, :], rhs=hT_[:, c, :], start=(c == 0), stop=(c == 7)), r=[bWq[k], bh_], w=[bk])
                    if hp % 4 == 3:
                        q4 = hp // 4
                        E("act", lambda e, q4=q4: e.copy(out=qT[:, q4 * 4:(q4 + 1) * 4, :], in_=bank(PB, q4 % 2).rearrange("p (a t) -> p a t", a=4)), r=[bk], w=[bQT])
                    yield
                for hp in range(16):
                    bk = bB[(hp // 4) % 2]
                    dst = bank(PB, (hp // 4) % 2)[:, (hp % 4) * 128:(hp % 4 + 1) * 128]
                    E("pe", lambda e, hp=hp, dst=dst: e.matmul(dst, lhsT=qT[:, hp, :], rhs=skt[:, hp, :], start=True, stop=True), r=[bQT, bSkt], w=[bk])
                    if hp % 4 == 3:
                        q4 = hp // 4
                        E("act", lambda e, q4=q4: e.copy(out=ssb[:, q4 * 4:(q4 + 1) * 4, :], in_=bank(PB, q4 % 2).rearrange("p (a t) -> p a t", a=4)), r=[bk], w=[bSs])
                        yield
                bTvh = [Buf("tvh%d" % i) for i in range(16)]; bStmph = [Buf("stmph%d" % i) for i in range(16)]
                for hp in range(16):
                    E("dve", lambda e, hp=hp: e.max(out=tv[:, hp, 0:8], in_=ssb[:, hp, :]), r=[bSs, bTv], w=[bTvh[hp]])
                    if hp % 4 == 3:
                        yield
                for hp in range(16):
                    E("dve", lambda e, hp=hp: e.match_replace(out=stmp[:, hp, :], in_to_replace=tv[:, hp, 0:8], in_values=ssb[:, hp, :], imm_value=NEG), r=[bSs, bTvh[hp], bStmp], w=[bStmph[hp]])
                    if hp % 4 == 3:
                        yield
                for hp in range(16):
                    E("dve", lambda e, hp=hp: e.max(out=tv[:, hp, 8:16], in_=stmp[:, hp, :]), r=[bStmph[hp]], w=[bTvh[hp]])
                    if hp % 4 == 3:
                        yield
                E("dve", lambda e: e.memset(ssq[:], 0.0), r=bTvh + bStmph, w=[bTv, bStmp, bSsq])
                in0 = mk(tv, 0, [[32, 8], [1, 16], [0, 16]])
                in1 = mk(tv, 16, [[32, 8], [0, 16], [1, 16]])
                E("dve", lambda e: e.tensor_tensor(out=cand.rearrange("p h (a b) -> p h a b", a=16), in0=in0, in1=in1, op=ALU.add), r=[bTv], w=[bCand])
                yield
                bC8h = [Buf("c8h%d" % i) for i in range(8)]; bCand2h = [Buf("cand2h%d" % i) for i in range(8)]
                for h in range(8):
                    E("dve", lambda e, h=h: e.max(out=c8[:, h, 0:8], in_=cand[:, h, :]), r=[bCand, bC8], w=[bC8h[h]])
                yield
                for h in range(8):
                    E("dve", lambda e, h=h: e.match_replace(out=cand2[:, h, :], in_to_replace=c8[:, h, 0:8], in_values=cand[:, h, :], imm_value=NEG), r=[bCand, bC8h[h], bCand2], w=[bCand2h[h]])
                yield
                for h in range(8):
                    E("dve", lambda e, h=h: e.max(out=c8[:, h, 8:16], in_=cand2[:, h, :]), r=[bCand2h[h]], w=[bC8h[h]])
                E("dve", lambda e: e.memset(ssq[:], 0.0), r=bC8h + bCand2h, w=[bC8, bCand2, bSsq])
                E("dve", lambda e: e.tensor_scalar(out=negm[:], in0=c8[:, :, 0], scalar1=-1.0, scalar2=None, op0=ALU.mult), r=[bC8], w=[bNegm])
                E("dve", lambda e: e.memset(Zs[:], 0.0), w=[bZs])
                yield
                for h in range(8):
                    zb, bZb = zbufs[h % 2]
                    zin0 = mk(ssb, (2 * h + 1) * 128, [[0, 16], [1, 128]])
                    zin1 = mk(tv, (2 * h) * 16, [[1, 16], [0, 128]])
                    slot = YT[:, h * 16:(h + 1) * 16, :].rearrange("p a b -> p (a b)")
                    E("pool", lambda e, zin0=zin0, zin1=zin1, zb=zb: e.tensor_tensor(out=zb[:], in0=zin0, in1=zin1, op=ALU.add), r=[bSs, bTv], w=[bZb])
                    E("act", lambda e, h=h, zb=zb, slot=slot: e.activation(out=slot, in_=zb[:].rearrange("p a b -> p (a b)"), func=AF.Exp, bias=negm[:, h:h + 1]), r=[bZb, bNegm], w=[bYTh[h]])
                    E("dve", lambda e, h=h, zb=zb, slot=slot: e.scalar_tensor_tensor(out=slot, in0=zb[:].rearrange("p a b -> p (a b)"), scalar=c8[:, h, 15:16], in1=slot, op0=ALU.is_ge, op1=ALU.mult, accum_out=Zs[:, h:h + 1]), r=[bZb, bC8, bYTh[h], bZs], w=[bYTh[h], bZs])
                    yield
                E("dve", lambda e: e.reciprocal(out=rZ[:], in_=Zs[:]), r=[bZs], w=[bRZ])
                yield from transposes(YT, bYTh, XT, [bXT])

            def tail(gi):
                for h in range(8):
                    zb, bZb = zbufs[h % 2]
                    yin0 = mk(ssb, (2 * h) * 128, [[0, 16], [1, 128]])
                    yin1 = mk(tv, (2 * h) * 16, [[1, 16], [0, 128]])
                    E("dve", lambda e, yin0=yin0, yin1=yin1, zb=zb: e.tensor_tensor(out=zb[:], in0=yin0, in1=yin1, op=ALU.is_equal), r=[bSs, bTv], w=[bZb])
                    E("dve", lambda e, h=h, zb=zb: e.tensor_scalar(out=bigA[:, h * 16:(h + 1) * 16, :].rearrange("p a b -> p (a b)"), in0=zb[:].rearrange("p a b -> p (a b)"), scalar1=rZ[:, h:h + 1], scalar2=None, op0=ALU.mult), r=[bZb, bRZ], w=[bBigAh[h]])
                for _ in transposes(bigA, bBigAh, YT, bYTh):
                    pass
                for t4 in range(32):
                    P, bP = (PA, bA) if (t4 // 2) % 2 == 0 else (PB, bB)
                    half = t4 % 2
                    for j in range(4):
                        t = t4 * 4 + j
                        E("pe", lambda e, t=t, j=j, P=P, half=half: e.matmul(bank(P, half)[:, j * 128:(j + 1) * 128], lhsT=XT[:, :, t], rhs=YT[:, :, t], start=True, stop=True), r=[bXT] + bYTh, w=[bP[half]])
                    src = bank(P, half)
                    dst = bigA[:, t4 * 4:(t4 + 1) * 4, :].rearrange("p a b -> p (a b)")
                    if t4 % 2 == 0:
                        E("act", lambda e, src=src, dst=dst: e.copy(out=dst, in_=src), r=[bP[half]], w=bBigAh)
                    else:
                        E("dve", lambda e, src=src, dst=dst: e.tensor_copy(out=dst, in_=src), r=[bP[half]], w=bBigAh)
                if gi == 0:
                    dump("ssb", ssb[:], [128, 16, 128], bSs)
                    dump("tv", tv[:], [128, 16, 16], bTv)
                    dump("c8", c8[:], [128, 8, 16], bC8)
                    dump("Zs", Zs[:], [128, 8], bZs)
                    dump("GT", bigA[:], [128, 128, 128], bBigAh[0], BF16)

            def mainloop(gi):
                hT_, bh_ = h2Ts[gi % 2], bH2Tb[gi % 2]
                for j4 in range(32):
                    k = j4 % NUV
                    k2 = j4 % 2
                    S.dma("sp", lambda e, j4=j4, k=k: e.dma_start(out=utb[k][:], in_=uts_d[j4 * 4:(j4 + 1) * 4].rearrange("j p c e -> p j c e")), bUt[k], reads=[bUs[j4 // 2]], writes=[bUt[k]])
                    S.dma("sp", lambda e, j4=j4, k=k: e.dma_start(out=vb[k][:], in_=vs_d[j4 * 512:(j4 + 1) * 512, :].rearrange("(j p) d -> p j d", p=128)), bVb[k], reads=[bVs[j4 // 2]], writes=[bVb[k]])
                    for jj in range(4):
                        for c in range(8):
                            E("pe", lambda e, c=c, k=k, k2=k2, jj=jj: e.matmul(bank(PA, k2)[:, jj * 128:(jj + 1) * 128], lhsT=utb[k][:, jj, c, :], rhs=hT_[:, c, :], start=(c == 0), stop=(c == 7)), r=[bUt[k], bh_], w=[bA[k2]])
                    E("act", lambda e, k2=k2: e.activation(out=ga[k2][:], in_=bank(PA, k2), func=AF.Gelu), r=[bA[k2]], w=[bGa[k2]])
                    E("dve", lambda e, k2=k2, j4=j4: e.tensor_tensor(out=wT[k2][:], in0=ga[k2][:].rearrange("p (a b) -> p a b", a=4), in1=mk(bigA, j4 * 4, [[1, 4], [128, 128]]), op=ALU.mult), r=[bGa[k2]] + bBigAh, w=[bWT[k2]])
                    for jj in range(4):
                        j = j4 * 4 + jj
                        for half in range(2):
                            E("pe", lambda e, half=half, k=k, k2=k2, j=j, jj=jj: e.matmul(bank(PC, half), lhsT=wT[k2][:, jj, :], rhs=vb[k][:, jj, half * 512:(half + 1) * 512], start=(j == 0), stop=(j == 127)), r=[bWT[k2], bVb[k]], w=[bC[half]])
                    yield

            def epilogue(gi):
                r0 = gi * 128
                ot_, bo_ = x1ts[gi % 2], bX1b[gi % 2]
                if do_peer:
                    for half in range(2):
                        hs = slice(half * 512, (half + 1) * 512)
                        E("dve", lambda e, half=half, hs=hs: e.tensor_tensor(out=junk[:, hs], in0=bank(PC, half), in1=gt2b[:, hs], op=ALU.mult), r=[bC[half], bGt2, bJunk], w=[bJunk])
                    E("pool", lambda e: e.tensor_tensor(out=ot_[:], in0=junk[:], in1=ot_[:], op=ALU.add), r=[bJunk, bo_], w=[bo_])
                E("dve", lambda e: e.memset(ssq[:], 0.0), w=[bSsq])
                E("act", lambda e: e.activation(out=junk[:], in_=ot_[:], func=AF.Square, accum_out=ssq[:, 0:1]), r=[bo_, bSsq], w=[bJunk, bSsq])
                E("dve", lambda e: e.tensor_scalar(out=ssq[:], in0=ssq[:], scalar1=1.0 / D, scalar2=EPS, op0=ALU.mult, op1=ALU.add), r=[bSsq], w=[bSsq])
                E("act", lambda e: e.activation(out=ssq[:], in_=ssq[:], func=AF.Sqrt), r=[bSsq], w=[bSsq])
                E("dve", lambda e: e.reciprocal(out=ssq[:], in_=ssq[:]), r=[bSsq], w=[bSsq])
                E("dve", lambda e: e.scalar_tensor_tensor(out=ot_[:], in0=ot_[:], scalar=ssq[:, 0:1], in1=fwb[:], op0=ALU.mult, op1=ALU.mult), r=[bo_, bSsq, bFw], w=[bo_])
                final_ops.append(S.dma("sp", lambda e: e.dma_start(out=out_d[r0:r0 + 128, :], in_=ot_[:]), bo_, reads=[bo_]))

            def run_all(g):
                for _ in g:
                    pass

            if not do_peer:
                for gi in range(ngroups):
                    r0 = gi * 128
                    S.dma("sp", lambda e, r0=r0, gi=gi: e.dma_start(out=x1ts[gi % 2][:], in_=x1s_d[r0:r0 + 128, :]), bX1b[gi % 2], reads=[bX1s[gi]], writes=[bX1b[gi % 2]])
                    epilogue(gi)
            else:
                run_all(front(0))
                tail(0)
                if dbg:
                    dump("h2T", h2Ts[0][:], [128, 8, 128], bH2Tb[0], BF16)
                for gi in range(ngroups):
                    nxt = front(gi + 1) if gi + 1 < ngroups else iter(())
                    for it, _ in enumerate(mainloop(gi)):
                        for _k in range(1 if it < 17 else 4):
                            next(nxt, None)
                    run_all(nxt)
                    epilogue(gi)
                    if gi + 1 < ngroups:
                        tail(gi + 1)

            S.finalize(final_ops + dumps)
            S.run()
    return nc


def _prep_shared(w_ada, b_ada, norm1_w, w_in, hgrn_lb_logits, hgrn_onorm_w, conv_w, conv_onorm_w, w_out,
                 norm2_w, peer_w_query, peer_sub_keys, peer_u, peer_v, final_norm_w):
    f = np.float32
    c = np.ascontiguousarray

    def pm(w):
        return c(np.asarray(w, f).reshape(8, 128, -1).transpose(1, 0, 2))
    sh = {}
    sh["wada"] = pm(w_ada[0])
    sh["bada"] = c(np.asarray(b_ada, f).reshape(1, -1))
    sh["n1w"] = c(np.asarray(norm1_w, f).reshape(1, -1))
    sh["n2w"] = c(np.asarray(norm2_w, f).reshape(1, -1))
    sh["fw"] = c(np.asarray(final_norm_w, f).reshape(1, -1))
    sh["win"] = pm(w_in[0])
    sh["lbl"] = c(np.asarray(hgrn_lb_logits, f).reshape(2, 4, 128).transpose(2, 0, 1))
    sh["onw"] = c(np.asarray(hgrn_onorm_w, f).reshape(4, 128).T)
    sh["cnw"] = c(np.asarray(conv_onorm_w, f).reshape(4, 128).T)
    sh["cw"] = c(np.asarray(conv_w, f).reshape(3, 4, 128).transpose(2, 0, 1))
    sh["wout"] = pm(w_out[0])
    sh["wq"] = c(np.asarray(peer_w_query[0], f).reshape(8, 128, 16, 128).transpose(2, 1, 0, 3))
    sh["skt"] = c(np.asarray(peer_sub_keys, f).reshape(16, 128, 128).transpose(2, 0, 1))
    sh["ut"] = c(np.asarray(peer_u, f).reshape(128, 128, 8, 128).transpose(0, 3, 2, 1))
    sh["v"] = c(np.asarray(peer_v, f).reshape(16384, 1024))
    return sh


def _in_maps(x, c, sh, npre):
    f = np.float32
    x = np.asarray(x, f); cc = np.asarray(c, f)
    maps = []
    for core in range(8):
        b, jj = core // 4, core % 4
        m = dict(sh)
        m["x"] = np.ascontiguousarray(x[b, jj * TOK:(jj + 1) * TOK])
        xpre = np.zeros((max(npre, 1) * TOK, D), f)
        msk = np.zeros((128, 4), f)
        for s in range(npre):
            src = jj - npre + s
            if src >= 0:
                xpre[s * TOK:(s + 1) * TOK] = x[b, src * TOK:(src + 1) * TOK]
                msk[:, s] = 1.0
        msk[:, 3] = 1.0 if (jj >= 1 and npre >= 1) else 0.0
        m["xpre"] = xpre
        m["msk"] = msk
        m["cT"] = np.ascontiguousarray(cc[b].reshape(8, 128).T)
        maps.append(m)
    return maps


_NC_CACHE = {}


def kernel(x, c, w_ada, b_ada, norm1_w, w_in, hgrn_lb_logits, hgrn_onorm_w, conv_w, conv_onorm_w, w_out,
           norm2_w, peer_w_query, peer_sub_keys, peer_u, peer_v, final_norm_w):
    sh = _prep_shared(w_ada, b_ada, norm1_w, w_in, hgrn_lb_logits, hgrn_onorm_w, conv_w, conv_onorm_w, w_out,
                      norm2_w, peer_w_query, peer_sub_keys, peer_u, peer_v, final_norm_w)
    maps = _in_maps(x, c, sh, NPRE)
    nc = build_nc(NPRE)
    res = run_bass_kernel_spmd(nc, maps, core_ids=list(range(8)))
    out = np.zeros((2, 8192, D), np.float32)
    for core in range(8):
        b, jj = core // 4, core % 4
        out[b, jj * TOK:(jj + 1) * TOK] = res.results[core]["out"]
    return out
```

```python
import contextlib
import numpy as np
import concourse.bass as bass
import concourse.mybir as mybir
from concourse.bass_utils import run_bass_kernel_spmd

F32 = mybir.dt.float32
BF16 = mybir.dt.bfloat16
ALU = mybir.AluOpType
AF = mybir.ActivationFunctionType
STRICT_SAME_ENGINE = True


class Buf:
    __slots__ = ("name", "w", "r", "sem", "cnt")

    def __init__(self, name):
        self.name = name
        self.w = None
        self.r = {}
        self.sem = None
        self.cnt = 0


class Op:
    __slots__ = ("eng", "fn", "deps", "flag", "val", "sem", "isdma", "name")

    def __init__(self, eng, fn, isdma=False, name=""):
        self.eng = eng
        self.fn = fn
        self.deps = []
        self.flag = False
        self.val = None
        self.sem = None
        self.isdma = isdma
        self.name = name


class Sched:
    ENGS = ("pe", "act", "dve", "pool", "sp")

    def __init__(self, nc, stack):
        self.nc = nc
        self.stack = stack
        self.q = {e: [] for e in self.ENGS}
        self.esem = {e: stack.enter_context(nc.semaphore("s_" + e)) for e in self.ENGS}
        self.nsem = len(self.ENGS)
        self.ndma = 0

    def newsem(self, name):
        self.nsem += 1
        return self.stack.enter_context(self.nc.semaphore(name))

    def _deps(self, op, reads, writes):
        deps = []
        for b in reads:
            if b.w is not None:
                deps.append(b.w)
        for b in writes:
            if b.w is not None:
                deps.append(b.w)
            deps.extend(b.r.values())
        seen = set()
        for d in deps:
            if d is op or id(d) in seen:
                continue
            seen.add(id(d))
            if (not d.isdma) and d.eng == op.eng and (op.eng == "pe" or not STRICT_SAME_ENGINE):
                continue
            op.deps.append(d)
        for b in reads:
            key = ("dma", id(op)) if op.isdma else op.eng
            b.r[key] = op
        for b in writes:
            b.w = op
            b.r = {}

    def emit(self, eng, fn, reads=(), writes=(), name=""):
        op = Op(eng, fn, name=name)
        self._deps(op, reads, writes)
        self.q[eng].append(op)
        return op

    def dma(self, eng, fn, sbuf, reads=(), writes=(), nparts=1, name=""):
        op = Op(eng, fn, isdma=True, name=name)
        self._deps(op, reads, writes)
        if sbuf.sem is None:
            sbuf.sem = self.newsem("d_" + sbuf.name)
        sbuf.cnt += 16 * nparts
        op.sem = sbuf.sem
        op.val = sbuf.cnt
        self.q[eng].append(op)
        self.ndma += nparts
        return op

    def finalize(self, final_ops):
        for e in self.ENGS:
            for op in self.q[e]:
                for d in op.deps:
                    if not d.isdma:
                        d.flag = True
        for e in self.ENGS:
            c = 0
            for op in self.q[e]:
                if not op.isdma and op.flag:
                    c += 1
                    op.val = c
                    op.sem = self.esem[e]
        self.final_ops = final_ops

    def replay(self, eng, e):
        seen = {}
        nwait = 0
        for op in self.q[eng]:
            for d in op.deps:
                k = id(d.sem)
                if seen.get(k, 0) >= d.val:
                    continue
                seen[k] = d.val
                e.wait_ge(d.sem, d.val)
                nwait += 1
            r = op.fn(e)
            if op.isdma:
                lst = r if isinstance(r, (list, tuple)) else [r]
                for ins in lst:
                    ins.then_inc(op.sem, 16)
            elif op.flag:
                r.then_inc(op.sem, 1)
        if eng == "sp":
            for d in self.final_ops:
                e.wait_ge(d.sem, d.val)
        return nwait

    def run(self):
        nc = self.nc
        with nc.Block() as block:
            @block.sync
            def _(e):
                self.replay("sp", e)

            @block.scalar
            def _(e):
                self.replay("act", e)

            @block.vector
            def _(e):
                self.replay("dve", e)

            @block.gpsimd
            def _(e):
                self.replay("pool", e)

            @block.tensor
            def _(e):
                self.replay("pe", e)


def ap_of(t):
    return t if isinstance(t, bass.AP) else t[:]


def mk(base, offset_elems, dims):
    b = ap_of(base)
    pstep, pcnt = b.ap[0]
    return bass.AP(b.tensor, b.offset + offset_elems, [[pstep, pcnt]] + [list(d) for d in dims])

D = 1024
TOK = 2048
NPRE = 3
EPS = 1e-6
NEG = -1.0e30


def build_nc(npre=NPRE, dbg=False, do_peer=True, ngroups=16):
    nc = bass.Bass("TRN2", target_bir_lowering=False)

    def din(name, shape):
        return nc.dram_tensor(name, shape, F32, kind="ExternalInput").ap()

    x_d = din("x", [TOK, D])
    xpre_d = din("xpre", [max(npre, 1) * TOK, D])
    msk_d = din("msk", [128, 4])
    cT_d = din("cT", [128, 8])
    wada_d = din("wada", [128, 8, 6 * D])
    bada_d = din("bada", [1, 6 * D])
    n1w_d = din("n1w", [1, D])
    n2w_d = din("n2w", [1, D])
    fw_d = din("fw", [1, D])
    win_d = din("win", [128, 8, 3584])
    lbl_d = din("lbl", [128, 2, 4])
    onw_d = din("onw", [128, 4])
    cnw_d = din("cnw", [128, 4])
    cw_d = din("cw", [128, 3, 4])
    wout_d = din("wout", [128, 8, D])
    wq_d = din("wq", [16, 128, 8, 128])
    skt_d = din("skt", [128, 16, 128])
    ut_d = din("ut", [128, 128, 8, 128])
    v_d = din("v", [16384, D])
    out_d = nc.dram_tensor("out", [TOK, D], F32, kind="ExternalOutput").ap()
    x1s_d = nc.dram_tensor("x1s", [TOK, D], F32, kind=("ExternalOutput" if dbg else "Internal")).ap()
    uts_d = nc.dram_tensor("uts", [128, 128, 8, 128], BF16, kind="Internal").ap()
    vs_d = nc.dram_tensor("vs", [16384, D], BF16, kind="Internal").ap()
    wqs_d = nc.dram_tensor("wqs", [16, 128, 8, 128], BF16, kind="Internal").ap()

    with contextlib.ExitStack() as st:
        S = Sched(nc, st)

        def sb(name, shape, dt=F32):
            return st.enter_context(nc.sbuf_tensor("t_" + name, shape, dt))

        def ps(name, shape, dt=F32):
            return st.enter_context(nc.psum_tensor("t_" + name, shape, dt))

        def E(eng, fn, r=(), w=()):
            return S.emit(eng, fn, reads=r, writes=w)

        dumps = []

        def dump(name, ap, shape, buf, dt=F32):
            if not dbg:
                return
            dd = nc.dram_tensor("dbg_" + name, shape, dt, kind="ExternalOutput").ap()
            dumps.append(S.dma("sp", lambda e: e.dma_start(out=dd, in_=ap), buf, reads=[buf]))

        pT0 = ps("pT0", [128, 1024], BF16); bT0 = Buf("pT0")
        pT1 = ps("pT1", [128, 1024], BF16); bT1 = Buf("pT1")
        PA = ps("PA", [128, 1024], F32); bA = [Buf("PA0"), Buf("PA1")]
        PB = ps("PB", [128, 1024], F32); bB = [Buf("PB0"), Buf("PB1")]
        PC = ps("PC", [128, 1024], F32); bC = [Buf("PC0"), Buf("PC1")]

        def bank(P, i):
            return P[:, i * 512:(i + 1) * 512]

        identf = sb("identf", [128, 128]); ident = sb("ident", [128, 128], BF16); bId = Buf("ident")
        E("pool", lambda e: e.memset(identf[:], 1.0), w=[bId])
        E("pool", lambda e: e.affine_select(out=identf[:], in_=identf[:], pattern=[[-1, 128]], compare_op=ALU.is_equal, fill=0.0, base=0, channel_multiplier=1), r=[bId], w=[bId])
        E("pool", lambda e: e.tensor_copy(out=ident[:], in_=identf[:]), r=[bId], w=[bId])
        onesf = sb("onesf", [128, 128]); ones_bf = sb("ones_bf", [128, 128], BF16); bOn = Buf("ones")
        E("pool", lambda e: e.memset(onesf[:], 1.0), w=[bOn])
        E("pool", lambda e: e.tensor_copy(out=ones_bf[:], in_=onesf[:]), r=[bOn], w=[bOn])
        maskST = sb("maskST", [128, 128]); bMk = Buf("maskST")
        E("pool", lambda e: e.memset(maskST[:], 1.0), w=[bMk])
        E("pool", lambda e: e.affine_select(out=maskST[:], in_=maskST[:], pattern=[[1, 128]], compare_op=ALU.is_ge, fill=0.0, base=0, channel_multiplier=-1), r=[bMk], w=[bMk])
        maskI = sb("maskI", [128, 128], mybir.dt.int32)
        E("pool", lambda e: e.tensor_copy(out=maskI[:], in_=maskST[:]), r=[bMk], w=[bMk])

        small = {}

        def load_small(name, src, shape):
            t = sb(name, shape); b = Buf(name)
            S.dma("sp", lambda e: e.dma_start(out=t[:], in_=src), b, writes=[b])
            small[name] = (t, b)
            return t, b

        msk, bMsk = load_small("msk", msk_d, [128, 4])
        cT, bcT = load_small("cT", cT_d, [128, 8])
        lbl, bLbl = load_small("lbl", lbl_d, [128, 2, 4])
        onw, bOnw = load_small("onw", onw_d, [128, 4])
        cnw, bCnw = load_small("cnw", cnw_d, [128, 4])
        cw, bCw = load_small("cw", cw_d, [128, 3, 4])

        lb = sb("lb", [128, 4]); oml = sb("oml", [128, 4]); noml = sb("noml", [128, 4]); bLb = Buf("lb")
        E("dve", lambda e: e.tensor_tensor(out=lb[:], in0=lbl[:, 0, :], in1=lbl[:, 1, :], op=ALU.subtract), r=[bLbl], w=[bLb])
        E("act", lambda e: e.activation(out=lb[:], in_=lb[:], func=AF.Sigmoid), r=[bLb], w=[bLb])
        E("dve", lambda e: e.tensor_scalar(out=oml[:], in0=lb[:], scalar1=-1.0, scalar2=1.0, op0=ALU.mult, op1=ALU.add), r=[bLb], w=[bLb])
        E("dve", lambda e: e.tensor_scalar(out=noml[:], in0=lb[:], scalar1=-1.0, scalar2=None, op0=ALU.add), r=[bLb], w=[bLb])

        g2b = sb("g2b", [128, D]); fwb = sb("fwb", [128, D]); sh2b = sb("sh2b", [128, D]); gt2b = sb("gt2b", [128, D])
        bG2 = Buf("g2b"); bFw = Buf("fwb"); bSh2 = Buf("sh2b"); bGt2 = Buf("gt2b")
        junk = sb("junk", [128, D]); bJunk = Buf("junk")
        ssq = sb("ssq", [128, 1]); bSsq = Buf("ssq")
        hb = sb("hb", [128, D], BF16); bHb = Buf("hb")

        def norm_mod(xap, bX, gb, bG, shiftap, bShift):
            E("dve", lambda e: e.memset(ssq[:], 0.0), w=[bSsq])
            E("act", lambda e: e.activation(out=junk[:], in_=xap, func=AF.Square, accum_out=ssq[:, 0:1]), r=[bX, bSsq], w=[bJunk, bSsq])
            E("dve", lambda e: e.tensor_scalar(out=ssq[:], in0=ssq[:], scalar1=1.0 / D, scalar2=EPS, op0=ALU.mult, op1=ALU.add), r=[bSsq], w=[bSsq])
            E("act", lambda e: e.activation(out=ssq[:], in_=ssq[:], func=AF.Sqrt), r=[bSsq], w=[bSsq])
            E("dve", lambda e: e.reciprocal(out=ssq[:], in_=ssq[:]), r=[bSsq], w=[bSsq])
            E("dve", lambda e: e.scalar_tensor_tensor(out=junk[:], in0=xap, scalar=ssq[:, 0:1], in1=gb[:], op0=ALU.mult, op1=ALU.mult), r=[bX, bSsq, bG, bJunk], w=[bJunk])
            E("pool", lambda e: e.tensor_tensor(out=hb[:], in0=junk[:], in1=shiftap, op=ALU.add), r=[bJunk, bShift], w=[bHb])

        def transpose_hb(dst3, bDst):
            for c in range(8):
                E("pe", lambda e, c=c: e.transpose(out=pT0[:, c * 128:(c + 1) * 128], in_=hb[:, c * 128:(c + 1) * 128], identity=ident[:]), r=[bHb, bId], w=[bT0])
            E("act", lambda e: e.copy(out=dst3, in_=pT0[:].rearrange("p (c t) -> p c t", c=8)), r=[bT0], w=[bDst])

        with contextlib.ExitStack() as st2:
            def sb2(name, shape, dt=F32):
                return st2.enter_context(nc.sbuf_tensor("t_" + name, shape, dt))
            win = sb2("win", [128, 8, 3584], BF16); bWin = Buf("win")
            for c in range(8):
                S.dma("pool", lambda e, c=c: e.dma_start(out=win[:, c, :], in_=win_d[:, c, :]), bWin, writes=[bWin])
            wout = sb2("wout", [128, 8, D], BF16); bWout = Buf("wout")
            for c in range(0, 8, 4):
                S.dma("pool", lambda e, c=c: e.dma_start(out=wout[:, c:c + 4, :], in_=wout_d[:, c:c + 4, :]), bWout, writes=[bWout])
            bUs = [Buf("uts%d" % i) for i in range(16)]; bVs = [Buf("vs%d" % i) for i in range(16)]
            xt = sb2("xt", [128, 4, D]); bXt = [Buf("xt%d" % i) for i in range(4)]
            h1T = sb2("h1T", [128, 8, 512], BF16); bH1T = [Buf("h1T%d" % i) for i in range(4)]
            itok = sb2("itok", [128, 4, 512], BF16); bItok = [Buf("itok%d" % i) for i in range(4)]
            sg = sb2("sg", [128, 512]); bSg = Buf("sg")
            lf = sb2("lf", [128, 512]); bLf = Buf("lf")
            kk = sb2("kk", [128, 512]); bKk = Buf("kk")
            qs = sb2("qs", [128, 512]); bQs = Buf("qs")
            gs = sb2("gs", [128, 512]); bGs = Buf("gs")
            btc = [sb2("btc%d" % i, [128, 128]) for i in range(4)]; bBtc = [Buf("btc%d" % i) for i in range(4)]
            exc = [sb2("exc%d" % i, [128, 128]) for i in range(4)]; bExc = [Buf("exc%d" % i) for i in range(4)]
            khTc = [sb2("khTc%d" % i, [128, 128], BF16) for i in range(4)]; bKhTc = [Buf("khTc%d" % i) for i in range(4)]
            khc = [sb2("khc%d" % i, [128, 128], BF16) for i in range(4)]; bKhc = [Buf("khc%d" % i) for i in range(4)]
            qtc = [sb2("qtc%d" % i, [128, 128], BF16) for i in range(4)]; bQtc = [Buf("qtc%d" % i) for i in range(4)]
            ktc = [sb2("ktc%d" % i, [128, 128], BF16) for i in range(4)]; bKtc = [Buf("ktc%d" % i) for i in range(4)]
            qhc = [sb2("qhc%d" % i, [128, 128], BF16) for i in range(4)]; bQhc = [Buf("qhc%d" % i) for i in range(4)]
            scmc = [sb2("scmc%d" % i, [128, 128], BF16) for i in range(4)]; bScmc = [Buf("scmc%d" % i) for i in range(4)]
            ebc = sb2("ebc", [128, 4]); bEbc = [Buf("ebc%d" % i) for i in range(4)]
            nbmc = sb2("nbmc", [128, 4]); bNbmc = [Buf("nbmc%d" % i) for i in range(4)]
            bT1s = [Buf("pT1s%d" % i) for i in range(4)]; bB0s = [Buf("PB0s%d" % i) for i in range(4)]; bC0s = [Buf("PC0s%d" % i) for i in range(4)]
            bt = btc[0]; bBt = bBtc[0]
            Sst = sb2("Sst", [128, 4, 128]); bS = [Buf("S%d" % i) for i in range(4)]
            Sbf = sb2("Sbf", [128, 4, 128], BF16); bSbf = [Buf("Sbf%d" % i) for i in range(4)]
            sq = sb2("sq", [128, 512], BF16); bSq = Buf("sq")
            rb = sb2("rb", [128, 512]); bRb = Buf("rb")
            yv = sb2("yv", [128, 512]); bYv = Buf("yv")
            ymT = sb2("ymT", [128, 8, 512], BF16); bYm = [Buf("ym%d" % i) for i in range(8)]
            u = sb2("u", [128, 4, 514]); bU = [Buf("u%d" % i) for i in range(4)]
            csb = sg; bCsb = bSg
            y0 = lf; bY0 = bLf
            y1 = kk; bY1 = bKk
            t1 = junk; bTt1 = bJunk

            modb = sb2("modb", [128, 6 * D]); bMod = Buf("modb")
            cact = sb2("cact", [128, 8]); crep = sb2("crep", [128, 8, 128]); bCrep = Buf("crep")
            E("act", lambda e: e.activation(out=cact[:], in_=cT[:], func=AF.Silu), r=[bcT], w=[bCrep])
            for c in range(8):
                E("dve", lambda e, c=c: e.tensor_scalar(out=crep[:, c, :], in0=onesf[:], scalar1=cact[:, c:c + 1], scalar2=None, op0=ALU.mult), r=[bOn, bCrep], w=[bCrep])
            S.dma("sp", lambda e: e.dma_start(out=modb[:], in_=bada_d.partition_broadcast(128)), bMod, writes=[bMod])
            wst = [xt[:, 0:2, :].rearrange("p a (b n) -> p (a b) n", n=256), xt[:, 2:4, :].rearrange("p a (b n) -> p (a b) n", n=256)]
            bWst = [[bXt[0], bXt[1]], [bXt[2], bXt[3]]]
            for g in range(24):
                k = g % 2
                S.dma("sp", lambda e, g=g, k=k: e.dma_start(out=wst[k], in_=wada_d[:, :, g * 256:(g + 1) * 256]), bWst[k][0], writes=bWst[k])
                for c in range(8):
                    E("pe", lambda e, c=c, k=k: e.matmul(bank(PA, k)[:, 0:256], lhsT=crep[:, c, :], rhs=wst[k][:, c, :], start=(c == 0), stop=(c == 7)), r=[bCrep] + bWst[k], w=[bA[k]])
                E("dve", lambda e, g=g, k=k: e.tensor_tensor(out=modb[:, g * 256:(g + 1) * 256], in0=bank(PA, k)[:, 0:256], in1=modb[:, g * 256:(g + 1) * 256], op=ALU.add), r=[bA[k], bMod], w=[bMod])
            g1b = sb2("g1b", [128, D]); bG1 = Buf("g1b")
            S.dma("sp", lambda e: e.dma_start(out=g1b[:], in_=n1w_d.partition_broadcast(128)), bG1, writes=[bG1])
            S.dma("sp", lambda e: e.dma_start(out=g2b[:], in_=n2w_d.partition_broadcast(128)), bG2, writes=[bG2])
            S.dma("sp", lambda e: e.dma_start(out=fwb[:], in_=fw_d.partition_broadcast(128)), bFw, writes=[bFw])
            E("dve", lambda e: e.scalar_tensor_tensor(out=g1b[:], in0=modb[:, D:2 * D], scalar=1.0, in1=g1b[:], op0=ALU.add, op1=ALU.mult), r=[bMod, bG1], w=[bG1])
            E("dve", lambda e: e.scalar_tensor_tensor(out=g2b[:], in0=modb[:, 4 * D:5 * D], scalar=1.0, in1=g2b[:], op0=ALU.add, op1=ALU.mult), r=[bMod, bG2], w=[bG2])
            shift1 = modb[:, 0:D]; gate1 = modb[:, 2 * D:3 * D]
            dump("modb", modb[:], [128, 6 * D], bMod)
            dump("g1b", g1b[:], [128, D], bG1)
            dump("lb", lb[:], [128, 4], bLb)
            E("dve", lambda e: e.tensor_copy(out=sh2b[:], in_=modb[:, 3 * D:4 * D]), r=[bMod], w=[bSh2])
            E("dve", lambda e: e.tensor_copy(out=gt2b[:], in_=modb[:, 5 * D:6 * D]), r=[bMod], w=[bGt2])

            bWqs = [Buf("wqs%d" % i) for i in range(2)]
            if do_peer:
                for i in range(2):
                    S.dma("pool", lambda e, i=i: e.dma_start(out=wqs_d[i * 8:(i + 1) * 8], in_=wq_d[i * 8:(i + 1) * 8]), bWqs[i], reads=[bSh2, bGt2, bG1], writes=[bWqs[i]])
                for i in range(16):
                    S.dma("pool", lambda e, i=i: e.dma_start(out=uts_d[i * 8:(i + 1) * 8], in_=ut_d[i * 8:(i + 1) * 8]), bUs[i], reads=[bSh2, bGt2, bG1], writes=[bUs[i]])
                    S.dma("pool", lambda e, i=i: e.dma_start(out=vs_d[i * 1024:(i + 1) * 1024, :], in_=v_d[i * 1024:(i + 1) * 1024, :]), bVs[i], reads=[bSh2, bGt2, bG1], writes=[bVs[i]])
            for ck in range(4):
                E("pool", lambda e, ck=ck: e.memset(scmc[ck][:], 0.0), w=[bScmc[ck]])
            for h in range(4):
                E("dve", lambda e, h=h: e.memset(Sst[:, h, :], 0.0), w=[bS[h]])
                E("pool", lambda e, h=h: e.memset(Sbf[:, h, :], 0.0), w=[bSbf[h]])
                E("pool", lambda e, h=h: e.memset(u[:, h, 0:2], 0.0), w=[bU[h]])

            rot = [0]

            def projbank():
                k = rot[0] % 2
                rot[0] += 1
                return k

            def proj_fm(col0, n):
                k = projbank()
                for c in range(8):
                    E("pe", lambda e, c=c, k=k: e.matmul(bank(PA, k)[:, 0:n], lhsT=win[:, c, col0:col0 + 128], rhs=h1T[:, c, 0:n], start=(c == 0), stop=(c == 7)), r=[bWin] + bH1T, w=[bA[k]])
                return bank(PA, k)[:, 0:n], bA[k]

            def hgrn_head(h, ntile, mode):
                n = ntile * 128
                fp, bf_ = proj_fm(512 + h * 128, n)
                E("act", lambda e: e.activation(out=sg[:, 0:n], in_=fp, func=AF.Sigmoid), r=[bf_], w=[bSg])
                E("act", lambda e: e.activation(out=lf[:, 0:n], in_=sg[:, 0:n], func=AF.Ln, scale=oml[:, h:h + 1], bias=lb[:, h:h + 1]), r=[bSg, bLb], w=[bLf])
                E("dve", lambda e: e.tensor_scalar(out=kk[:, 0:n], in0=sg[:, 0:n], scalar1=noml[:, h:h + 1], scalar2=oml[:, h:h + 1], op0=ALU.mult, op1=ALU.add), r=[bSg, bLb], w=[bKk])
                if mode == "main":
                    qp, bq_ = proj_fm(h * 128, n)
                    E("act", lambda e: e.copy(out=qs[:, 0:n], in_=qp), r=[bq_], w=[bQs])
                    gp, bg_ = proj_fm(1536 + h * 128, n)
                    E("act", lambda e: e.activation(out=gs[:, 0:n], in_=gp, func=AF.Silu), r=[bg_], w=[bGs])
                NT = ntile
                css = [slice(ck * 128, (ck + 1) * 128) for ck in range(NT)]
                isls = [itok[:, ck, h * 128:(h + 1) * 128] for ck in range(NT)]
                for ck in range(NT):
                    E("dve", lambda e, ck=ck: e.tensor_tensor_scan(out=btc[ck][:], data0=onesf[:], data1=lf[:, css[ck]], initial=0.0, op0=ALU.mult, op1=ALU.add), r=[bOn, bLf], w=[bBtc[ck]])
                for ck in range(NT):
                    E("act", lambda e, ck=ck: e.activation(out=exc[ck][:], in_=btc[ck][:], func=AF.Exp, scale=-1.0, bias=btc[ck][:, 127:128]), r=[bBtc[ck]], w=[bExc[ck]])
                for ck in range(NT):
                    E("dve", lambda e, ck=ck: e.tensor_tensor(out=khTc[ck][:], in0=kk[:, css[ck]], in1=exc[ck][:], op=ALU.mult), r=[bKk, bExc[ck]], w=[bKhTc[ck]])
                for ck in range(NT):
                    E("pe", lambda e, ck=ck: e.transpose(out=pT1[:, ck * 128:(ck + 1) * 128], in_=khTc[ck][:], identity=ident[:]), r=[bKhTc[ck], bId], w=[bT1])
                for ck in range(NT):
                    E("act", lambda e, ck=ck: e.copy(out=khc[ck][:], in_=pT1[:, ck * 128:(ck + 1) * 128]), r=[bT1], w=[bKhc[ck]])
                for ck in range(NT):
                    E("pe", lambda e, ck=ck: e.matmul(bank(PC, 0)[:, css[ck]], lhsT=khc[ck][:], rhs=isls[ck], start=True, stop=True), r=[bKhc[ck], bItok[ck]], w=[bC[0]])
                for ck in range(NT):
                    E("act", lambda e, ck=ck: e.activation(out=ebc[:, ck:ck + 1], in_=btc[ck][:, 127:128], func=AF.Exp), r=[bBtc[ck]], w=[bEbc[ck]])
                if mode == "main":
                    for ck in range(NT):
                        E("dve", lambda e, ck=ck: e.tensor_scalar(out=nbmc[:, ck:ck + 1], in0=btc[ck][:, 63:64], scalar1=-1.0, scalar2=None, op0=ALU.mult), r=[bBtc[ck]], w=[bNbmc[ck]])
                    for ck in range(NT):
                        E("act", lambda e, ck=ck: e.activation(out=exc[ck][:], in_=btc[ck][:], func=AF.Exp, bias=nbmc[:, ck:ck + 1]), r=[bBtc[ck], bNbmc[ck], bExc[ck]], w=[bExc[ck]])
                    for ck in range(NT):
                        E("dve", lambda e, ck=ck: e.tensor_tensor(out=qtc[ck][:], in0=qs[:, css[ck]], in1=exc[ck][:], op=ALU.mult), r=[bQs, bExc[ck]], w=[bQtc[ck]])
                    for ck in range(NT):
                        E("act", lambda e, ck=ck: e.activation(out=exc[ck][:], in_=btc[ck][:], func=AF.Exp, scale=-1.0, bias=btc[ck][:, 63:64]), r=[bBtc[ck], bExc[ck]], w=[bExc[ck]])
                    for ck in range(NT):
                        E("pool", lambda e, ck=ck: e.tensor_tensor(out=ktc[ck][:], in0=kk[:, css[ck]], in1=exc[ck][:], op=ALU.mult), r=[bKk, bExc[ck]], w=[bKtc[ck]])
                    for ck in range(NT):
                        E("act", lambda e, ck=ck: e.activation(out=exc[ck][:], in_=btc[ck][:], func=AF.Exp), r=[bBtc[ck], bExc[ck]], w=[bExc[ck]])
                    for ck in range(NT):
                        E("dve", lambda e, ck=ck: e.tensor_tensor(out=qhc[ck][:], in0=qs[:, css[ck]], in1=exc[ck][:], op=ALU.mult), r=[bQs, bExc[ck]], w=[bQhc[ck]])
                    for ck in range(NT):
                        E("pe", lambda e, ck=ck: e.matmul(bank(PB, 0)[:, css[ck]], lhsT=ktc[ck][:], rhs=qtc[ck][:], start=True, stop=True), r=[bKtc[ck], bQtc[ck]], w=[bB[0]])
                    for ck in range(NT):
                        E("dve", lambda e, ck=ck: e.copy_predicated(out=scmc[ck][:], mask=maskI[:], data=bank(PB, 0)[:, css[ck]]), r=[bB[0], bMk, bScmc[ck]], w=[bScmc[ck]])
                for ck in range(NT):
                    if mode == "main":
                        E("pe", lambda e, ck=ck: e.matmul(bank(PB, 1)[:, css[ck]], lhsT=isls[ck], rhs=scmc[ck][:], start=True, stop=False), r=[bItok[ck], bScmc[ck]], w=[bB[1]])
                        E("pe", lambda e, ck=ck: e.matmul(bank(PB, 1)[:, css[ck]], lhsT=Sbf[:, h, :], rhs=qhc[ck][:], start=False, stop=True), r=[bSbf[h], bQhc[ck]], w=[bB[1]])
                    E("dve", lambda e, ck=ck: e.scalar_tensor_tensor(out=Sst[:, h, :], in0=Sst[:, h, :], scalar=ebc[:, ck:ck + 1], in1=bank(PC, 0)[:, css[ck]], op0=ALU.mult, op1=ALU.add), r=[bS[h], bEbc[ck], bC[0]], w=[bS[h]])
                    if mode == "main":
                        E("pool", lambda e: e.tensor_copy(out=Sbf[:, h, :], in_=Sst[:, h, :]), r=[bS[h]], w=[bSbf[h]])
                if mode == "main":
                    groupnorm_out(bank(PB, 1), bB[1], onw[:, h:h + 1], bOnw, h, mulgs=True)

            def groupnorm_out(src, bSrc, wcol, bW, slot, mulgs):
                E("act", lambda e: e.activation(out=sq[:], in_=src, func=AF.Square), r=[bSrc], w=[bSq])
                E("pe", lambda e: e.matmul(bank(PC, 1), lhsT=ones_bf[:], rhs=sq[:], start=True, stop=True), r=[bOn, bSq], w=[bC[1]])
                E("dve", lambda e: e.tensor_scalar(out=rb[:], in0=bank(PC, 1), scalar1=1.0 / 128, scalar2=EPS, op0=ALU.mult, op1=ALU.add), r=[bC[1]], w=[bRb])
                E("act", lambda e: e.activation(out=rb[:], in_=rb[:], func=AF.Sqrt), r=[bRb], w=[bRb])
                E("dve", lambda e: e.reciprocal(out=rb[:], in_=rb[:]), r=[bRb], w=[bRb])
                if mulgs:
                    E("dve", lambda e: e.scalar_tensor_tensor(out=yv[:], in0=src, scalar=wcol, in1=rb[:], op0=ALU.mult, op1=ALU.mult), r=[bSrc, bW, bRb], w=[bYv])
                    E("pool", lambda e: e.tensor_tensor(out=ymT[:, slot, :], in0=yv[:], in1=gs[:], op=ALU.mult), r=[bYv, bGs], w=[bYm[slot]])
                else:
                    E("dve", lambda e: e.scalar_tensor_tensor(out=ymT[:, slot, :], in0=src, scalar=wcol, in1=rb[:], op0=ALU.mult, op1=ALU.mult), r=[bSrc, bW, bRb], w=[bYm[slot]])

            def conv_group(g, mode):
                if mode == "halo":
                    cp, bc_ = proj_fm(2560 + g * 128, 128)
                    E("act", lambda e: e.copy(out=csb[:, 0:128], in_=cp), r=[bc_], w=[bCsb])
                    xp, bx_ = proj_fm(3072 + g * 128, 128)
                    E("dve", lambda e: e.tensor_tensor(out=y0[:, 0:128], in0=csb[:, 0:128], in1=xp, op=ALU.mult), r=[bCsb, bx_], w=[bY0])
                    E("dve", lambda e: e.tensor_scalar(out=u[:, g, 0:2], in0=y0[:, 126:128], scalar1=msk[:, 3:4], scalar2=None, op0=ALU.mult), r=[bY0, bMsk], w=[bU[g]])
                    return
                cp, bc_ = proj_fm(2560 + g * 128, 512)
                E("act", lambda e: e.copy(out=csb[:], in_=cp), r=[bc_], w=[bCsb])
                xp, bx_ = proj_fm(3072 + g * 128, 512)
                E("dve", lambda e: e.tensor_tensor(out=u[:, g, 2:514], in0=csb[:], in1=xp, op=ALU.mult), r=[bCsb, bx_], w=[bU[g]])
                bp, bb_ = proj_fm(2048 + g * 128, 512)
                E("dve", lambda e: e.tensor_scalar(out=y0[:], in0=u[:, g, 2:514], scalar1=cw[:, 2, g:g + 1], scalar2=None, op0=ALU.mult), r=[bU[g], bCw], w=[bY0])
                E("dve", lambda e: e.scalar_tensor_tensor(out=y1[:], in0=u[:, g, 1:513], scalar=cw[:, 1, g:g + 1], in1=y0[:], op0=ALU.mult, op1=ALU.add), r=[bU[g], bCw, bY0], w=[bY1])
                E("dve", lambda e: e.scalar_tensor_tensor(out=y0[:], in0=u[:, g, 0:512], scalar=cw[:, 0, g:g + 1], in1=y1[:], op0=ALU.mult, op1=ALU.add), r=[bU[g], bCw, bY1], w=[bY0])
                E("dve", lambda e: e.tensor_tensor(out=y1[:], in0=y0[:], in1=bp, op=ALU.mult), r=[bY0, bb_], w=[bY1])
                E("pool", lambda e: e.tensor_copy(out=u[:, g, 0:2], in_=u[:, g, 512:514]), r=[bU[g]], w=[bU[g]])
                groupnorm_out(y1[:], bY1, cnw[:, g:g + 1], bCnw, 4 + g, mulgs=False)

            def mixer_macro(src_d, row0, ntile, mode):
                n = ntile * 128
                for tt in range(ntile):
                    S.dma("sp", lambda e, tt=tt: e.dma_start(out=xt[:, tt, :], in_=src_d[row0 + tt * 128: row0 + (tt + 1) * 128, :]), bXt[tt], writes=[bXt[tt]])
                    norm_mod(xt[:, tt, :], bXt[tt], g1b, bG1, shift1, bMod)
                    transpose_hb(h1T[:, :, tt * 128:(tt + 1) * 128], bH1T[tt])
                if mode == "halo":
                    for g in range(4):
                        conv_group(g, "halo")
                    return
                for tt in range(ntile):
                    k = projbank()
                    for c in range(8):
                        E("pe", lambda e, c=c, k=k, tt=tt: e.matmul(bank(PA, k), lhsT=h1T[:, c, tt * 128:(tt + 1) * 128], rhs=win[:, c, 1024:1536], start=(c == 0), stop=(c == 7)), r=[bWin, bH1T[tt]], w=[bA[k]])
                    E("act", lambda e, k=k, tt=tt: e.copy(out=itok[:, tt, :], in_=bank(PA, k)), r=[bA[k]], w=[bItok[tt]])
                for h in range(4):
                    hgrn_head(h, ntile, mode)
                if mode != "main":
                    return
                for g in range(4):
                    conv_group(g, "main")
                for tt in range(ntile):
                    for half in range(2):
                        for cc in range(8):
                            E("pe", lambda e, cc=cc, half=half, tt=tt: e.matmul(bank(PA, half), lhsT=ymT[:, cc, tt * 128:(tt + 1) * 128], rhs=wout[:, cc, half * 512:(half + 1) * 512], start=(cc == 0), stop=(cc == 7)), r=[bYm[cc], bWout], w=[bA[half]])
                    for half in range(2):
                        hs = slice(half * 512, (half + 1) * 512)
                        E("dve", lambda e, half=half, hs=hs: e.tensor_tensor(out=t1[:, hs], in0=bank(PA, half), in1=gate1[:, hs], op=ALU.mult), r=[bA[half], bMod, bTt1], w=[bTt1])
                    E("pool", lambda e, tt=tt: e.tensor_tensor(out=xt[:, tt, :], in0=t1[:], in1=xt[:, tt, :], op=ALU.add), r=[bTt1, bXt[tt]], w=[bXt[tt]])
                    S.dma("sp", lambda e, tt=tt: e.dma_start(out=x1s_d[row0 + tt * 128: row0 + (tt + 1) * 128, :], in_=xt[:, tt, :]), bXt[tt], reads=[bXt[tt]], writes=[bX1s[(row0 + tt * 128) // 128]])

            bX1s = [Buf("x1s%d" % i) for i in range(16)]
            for seg in range(npre):
                for m in range(4):
                    mixer_macro(xpre_d, seg * TOK + m * 512, 4, "pre")
                for h in range(4):
                    E("dve", lambda e, h=h, seg=seg: e.tensor_scalar(out=Sst[:, h, :], in0=Sst[:, h, :], scalar1=msk[:, seg:seg + 1], scalar2=None, op0=ALU.mult), r=[bS[h], bMsk], w=[bS[h]])
            if npre > 0:
                mixer_macro(xpre_d, npre * TOK - 128, 1, "halo")
            for h in range(4):
                E("pool", lambda e, h=h: e.tensor_copy(out=Sbf[:, h, :], in_=Sst[:, h, :]), r=[bS[h]], w=[bSbf[h]])
            for m in range(4):
                mixer_macro(x_d, m * 512, 4, "main")
                if m == 0:
                    dump("ymT", ymT[:], [128, 8, 512], bYm[7], BF16)
                    dump("h1T", h1T[:], [128, 8, 512], bH1T[3], BF16)
                    dump("itok", itok[:], [128, 4, 512], bItok[3], BF16)
                    dump("Sst", Sst[:], [128, 4, 128], bS[3])
                    dump("u", u[:], [128, 4, 514], bU[3])
            mixer_tail_bufs = [bWin, bWout] + bXt + bH1T + bItok + [bSg, bLf, bKk, bQs, bGs] + bBtc + bExc + bKhTc + bKhc + bQtc + bKtc + bQhc + bScmc + bEbc + bNbmc + bT1s + bB0s + bC0s + bS + bSbf + [bSq, bRb, bYv] + bYm + bU + [bMod, bG1, bCrep]

        final_ops = []
        with contextlib.ExitStack() as st3:
            def sb3(name, shape, dt=F32):
                return st3.enter_context(nc.sbuf_tensor("t_" + name, shape, dt))
            bFence = Buf("fence")
            E("dve", lambda e: e.memset(ssq[:], 0.0), w=mixer_tail_bufs + [bFence, bSsq])
            E("act", lambda e: e.copy(out=junk[:, 0:1], in_=ssq[:]), r=[bFence, bSsq], w=[bFence, bJunk])
            E("pool", lambda e: e.tensor_copy(out=junk[:, 1:2], in_=ssq[:]), r=[bFence, bSsq], w=[bFence, bJunk])
            E("pe", lambda e: e.transpose(out=pT0[:, 0:128], in_=ident[:], identity=ident[:]), r=[bFence, bId], w=[bFence, bT0])
            S.dma("sp", lambda e: e.dma_start(out=msk[:], in_=msk_d), bMsk, reads=[bFence], writes=[bFence, bMsk])
            S.dma("pool", lambda e: e.dma_start(out=msk[:], in_=msk_d), bMsk, reads=[bFence], writes=[bFence, bMsk])

            skt = sb3("skt", [128, 16, 128], BF16); bSkt = Buf("skt")
            S.dma("pool", lambda e: e.dma_start(out=skt[:], in_=skt_d), bSkt, reads=[bFence], writes=[bSkt])
            wqb = [sb3("wqb%d" % i, [128, 8, 128], BF16) for i in range(2)]; bWq = [Buf("wqb%d" % i) for i in range(2)]
            x1t = sb3("x1t", [128, D]); bX1 = Buf("x1t")
            h2T = sb3("h2T", [128, 8, 128], BF16); bH2T = Buf("h2T")
            qT = sb3("qT", [128, 16, 128], BF16); bQT = Buf("qT")
            ssb = sb3("ssb", [128, 16, 128]); bSs = Buf("ssb")
            stmp = sb3("stmp", [128, 16, 128]); bStmp = Buf("stmp")
            tv = sb3("tv", [128, 16, 16]); bTv = Buf("tv")
            c8 = sb3("c8", [128, 8, 16]); bC8 = Buf("c8")
            negm = sb3("negm", [128, 8]); bNegm = Buf("negm")
            Zs = sb3("Zs", [128, 8]); bZs = Buf("Zs")
            rZ = sb3("rZ", [128, 8]); bRZ = Buf("rZ")
            ez = sb3("ez", [128, 16, 128]); bEz = Buf("ez")
            cand = stmp[:].rearrange("p (h a) b -> p h (a b)", a=2); bCand = bStmp
            cand2 = ez[:].rearrange("p (h a) b -> p h (a b)", a=2); bCand2 = bEz
            bigA = sb3("bigA", [128, 128, 128], BF16); bBigAh = [Buf("bigA%d" % i) for i in range(8)]
            XT = sb3("XT", [128, 128, 128], BF16); bXT = Buf("XT")
            YT = sb3("YT", [128, 128, 128], BF16); bYT = Buf("YT")
            NUV = 2
            utb = [sb3("utb%d" % i, [128, 4, 8, 128], BF16) for i in range(NUV)]; bUt = [Buf("utb%d" % i) for i in range(NUV)]
            vb = [sb3("vb%d" % i, [128, 4, D], BF16) for i in range(NUV)]; bVb = [Buf("vb%d" % i) for i in range(NUV)]
            ga = [sb3("ga%d" % i, [128, 512], BF16) for i in range(2)]; bGa = [Buf("ga%d" % i) for i in range(2)]
            wT = [sb3("wT%d" % i, [128, 4, 128], BF16) for i in range(2)]; bWT = [Buf("wT%d" % i) for i in range(2)]

            x1ts = [x1t, sb3("x1tB", [128, D])]; bX1b = [bX1, Buf("x1tB")]
            h2Ts = [h2T, sb3("h2TB", [128, 8, 128], BF16)]; bH2Tb = [bH2T, Buf("h2TB")]
            bYTh = [Buf("YTh%d" % i) for i in range(8)]
            zbufs = [(stmp, bStmp), (ez, bEz)]

            def transposes(srcTok, bSrc, dstT, bDstW):
                for i8 in range(16):
                    pt, bpt = (pT0, bT0) if i8 % 2 == 0 else (pT1, bT1)
                    for j in range(8):
                        i = i8 * 8 + j
                        E("pe", lambda e, i=i, j=j, pt=pt: e.transpose(out=pt[:, j * 128:(j + 1) * 128], in_=srcTok[:, :, i], identity=ident[:]), r=bSrc + [bId], w=[bpt])
                    src = pt[:]
                    dst = dstT[:, i8 * 8:(i8 + 1) * 8, :].rearrange("p a b -> p (a b)")
                    if i8 % 2 == 0:
                        E("act", lambda e, src=src, dst=dst: e.copy(out=dst, in_=src), r=[bpt], w=bDstW)
                    else:
                        E("dve", lambda e, src=src, dst=dst: e.tensor_copy(out=dst, in_=src), r=[bpt], w=bDstW)
                    yield

            def front(gi):
                r0 = gi * 128
                xt_, bx_ = x1ts[gi % 2], bX1b[gi % 2]
                hT_, bh_ = h2Ts[gi % 2], bH2Tb[gi % 2]
                S.dma("act", lambda e: e.dma_start(out=xt_[:], in_=x1s_d[r0:r0 + 128, :]), bx_, reads=[bX1s[gi]], writes=[bx_])
                norm_mod(xt_[:], bx_, g2b, bG2, sh2b[:], bSh2)
                transpose_hb(hT_[:], bh_)
                yield
                S.dma("act", lambda e: e.dma_start(out=wqb[0][:], in_=wqs_d[0]), bWq[0], reads=[bWqs[0]], writes=[bWq[0]])
                for hp in range(16):
                    k = hp % 2
                    if hp + 1 < 16:
                        S.dma("act", lambda e, hp=hp: e.dma_start(out=wqb[(hp + 1) % 2][:], in_=wqs_d[hp + 1]), bWq[(hp + 1) % 2], reads=[bWqs[(hp + 1) // 8]], writes=[bWq[(hp + 1) % 2]])
                    bk = bB[(hp // 4) % 2]
                    dst = bank(PB, (hp // 4) % 2)[:, (hp % 4) * 128:(hp % 4 + 1) * 128]
                    for c in range(8):
                        E("pe", lambda e, c=c, k=k, dst=dst: e.matmul(dst, lhsT=wqb[k][:, c, :], rhs=hT_[:, c, :], start=(c == 0), stop=(c == 7)), r=[bWq[k], bh_], w=[bk])
                    if hp % 4 == 3:
                        q4 = hp // 4
                        E("act", lambda e, q4=q4: e.copy(out=qT[:, q4 * 4:(q4 + 1) * 4, :], in_=bank(PB, q4 % 2).rearrange("p (a t) -> p a t", a=4)), r=[bk], w=[bQT])
                    yield
                for hp in range(16):
                    bk = bB[(hp // 4) % 2]
                    dst = bank(PB, (hp // 4) % 2)[:, (hp % 4) * 128:(hp % 4 + 1) * 128]
                    E("pe", lambda e, hp=hp, dst=dst: e.matmul(dst, lhsT=qT[:, hp, :], rhs=skt[:, hp, :], start=True, stop=True), r=[bQT, bSkt], w=[bk])
                    if hp % 4 == 3:
                        q4 = hp // 4
                        E("act", lambda e, q4=q4: e.copy(out=ssb[:, q4 * 4:(q4 + 1) * 4, :], in_=bank(PB, q4 % 2).rearrange("p (a t) -> p a t", a=4)), r=[bk], w=[bSs])
                        yield
                bTvh = [Buf("tvh%d" % i) for i in range(16)]; bStmph = [Buf("stmph%d" % i) for i in range(16)]
                for hp in range(16):
                    E("dve", lambda e, hp=hp: e.max(out=tv[:, hp, 0:8], in_=ssb[:, hp, :]), r=[bSs, bTv], w=[bTvh[hp]])
                    if hp % 4 == 3:
                        yield
                for hp in range(16):
                    E("dve", lambda e, hp=hp: e.match_replace(out=stmp[:, hp, :], in_to_replace=tv[:, hp, 0:8], in_values=ssb[:, hp, :], imm_value=NEG), r=[bSs, bTvh[hp], bStmp], w=[bStmph[hp]])
                    if hp % 4 == 3:
                        yield
                for hp in range(16):
                    E("dve", lambda e, hp=hp: e.max(out=tv[:, hp, 8:16], in_=stmp[:, hp, :]), r=[bStmph[hp]], w=[bTvh[hp]])
                    if hp % 4 == 3:
                        yield
                E("dve", lambda e: e.memset(ssq[:], 0.0), r=bTvh + bStmph, w=[bTv, bStmp, bSsq])
                in0 = mk(tv, 0, [[32, 8], [1, 16], [0, 16]])
                in1 = mk(tv, 16, [[32, 8], [0, 16], [1, 16]])
                E("dve", lambda e: e.tensor_tensor(out=cand.rearrange("p h (a b) -> p h a b", a=16), in0=in0, in1=in1, op=ALU.add), r=[bTv], w=[bCand])
                yield
                bC8h = [Buf("c8h%d" % i) for i in range(8)]; bCand2h = [Buf("cand2h%d" % i) for i in range(8)]
                for h in range(8):
                    E("dve", lambda e, h=h: e.max(out=c8[:, h, 0:8], in_=cand[:, h, :]), r=[bCand, bC8], w=[bC8h[h]])
                yield
                for h in range(8):
                    E("dve", lambda e, h=h: e.match_replace(out=cand2[:, h, :], in_to_replace=c8[:, h, 0:8], in_values=cand[:, h, :], imm_value=NEG), r=[bCand, bC8h[h], bCand2], w=[bCand2h[h]])
                yield
                for h in range(8):
                    E("dve", lambda e, h=h: e.max(out=c8[:, h, 8:16], in_=cand2[:, h, :]), r=[bCand2h[h]], w=[bC8h[h]])
                E("dve", lambda e: e.memset(ssq[:], 0.0), r=bC8h + bCand2h, w=[bC8, bCand2, bSsq])
                E("dve", lambda e: e.tensor_scalar(out=negm[:], in0=c8[:, :, 0], scalar1=-1.0, scalar2=None, op0=ALU.mult), r=[bC8], w=[bNegm])
                E("dve", lambda e: e.memset(Zs[:], 0.0), w=[bZs])
                yield
                for h in range(8):
                    zb, bZb = zbufs[h % 2]
                    zin0 = mk(ssb, (2 * h + 1) * 128, [[0, 16], [1, 128]])
                    zin1 = mk(tv, (2 * h) * 16, [[1, 16], [0, 128]])
                    slot = YT[:, h * 16:(h + 1) * 16, :].rearrange("p a b -> p (a b)")
                    E("pool", lambda e, zin0=zin0, zin1=zin1, zb=zb: e.tensor_tensor(out=zb[:], in0=zin0, in1=zin1, op=ALU.add), r=[bSs, bTv], w=[bZb])
                    E("act", lambda e, h=h, zb=zb, slot=slot: e.activation(out=slot, in_=zb[:].rearrange("p a b -> p (a b)"), func=AF.Exp, bias=negm[:, h:h + 1]), r=[bZb, bNegm], w=[bYTh[h]])
                    E("dve", lambda e, h=h, zb=zb, slot=slot: e.scalar_tensor_tensor(out=slot, in0=zb[:].rearrange("p a b -> p (a b)"), scalar=c8[:, h, 15:16], in1=slot, op0=ALU.is_ge, op1=ALU.mult, accum_out=Zs[:, h:h + 1]), r=[bZb, bC8, bYTh[h], bZs], w=[bYTh[h], bZs])
                    yield
                E("dve", lambda e: e.reciprocal(out=rZ[:], in_=Zs[:]), r=[bZs], w=[bRZ])
                yield from transposes(YT, bYTh, XT, [bXT])

            def tail(gi):
                for h in range(8):
                    zb, bZb = zbufs[h % 2]
                    yin0 = mk(ssb, (2 * h) * 128, [[0, 16], [1, 128]])
                    yin1 = mk(tv, (2 * h) * 16, [[1, 16], [0, 128]])
                    E("dve", lambda e, yin0=yin0, yin1=yin1, zb=zb: e.tensor_tensor(out=zb[:], in0=yin0, in1=yin1, op=ALU.is_equal), r=[bSs, bTv], w=[bZb])
                    E("dve", lambda e, h=h, zb=zb: e.tensor_scalar(out=bigA[:, h * 16:(h + 1) * 16, :].rearrange("p a b -> p (a b)"), in0=zb[:].rearrange("p a b -> p (a b)"), scalar1=rZ[:, h:h + 1], scalar2=None, op0=ALU.mult), r=[bZb, bRZ], w=[bBigAh[h]])
                for _ in transposes(bigA, bBigAh, YT, bYTh):
                    pass
                for t4 in range(32):
                    P, bP = (PA, bA) if (t4 // 2) % 2 == 0 else (PB, bB)
                    half = t4 % 2
                    for j in range(4):
                        t = t4 * 4 + j
                        E("pe", lambda e, t=t, j=j, P=P, half=half: e.matmul(bank(P, half)[:, j * 128:(j + 1) * 128], lhsT=XT[:, :, t], rhs=YT[:, :, t], start=True, stop=True), r=[bXT] + bYTh, w=[bP[half]])
                    src = bank(P, half)
                    dst = bigA[:, t4 * 4:(t4 + 1) * 4, :].rearrange("p a b -> p (a b)")
                    if t4 % 2 == 0:
                        E("act", lambda e, src=src, dst=dst: e.copy(out=dst, in_=src), r=[bP[half]], w=bBigAh)
                    else:
                        E("dve", lambda e, src=src, dst=dst: e.tensor_copy(out=dst, in_=src), r=[bP[half]], w=bBigAh)
                if gi == 0:
                    dump("ssb", ssb[:], [128, 16, 128], bSs)
                    dump("tv", tv[:], [128, 16, 16], bTv)
                    dump("c8", c8[:], [128, 8, 16], bC8)
                    dump("Zs", Zs[:], [128, 8], bZs)
                    dump("GT", bigA[:], [128, 128, 128], bBigAh[0], BF16)

            def mainloop(gi):
                hT_, bh_ = h2Ts[gi % 2], bH2Tb[gi % 2]

                def emitA(j4):
                    k = j4 % NUV
                    k2 = j4 % 2
                    S.dma("sp", lambda e: e.dma_start(out=utb[k][:], in_=uts_d[j4 * 4:(j4 + 1) * 4].rearrange("j p c e -> p j c e")), bUt[k], reads=[bUs[j4 // 2]], writes=[bUt[k]])
                    S.dma("sp", lambda e: e.dma_start(out=vb[k][:], in_=vs_d[j4 * 512:(j4 + 1) * 512, :].rearrange("(j p) d -> p j d", p=128)), bVb[k], reads=[bVs[j4 // 2]], writes=[bVb[k]])
                    for jj in range(4):
                        for c in range(8):
                            E("pe", lambda e, c=c, jj=jj: e.matmul(bank(PA, k2)[:, jj * 128:(jj + 1) * 128], lhsT=utb[k][:, jj, c, :], rhs=hT_[:, c, :], start=(c == 0), stop=(c == 7)), r=[bUt[k], bh_], w=[bA[k2]])

                emitA(0)
                for j4 in range(32):
                    k = j4 % NUV
                    k2 = j4 % 2
                    E("act", lambda e, k2=k2: e.activation(out=ga[k2][:], in_=bank(PA, k2), func=AF.Gelu), r=[bA[k2]], w=[bGa[k2]])
                    E("pool", lambda e, k2=k2, j4=j4: e.tensor_tensor(out=wT[k2][:], in0=ga[k2][:].rearrange("p (a b) -> p a b", a=4), in1=mk(bigA, j4 * 4, [[1, 4], [128, 128]]), op=ALU.mult), r=[bGa[k2]] + bBigAh, w=[bWT[k2]])
                    if j4 + 1 < 32:
                        emitA(j4 + 1)
                    for jj in range(4):
                        j = j4 * 4 + jj
                        for half in range(2):
                            E("pe", lambda e, half=half, k=k, k2=k2, j=j, jj=jj: e.matmul(bank(PC, half), lhsT=wT[k2][:, jj, :], rhs=vb[k][:, jj, half * 512:(half + 1) * 512], start=(j == 0), stop=(j == 127)), r=[bWT[k2], bVb[k]], w=[bC[half]])
                    yield

            def epilogue(gi):
                r0 = gi * 128
                ot_, bo_ = x1ts[gi % 2], bX1b[gi % 2]
                if do_peer:
                    for half in range(2):
                        hs = slice(half * 512, (half + 1) * 512)
                        E("dve", lambda e, half=half, hs=hs: e.tensor_tensor(out=junk[:, hs], in0=bank(PC, half), in1=gt2b[:, hs], op=ALU.mult), r=[bC[half], bGt2, bJunk], w=[bJunk])
                    E("pool", lambda e: e.tensor_tensor(out=ot_[:], in0=junk[:], in1=ot_[:], op=ALU.add), r=[bJunk, bo_], w=[bo_])
                E("dve", lambda e: e.memset(ssq[:], 0.0), w=[bSsq])
                E("act", lambda e: e.activation(out=junk[:], in_=ot_[:], func=AF.Square, accum_out=ssq[:, 0:1]), r=[bo_, bSsq], w=[bJunk, bSsq])
                E("dve", lambda e: e.tensor_scalar(out=ssq[:], in0=ssq[:], scalar1=1.0 / D, scalar2=EPS, op0=ALU.mult, op1=ALU.add), r=[bSsq], w=[bSsq])
                E("act", lambda e: e.activation(out=ssq[:], in_=ssq[:], func=AF.Sqrt), r=[bSsq], w=[bSsq])
                E("dve", lambda e: e.reciprocal(out=ssq[:], in_=ssq[:]), r=[bSsq], w=[bSsq])
                E("dve", lambda e: e.scalar_tensor_tensor(out=ot_[:], in0=ot_[:], scalar=ssq[:, 0:1], in1=fwb[:], op0=ALU.mult, op1=ALU.mult), r=[bo_, bSsq, bFw], w=[bo_])
                final_ops.append(S.dma("sp", lambda e: e.dma_start(out=out_d[r0:r0 + 128, :], in_=ot_[:]), bo_, reads=[bo_]))

            def run_all(g):
                for _ in g:
                    pass

            if not do_peer:
                for gi in range(ngroups):
                    r0 = gi * 128
                    S.dma("sp", lambda e, r0=r0, gi=gi: e.dma_start(out=x1ts[gi % 2][:], in_=x1s_d[r0:r0 + 128, :]), bX1b[gi % 2], reads=[bX1s[gi]], writes=[bX1b[gi % 2]])
                    epilogue(gi)
            else:
                run_all(front(0))
                tail(0)
                if dbg:
                    dump("h2T", h2Ts[0][:], [128, 8, 128], bH2Tb[0], BF16)
                for gi in range(ngroups):
                    nxt = front(gi + 1) if gi + 1 < ngroups else iter(())
                    for it, _ in enumerate(mainloop(gi)):
                        for _k in range(1 if it < 17 else 4):
                            next(nxt, None)
                    run_all(nxt)
                    epilogue(gi)
                    if gi + 1 < ngroups:
                        tail(gi + 1)

            S.finalize(final_ops + dumps)
            S.run()
    return nc


def _prep_shared(w_ada, b_ada, norm1_w, w_in, hgrn_lb_logits, hgrn_onorm_w, conv_w, conv_onorm_w, w_out,
                 norm2_w, peer_w_query, peer_sub_keys, peer_u, peer_v, final_norm_w):
    f = np.float32
    c = np.ascontiguousarray

    def pm(w):
        return c(np.asarray(w, f).reshape(8, 128, -1).transpose(1, 0, 2))
    sh = {}
    sh["wada"] = pm(w_ada[0])
    sh["bada"] = c(np.asarray(b_ada, f).reshape(1, -1))
    sh["n1w"] = c(np.asarray(norm1_w, f).reshape(1, -1))
    sh["n2w"] = c(np.asarray(norm2_w, f).reshape(1, -1))
    sh["fw"] = c(np.asarray(final_norm_w, f).reshape(1, -1))
    sh["win"] = pm(w_in[0])
    sh["lbl"] = c(np.asarray(hgrn_lb_logits, f).reshape(2, 4, 128).transpose(2, 0, 1))
    sh["onw"] = c(np.asarray(hgrn_onorm_w, f).reshape(4, 128).T)
    sh["cnw"] = c(np.asarray(conv_onorm_w, f).reshape(4, 128).T)
    sh["cw"] = c(np.asarray(conv_w, f).reshape(3, 4, 128).transpose(2, 0, 1))
    sh["wout"] = pm(w_out[0])
    sh["wq"] = c(np.asarray(peer_w_query[0], f).reshape(8, 128, 16, 128).transpose(2, 1, 0, 3))
    sh["skt"] = c(np.asarray(peer_sub_keys, f).reshape(16, 128, 128).transpose(2, 0, 1))
    sh["ut"] = c(np.asarray(peer_u, f).reshape(128, 128, 8, 128).transpose(0, 3, 2, 1))
    sh["v"] = c(np.asarray(peer_v, f).reshape(16384, 1024))
    return sh


def _in_maps(x, c, sh, npre):
    f = np.float32
    x = np.asarray(x, f); cc = np.asarray(c, f)
    maps = []
    for core in range(8):
        b, jj = core // 4, core % 4
        m = dict(sh)
        m["x"] = np.ascontiguousarray(x[b, jj * TOK:(jj + 1) * TOK])
        xpre = np.zeros((max(npre, 1) * TOK, D), f)
        msk = np.zeros((128, 4), f)
        for s in range(npre):
            src = jj - npre + s
            if src >= 0:
                xpre[s * TOK:(s + 1) * TOK] = x[b, src * TOK:(src + 1) * TOK]
                msk[:, s] = 1.0
        msk[:, 3] = 1.0 if (jj >= 1 and npre >= 1) else 0.0
        m["xpre"] = xpre
        m["msk"] = msk
        m["cT"] = np.ascontiguousarray(cc[b].reshape(8, 128).T)
        maps.append(m)
    return maps


_NC_CACHE = {}


def kernel(x, c, w_ada, b_ada, norm1_w, w_in, hgrn_lb_logits, hgrn_onorm_w, conv_w, conv_onorm_w, w_out,
           norm2_w, peer_w_query, peer_sub_keys, peer_u, peer_v, final_norm_w):
    sh = _prep_shared(w_ada, b_ada, norm1_w, w_in, hgrn_lb_logits, hgrn_onorm_w, conv_w, conv_onorm_w, w_out,
                      norm2_w, peer_w_query, peer_sub_keys, peer_u, peer_v, final_norm_w)
    maps = _in_maps(x, c, sh, NPRE)
    nc = build_nc(NPRE)
    res = run_bass_kernel_spmd(nc, maps, core_ids=list(range(8)))
    out = np.zeros((2, 8192, D), np.float32)
    for core in range(8):
        b, jj = core // 4, core % 4
        out[b, jj * TOK:(jj + 1) * TOK] = res.results[core]["out"]
    return out
```

```python
import contextlib
import numpy as np
import concourse.bass as bass
import concourse.mybir as mybir
from concourse.bass_utils import run_bass_kernel_spmd

F32 = mybir.dt.float32
BF16 = mybir.dt.bfloat16
ALU = mybir.AluOpType
AF = mybir.ActivationFunctionType
STRICT_SAME_ENGINE = True


class Buf:
    __slots__ = ("name", "w", "r", "sem", "cnt")

    def __init__(self, name):
        self.name = name
        self.w = None
        self.r = {}
        self.sem = None
        self.cnt = 0


class Op:
    __slots__ = ("eng", "fn", "deps", "flag", "val", "sem", "isdma", "name")

    def __init__(self, eng, fn, isdma=False, name=""):
        self.eng = eng
        self.fn = fn
        self.deps = []
        self.flag = False
        self.val = None
        self.sem = None
        self.isdma = isdma
        self.name = name


class Sched:
    ENGS = ("pe", "act", "dve", "pool", "sp")

    def __init__(self, nc, stack):
        self.nc = nc
        self.stack = stack
        self.q = {e: [] for e in self.ENGS}
        self.esem = {e: stack.enter_context(nc.semaphore("s_" + e)) for e in self.ENGS}
        self.nsem = len(self.ENGS)
        self.ndma = 0

    def newsem(self, name):
        self.nsem += 1
        return self.stack.enter_context(self.nc.semaphore(name))

    def _deps(self, op, reads, writes):
        deps = []
        for b in reads:
            if b.w is not None:
                deps.append(b.w)
        for b in writes:
            if b.w is not None:
                deps.append(b.w)
            deps.extend(b.r.values())
        seen = set()
        for d in deps:
            if d is op or id(d) in seen:
                continue
            seen.add(id(d))
            if (not d.isdma) and d.eng == op.eng and (op.eng == "pe" or not STRICT_SAME_ENGINE):
                continue
            op.deps.append(d)
        for b in reads:
            key = ("dma", id(op)) if op.isdma else op.eng
            b.r[key] = op
        for b in writes:
            b.w = op
            b.r = {}

    def emit(self, eng, fn, reads=(), writes=(), name=""):
        op = Op(eng, fn, name=name)
        self._deps(op, reads, writes)
        self.q[eng].append(op)
        return op

    def dma(self, eng, fn, sbuf, reads=(), writes=(), nparts=1, name=""):
        op = Op(eng, fn, isdma=True, name=name)
        self._deps(op, reads, writes)
        if sbuf.sem is None:
            sbuf.sem = self.newsem("d_" + sbuf.name)
        sbuf.cnt += 16 * nparts
        op.sem = sbuf.sem
        op.val = sbuf.cnt
        self.q[eng].append(op)
        self.ndma += nparts
        return op

    def finalize(self, final_ops):
        for e in self.ENGS:
            for op in self.q[e]:
                for d in op.deps:
                    if not d.isdma:
                        d.flag = True
        for e in self.ENGS:
            c = 0
            for op in self.q[e]:
                if not op.isdma and op.flag:
                    c += 1
                    op.val = c
                    op.sem = self.esem[e]
        self.final_ops = final_ops

    def replay(self, eng, e):
        seen = {}
        nwait = 0
        for op in self.q[eng]:
            for d in op.deps:
                k = id(d.sem)
                if seen.get(k, 0) >= d.val:
                    continue
                seen[k] = d.val
                e.wait_ge(d.sem, d.val)
                nwait += 1
            r = op.fn(e)
            if op.isdma:
                lst = r if isinstance(r, (list, tuple)) else [r]
                for ins in lst:
                    ins.then_inc(op.sem, 16)
            elif op.flag:
                r.then_inc(op.sem, 1)
        if eng == "sp":
            for d in self.final_ops:
                e.wait_ge(d.sem, d.val)
        return nwait

    def run(self):
        nc = self.nc
        with nc.Block() as block:
            @block.sync
            def _(e):
                self.replay("sp", e)

            @block.scalar
            def _(e):
                self.replay("act", e)

            @block.vector
            def _(e):
                self.replay("dve", e)

            @block.gpsimd
            def _(e):
                self.replay("pool", e)

            @block.tensor
            def _(e):
                self.replay("pe", e)


def ap_of(t):
    return t if isinstance(t, bass.AP) else t[:]


def mk(base, offset_elems, dims):
    b = ap_of(base)
    pstep, pcnt = b.ap[0]
    return bass.AP(b.tensor, b.offset + offset_elems, [[pstep, pcnt]] + [list(d) for d in dims])

D = 1024
TOK = 2048
NPRE = 3
EPS = 1e-6
NEG = -1.0e30


def build_nc(npre=NPRE, dbg=False, do_peer=True, ngroups=16):
    nc = bass.Bass("TRN2", target_bir_lowering=False)

    def din(name, shape):
        return nc.dram_tensor(name, shape, F32, kind="ExternalInput").ap()

    x_d = din("x", [TOK, D])
    xpre_d = din("xpre", [max(npre, 1) * TOK, D])
    msk_d = din("msk", [128, 4])
    cT_d = din("cT", [128, 8])
    wada_d = din("wada", [128, 8, 6 * D])
    bada_d = din("bada", [1, 6 * D])
    n1w_d = din("n1w", [1, D])
    n2w_d = din("n2w", [1, D])
    fw_d = din("fw", [1, D])
    win_d = din("win", [128, 8, 3584])
    lbl_d = din("lbl", [128, 2, 4])
    onw_d = din("onw", [128, 4])
    cnw_d = din("cnw", [128, 4])
    cw_d = din("cw", [128, 3, 4])
    wout_d = din("wout", [128, 8, D])
    wq_d = din("wq", [16, 128, 8, 128])
    skt_d = din("skt", [128, 16, 128])
    ut_d = din("ut", [128, 128, 8, 128])
    v_d = din("v", [16384, D])
    out_d = nc.dram_tensor("out", [TOK, D], F32, kind="ExternalOutput").ap()
    x1s_d = nc.dram_tensor("x1s", [TOK, D], F32, kind=("ExternalOutput" if dbg else "Internal")).ap()
    uts_d = nc.dram_tensor("uts", [128, 128, 8, 128], BF16, kind="Internal").ap()
    vs_d = nc.dram_tensor("vs", [16384, D], BF16, kind="Internal").ap()
    wqs_d = nc.dram_tensor("wqs", [16, 128, 8, 128], BF16, kind="Internal").ap()

    with contextlib.ExitStack() as st:
        S = Sched(nc, st)

        def sb(name, shape, dt=F32):
            return st.enter_context(nc.sbuf_tensor("t_" + name, shape, dt))

        def ps(name, shape, dt=F32):
            return st.enter_context(nc.psum_tensor("t_" + name, shape, dt))

        def E(eng, fn, r=(), w=()):
            return S.emit(eng, fn, reads=r, writes=w)

        dumps = []

        def dump(name, ap, shape, buf, dt=F32):
            if not dbg:
                return
            dd = nc.dram_tensor("dbg_" + name, shape, dt, kind="ExternalOutput").ap()
            dumps.append(S.dma("sp", lambda e: e.dma_start(out=dd, in_=ap), buf, reads=[buf]))

        pT0 = ps("pT0", [128, 1024], BF16); bT0 = Buf("pT0")
        pT1 = ps("pT1", [128, 1024], BF16); bT1 = Buf("pT1")
        PA = ps("PA", [128, 1024], F32); bA = [Buf("PA0"), Buf("PA1")]
        PB = ps("PB", [128, 1024], F32); bB = [Buf("PB0"), Buf("PB1")]
        PC = ps("PC", [128, 1024], F32); bC = [Buf("PC0"), Buf("PC1")]

        def bank(P, i):
            return P[:, i * 512:(i + 1) * 512]

        identf = sb("identf", [128, 128]); ident = sb("ident", [128, 128], BF16); bId = Buf("ident")
        E("pool", lambda e: e.memset(identf[:], 1.0), w=[bId])
        E("pool", lambda e: e.affine_select(out=identf[:], in_=identf[:], pattern=[[-1, 128]], compare_op=ALU.is_equal, fill=0.0, base=0, channel_multiplier=1), r=[bId], w=[bId])
        E("pool", lambda e: e.tensor_copy(out=ident[:], in_=identf[:]), r=[bId], w=[bId])
        onesf = sb("onesf", [128, 128]); ones_bf = sb("ones_bf", [128, 128], BF16); bOn = Buf("ones")
        E("pool", lambda e: e.memset(onesf[:], 1.0), w=[bOn])
        E("pool", lambda e: e.tensor_copy(out=ones_bf[:], in_=onesf[:]), r=[bOn], w=[bOn])
        maskST = sb("maskST", [128, 128]); bMk = Buf("maskST")
        E("pool", lambda e: e.memset(maskST[:], 1.0), w=[bMk])
        E("pool", lambda e: e.affine_select(out=maskST[:], in_=maskST[:], pattern=[[1, 128]], compare_op=ALU.is_ge, fill=0.0, base=0, channel_multiplier=-1), r=[bMk], w=[bMk])
        maskI = sb("maskI", [128, 128], mybir.dt.int32)
        E("pool", lambda e: e.tensor_copy(out=maskI[:], in_=maskST[:]), r=[bMk], w=[bMk])

        iotai = sb("iotai", [128, 128], mybir.dt.int32); iotaf = sb("iotaf", [128, 128]); bIota = Buf("iota")
        E("pool", lambda e: e.iota(out=iotai[:], pattern=[[1, 128]], base=0, channel_multiplier=0), w=[bIota])
        E("pool", lambda e: e.tensor_copy(out=iotaf[:], in_=iotai[:]), r=[bIota], w=[bIota])
        small = {}

        def load_small(name, src, shape):
            t = sb(name, shape); b = Buf(name)
            S.dma("sp", lambda e: e.dma_start(out=t[:], in_=src), b, writes=[b])
            small[name] = (t, b)
            return t, b

        msk, bMsk = load_small("msk", msk_d, [128, 4])
        cT, bcT = load_small("cT", cT_d, [128, 8])
        lbl, bLbl = load_small("lbl", lbl_d, [128, 2, 4])
        onw, bOnw = load_small("onw", onw_d, [128, 4])
        cnw, bCnw = load_small("cnw", cnw_d, [128, 4])
        cw, bCw = load_small("cw", cw_d, [128, 3, 4])

        lb = sb("lb", [128, 4]); oml = sb("oml", [128, 4]); noml = sb("noml", [128, 4]); bLb = Buf("lb")
        E("dve", lambda e: e.tensor_tensor(out=lb[:], in0=lbl[:, 0, :], in1=lbl[:, 1, :], op=ALU.subtract), r=[bLbl], w=[bLb])
        E("act", lambda e: e.activation(out=lb[:], in_=lb[:], func=AF.Sigmoid), r=[bLb], w=[bLb])
        E("dve", lambda e: e.tensor_scalar(out=oml[:], in0=lb[:], scalar1=-1.0, scalar2=1.0, op0=ALU.mult, op1=ALU.add), r=[bLb], w=[bLb])
        E("dve", lambda e: e.tensor_scalar(out=noml[:], in0=lb[:], scalar1=-1.0, scalar2=None, op0=ALU.add), r=[bLb], w=[bLb])

        g2b = sb("g2b", [128, D]); fwb = sb("fwb", [128, D]); sh2b = sb("sh2b", [128, D]); gt2b = sb("gt2b", [128, D])
        bG2 = Buf("g2b"); bFw = Buf("fwb"); bSh2 = Buf("sh2b"); bGt2 = Buf("gt2b")
        junk = sb("junk", [128, D]); bJunk = Buf("junk")
        ssq = sb("ssq", [128, 1]); bSsq = Buf("ssq")
        hb = sb("hb", [128, D], BF16); bHb = Buf("hb")

        def norm_mod(xap, bX, gb, bG, shiftap, bShift):
            E("dve", lambda e: e.memset(ssq[:], 0.0), w=[bSsq])
            E("act", lambda e: e.activation(out=junk[:], in_=xap, func=AF.Square, accum_out=ssq[:, 0:1]), r=[bX, bSsq], w=[bJunk, bSsq])
            E("dve", lambda e: e.tensor_scalar(out=ssq[:], in0=ssq[:], scalar1=1.0 / D, scalar2=EPS, op0=ALU.mult, op1=ALU.add), r=[bSsq], w=[bSsq])
            E("act", lambda e: e.activation(out=ssq[:], in_=ssq[:], func=AF.Sqrt), r=[bSsq], w=[bSsq])
            E("dve", lambda e: e.reciprocal(out=ssq[:], in_=ssq[:]), r=[bSsq], w=[bSsq])
            E("dve", lambda e: e.scalar_tensor_tensor(out=junk[:], in0=xap, scalar=ssq[:, 0:1], in1=gb[:], op0=ALU.mult, op1=ALU.mult), r=[bX, bSsq, bG, bJunk], w=[bJunk])
            E("pool", lambda e: e.tensor_tensor(out=hb[:], in0=junk[:], in1=shiftap, op=ALU.add), r=[bJunk, bShift], w=[bHb])

        def transpose_hb(dst3, bDst):
            for c in range(8):
                E("pe", lambda e, c=c: e.transpose(out=pT0[:, c * 128:(c + 1) * 128], in_=hb[:, c * 128:(c + 1) * 128], identity=ident[:]), r=[bHb, bId], w=[bT0])
            E("act", lambda e: e.copy(out=dst3, in_=pT0[:].rearrange("p (c t) -> p c t", c=8)), r=[bT0], w=[bDst])

        with contextlib.ExitStack() as st2:
            def sb2(name, shape, dt=F32):
                return st2.enter_context(nc.sbuf_tensor("t_" + name, shape, dt))
            win = sb2("win", [128, 8, 3584], BF16); bWin = Buf("win")
            for c in range(8):
                S.dma("pool", lambda e, c=c: e.dma_start(out=win[:, c, :], in_=win_d[:, c, :]), bWin, writes=[bWin])
            wout = sb2("wout", [128, 8, D], BF16); bWout = Buf("wout")
            for c in range(0, 8, 4):
                S.dma("pool", lambda e, c=c: e.dma_start(out=wout[:, c:c + 4, :], in_=wout_d[:, c:c + 4, :]), bWout, writes=[bWout])
            bUs = [Buf("uts%d" % i) for i in range(16)]; bVs = [Buf("vs%d" % i) for i in range(16)]
            xt = sb2("xt", [128, 4, D]); bXt = [Buf("xt%d" % i) for i in range(4)]
            h1T = sb2("h1T", [128, 8, 512], BF16); bH1T = [Buf("h1T%d" % i) for i in range(4)]
            itok = sb2("itok", [128, 4, 512], BF16); bItok = [Buf("itok%d" % i) for i in range(4)]
            sg = sb2("sg", [128, 512]); bSg = Buf("sg")
            lf = sb2("lf", [128, 512]); bLf = Buf("lf")
            kk = sb2("kk", [128, 512]); bKk = Buf("kk")
            qs = sb2("qs", [128, 512]); bQs = Buf("qs")
            gs = sb2("gs", [128, 512]); bGs = Buf("gs")
            btc = [sb2("btc%d" % i, [128, 128]) for i in range(4)]; bBtc = [Buf("btc%d" % i) for i in range(4)]
            exc = [sb2("exc%d" % i, [128, 128]) for i in range(4)]; bExc = [Buf("exc%d" % i) for i in range(4)]
            khTc = [sb2("khTc%d" % i, [128, 128], BF16) for i in range(4)]; bKhTc = [Buf("khTc%d" % i) for i in range(4)]
            khc = [sb2("khc%d" % i, [128, 128], BF16) for i in range(4)]; bKhc = [Buf("khc%d" % i) for i in range(4)]
            qtc = [sb2("qtc%d" % i, [128, 128], BF16) for i in range(4)]; bQtc = [Buf("qtc%d" % i) for i in range(4)]
            ktc = [sb2("ktc%d" % i, [128, 128], BF16) for i in range(4)]; bKtc = [Buf("ktc%d" % i) for i in range(4)]
            qhc = [sb2("qhc%d" % i, [128, 128], BF16) for i in range(4)]; bQhc = [Buf("qhc%d" % i) for i in range(4)]
            scmc = [sb2("scmc%d" % i, [128, 128], BF16) for i in range(4)]; bScmc = [Buf("scmc%d" % i) for i in range(4)]
            ebc = sb2("ebc", [128, 4]); bEbc = [Buf("ebc%d" % i) for i in range(4)]
            nbmc = sb2("nbmc", [128, 4]); bNbmc = [Buf("nbmc%d" % i) for i in range(4)]
            bT1s = [Buf("pT1s%d" % i) for i in range(4)]; bB0s = [Buf("PB0s%d" % i) for i in range(4)]; bC0s = [Buf("PC0s%d" % i) for i in range(4)]
            bt = btc[0]; bBt = bBtc[0]
            Sst = sb2("Sst", [128, 4, 128]); bS = [Buf("S%d" % i) for i in range(4)]
            Sbf = sb2("Sbf", [128, 4, 128], BF16); bSbf = [Buf("Sbf%d" % i) for i in range(4)]
            sq = sb2("sq", [128, 512], BF16); bSq = Buf("sq")
            rb = sb2("rb", [128, 512]); bRb = Buf("rb")
            yv = sb2("yv", [128, 512]); bYv = Buf("yv")
            ymT = sb2("ymT", [128, 8, 512], BF16); bYm = [Buf("ym%d" % i) for i in range(8)]
            u = sb2("u", [128, 4, 514]); bU = [Buf("u%d" % i) for i in range(4)]
            csb = sg; bCsb = bSg
            y0 = lf; bY0 = bLf
            y1 = kk; bY1 = bKk
            t1 = junk; bTt1 = bJunk

            modb = sb2("modb", [128, 6 * D]); bMod = Buf("modb")
            cact = sb2("cact", [128, 8]); crep = sb2("crep", [128, 8, 128]); bCrep = Buf("crep")
            E("act", lambda e: e.activation(out=cact[:], in_=cT[:], func=AF.Silu), r=[bcT], w=[bCrep])
            for c in range(8):
                E("dve", lambda e, c=c: e.tensor_scalar(out=crep[:, c, :], in0=onesf[:], scalar1=cact[:, c:c + 1], scalar2=None, op0=ALU.mult), r=[bOn, bCrep], w=[bCrep])
            S.dma("sp", lambda e: e.dma_start(out=modb[:], in_=bada_d.partition_broadcast(128)), bMod, writes=[bMod])
            wst = [xt[:, 0:2, :].rearrange("p a (b n) -> p (a b) n", n=256), xt[:, 2:4, :].rearrange("p a (b n) -> p (a b) n", n=256)]
            bWst = [[bXt[0], bXt[1]], [bXt[2], bXt[3]]]
            for g in range(24):
                k = g % 2
                S.dma("sp", lambda e, g=g, k=k: e.dma_start(out=wst[k], in_=wada_d[:, :, g * 256:(g + 1) * 256]), bWst[k][0], writes=bWst[k])
                for c in range(8):
                    E("pe", lambda e, c=c, k=k: e.matmul(bank(PA, k)[:, 0:256], lhsT=crep[:, c, :], rhs=wst[k][:, c, :], start=(c == 0), stop=(c == 7)), r=[bCrep] + bWst[k], w=[bA[k]])
                E("dve", lambda e, g=g, k=k: e.tensor_tensor(out=modb[:, g * 256:(g + 1) * 256], in0=bank(PA, k)[:, 0:256], in1=modb[:, g * 256:(g + 1) * 256], op=ALU.add), r=[bA[k], bMod], w=[bMod])
            g1b = sb2("g1b", [128, D]); bG1 = Buf("g1b")
            S.dma("sp", lambda e: e.dma_start(out=g1b[:], in_=n1w_d.partition_broadcast(128)), bG1, writes=[bG1])
            S.dma("sp", lambda e: e.dma_start(out=g2b[:], in_=n2w_d.partition_broadcast(128)), bG2, writes=[bG2])
            S.dma("sp", lambda e: e.dma_start(out=fwb[:], in_=fw_d.partition_broadcast(128)), bFw, writes=[bFw])
            E("dve", lambda e: e.scalar_tensor_tensor(out=g1b[:], in0=modb[:, D:2 * D], scalar=1.0, in1=g1b[:], op0=ALU.add, op1=ALU.mult), r=[bMod, bG1], w=[bG1])
            E("dve", lambda e: e.scalar_tensor_tensor(out=g2b[:], in0=modb[:, 4 * D:5 * D], scalar=1.0, in1=g2b[:], op0=ALU.add, op1=ALU.mult), r=[bMod, bG2], w=[bG2])
            shift1 = modb[:, 0:D]; gate1 = modb[:, 2 * D:3 * D]
            dump("modb", modb[:], [128, 6 * D], bMod)
            dump("g1b", g1b[:], [128, D], bG1)
            dump("lb", lb[:], [128, 4], bLb)
            E("dve", lambda e: e.tensor_copy(out=sh2b[:], in_=modb[:, 3 * D:4 * D]), r=[bMod], w=[bSh2])
            E("dve", lambda e: e.tensor_copy(out=gt2b[:], in_=modb[:, 5 * D:6 * D]), r=[bMod], w=[bGt2])

            bWqs = [Buf("wqs%d" % i) for i in range(2)]
            if do_peer:
                for i in range(2):
                    S.dma("pool", lambda e, i=i: e.dma_start(out=wqs_d[i * 8:(i + 1) * 8], in_=wq_d[i * 8:(i + 1) * 8]), bWqs[i], reads=[bSh2, bGt2, bG1], writes=[bWqs[i]])
                for i in range(16):
                    S.dma("pool", lambda e, i=i: e.dma_start(out=uts_d[i * 8:(i + 1) * 8], in_=ut_d[i * 8:(i + 1) * 8]), bUs[i], reads=[bSh2, bGt2, bG1], writes=[bUs[i]])
                    S.dma("pool", lambda e, i=i: e.dma_start(out=vs_d[i * 1024:(i + 1) * 1024, :], in_=v_d[i * 1024:(i + 1) * 1024, :]), bVs[i], reads=[bSh2, bGt2, bG1], writes=[bVs[i]])
            for ck in range(4):
                E("pool", lambda e, ck=ck: e.memset(scmc[ck][:], 0.0), w=[bScmc[ck]])
            for h in range(4):
                E("dve", lambda e, h=h: e.memset(Sst[:, h, :], 0.0), w=[bS[h]])
                E("pool", lambda e, h=h: e.memset(Sbf[:, h, :], 0.0), w=[bSbf[h]])
                E("pool", lambda e, h=h: e.memset(u[:, h, 0:2], 0.0), w=[bU[h]])

            rot = [0]

            def projbank():
                k = rot[0] % 2
                rot[0] += 1
                return k

            def proj_fm(col0, n):
                k = projbank()
                for c in range(8):
                    E("pe", lambda e, c=c, k=k: e.matmul(bank(PA, k)[:, 0:n], lhsT=win[:, c, col0:col0 + 128], rhs=h1T[:, c, 0:n], start=(c == 0), stop=(c == 7)), r=[bWin] + bH1T, w=[bA[k]])
                return bank(PA, k)[:, 0:n], bA[k]

            def hgrn_head(h, ntile, mode):
                n = ntile * 128
                fp, bf_ = proj_fm(512 + h * 128, n)
                E("act", lambda e: e.activation(out=sg[:, 0:n], in_=fp, func=AF.Sigmoid), r=[bf_], w=[bSg])
                E("act", lambda e: e.activation(out=lf[:, 0:n], in_=sg[:, 0:n], func=AF.Ln, scale=oml[:, h:h + 1], bias=lb[:, h:h + 1]), r=[bSg, bLb], w=[bLf])
                E("dve", lambda e: e.tensor_scalar(out=kk[:, 0:n], in0=sg[:, 0:n], scalar1=noml[:, h:h + 1], scalar2=oml[:, h:h + 1], op0=ALU.mult, op1=ALU.add), r=[bSg, bLb], w=[bKk])
                if mode == "main":
                    qp, bq_ = proj_fm(h * 128, n)
                    E("act", lambda e: e.copy(out=qs[:, 0:n], in_=qp), r=[bq_], w=[bQs])
                    gp, bg_ = proj_fm(1536 + h * 128, n)
                    E("act", lambda e: e.activation(out=gs[:, 0:n], in_=gp, func=AF.Silu), r=[bg_], w=[bGs])
                NT = ntile
                css = [slice(ck * 128, (ck + 1) * 128) for ck in range(NT)]
                isls = [itok[:, ck, h * 128:(h + 1) * 128] for ck in range(NT)]
                for ck in range(NT):
                    E("dve", lambda e, ck=ck: e.tensor_tensor_scan(out=btc[ck][:], data0=onesf[:], data1=lf[:, css[ck]], initial=0.0, op0=ALU.mult, op1=ALU.add), r=[bOn, bLf], w=[bBtc[ck]])
                for ck in range(NT):
                    E("act", lambda e, ck=ck: e.activation(out=exc[ck][:], in_=btc[ck][:], func=AF.Exp, scale=-1.0, bias=btc[ck][:, 127:128]), r=[bBtc[ck]], w=[bExc[ck]])
                for ck in range(NT):
                    E("dve", lambda e, ck=ck: e.tensor_tensor(out=khTc[ck][:], in0=kk[:, css[ck]], in1=exc[ck][:], op=ALU.mult), r=[bKk, bExc[ck]], w=[bKhTc[ck]])
                for ck in range(NT):
                    E("pe", lambda e, ck=ck: e.transpose(out=pT1[:, ck * 128:(ck + 1) * 128], in_=khTc[ck][:], identity=ident[:]), r=[bKhTc[ck], bId], w=[bT1])
                for ck in range(NT):
                    E("act", lambda e, ck=ck: e.copy(out=khc[ck][:], in_=pT1[:, ck * 128:(ck + 1) * 128]), r=[bT1], w=[bKhc[ck]])
                for ck in range(NT):
                    E("pe", lambda e, ck=ck: e.matmul(bank(PC, 0)[:, css[ck]], lhsT=khc[ck][:], rhs=isls[ck], start=True, stop=True), r=[bKhc[ck], bItok[ck]], w=[bC[0]])
                for ck in range(NT):
                    E("act", lambda e, ck=ck: e.activation(out=ebc[:, ck:ck + 1], in_=btc[ck][:, 127:128], func=AF.Exp), r=[bBtc[ck]], w=[bEbc[ck]])
                if mode == "main":
                    for ck in range(NT):
                        E("dve", lambda e, ck=ck: e.tensor_scalar(out=nbmc[:, ck:ck + 1], in0=btc[ck][:, 63:64], scalar1=-1.0, scalar2=None, op0=ALU.mult), r=[bBtc[ck]], w=[bNbmc[ck]])
                    for ck in range(NT):
                        E("act", lambda e, ck=ck: e.activation(out=exc[ck][:], in_=btc[ck][:], func=AF.Exp, bias=nbmc[:, ck:ck + 1]), r=[bBtc[ck], bNbmc[ck], bExc[ck]], w=[bExc[ck]])
                    for ck in range(NT):
                        E("dve", lambda e, ck=ck: e.tensor_tensor(out=qtc[ck][:], in0=qs[:, css[ck]], in1=exc[ck][:], op=ALU.mult), r=[bQs, bExc[ck]], w=[bQtc[ck]])
                    for ck in range(NT):
                        E("act", lambda e, ck=ck: e.activation(out=exc[ck][:], in_=btc[ck][:], func=AF.Exp, scale=-1.0, bias=btc[ck][:, 63:64]), r=[bBtc[ck], bExc[ck]], w=[bExc[ck]])
                    for ck in range(NT):
                        E("pool", lambda e, ck=ck: e.tensor_tensor(out=ktc[ck][:], in0=kk[:, css[ck]], in1=exc[ck][:], op=ALU.mult), r=[bKk, bExc[ck]], w=[bKtc[ck]])
                    for ck in range(NT):
                        E("act", lambda e, ck=ck: e.activation(out=exc[ck][:], in_=btc[ck][:], func=AF.Exp), r=[bBtc[ck], bExc[ck]], w=[bExc[ck]])
                    for ck in range(NT):
                        E("dve", lambda e, ck=ck: e.tensor_tensor(out=qhc[ck][:], in0=qs[:, css[ck]], in1=exc[ck][:], op=ALU.mult), r=[bQs, bExc[ck]], w=[bQhc[ck]])
                    for ck in range(NT):
                        E("pe", lambda e, ck=ck: e.matmul(bank(PB, 0)[:, css[ck]], lhsT=ktc[ck][:], rhs=qtc[ck][:], start=True, stop=True), r=[bKtc[ck], bQtc[ck]], w=[bB[0]])
                    for ck in range(NT):
                        E("dve", lambda e, ck=ck: e.copy_predicated(out=scmc[ck][:], mask=maskI[:], data=bank(PB, 0)[:, css[ck]]), r=[bB[0], bMk, bScmc[ck]], w=[bScmc[ck]])
                for ck in range(NT):
                    if mode == "main":
                        E("pe", lambda e, ck=ck: e.matmul(bank(PB, 1)[:, css[ck]], lhsT=isls[ck], rhs=scmc[ck][:], start=True, stop=False), r=[bItok[ck], bScmc[ck]], w=[bB[1]])
                        E("pe", lambda e, ck=ck: e.matmul(bank(PB, 1)[:, css[ck]], lhsT=Sbf[:, h, :], rhs=qhc[ck][:], start=False, stop=True), r=[bSbf[h], bQhc[ck]], w=[bB[1]])
                    E("dve", lambda e, ck=ck: e.scalar_tensor_tensor(out=Sst[:, h, :], in0=Sst[:, h, :], scalar=ebc[:, ck:ck + 1], in1=bank(PC, 0)[:, css[ck]], op0=ALU.mult, op1=ALU.add), r=[bS[h], bEbc[ck], bC[0]], w=[bS[h]])
                    if mode == "main":
                        E("pool", lambda e: e.tensor_copy(out=Sbf[:, h, :], in_=Sst[:, h, :]), r=[bS[h]], w=[bSbf[h]])
                if mode == "main":
                    groupnorm_out(bank(PB, 1), bB[1], onw[:, h:h + 1], bOnw, h, mulgs=True)

            def groupnorm_out(src, bSrc, wcol, bW, slot, mulgs):
                E("act", lambda e: e.activation(out=sq[:], in_=src, func=AF.Square), r=[bSrc], w=[bSq])
                E("pe", lambda e: e.matmul(bank(PC, 1), lhsT=ones_bf[:], rhs=sq[:], start=True, stop=True), r=[bOn, bSq], w=[bC[1]])
                E("dve", lambda e: e.tensor_scalar(out=rb[:], in0=bank(PC, 1), scalar1=1.0 / 128, scalar2=EPS, op0=ALU.mult, op1=ALU.add), r=[bC[1]], w=[bRb])
                E("act", lambda e: e.activation(out=rb[:], in_=rb[:], func=AF.Sqrt), r=[bRb], w=[bRb])
                E("dve", lambda e: e.reciprocal(out=rb[:], in_=rb[:]), r=[bRb], w=[bRb])
                if mulgs:
                    E("dve", lambda e: e.scalar_tensor_tensor(out=yv[:], in0=src, scalar=wcol, in1=rb[:], op0=ALU.mult, op1=ALU.mult), r=[bSrc, bW, bRb], w=[bYv])
                    E("pool", lambda e: e.tensor_tensor(out=ymT[:, slot, :], in0=yv[:], in1=gs[:], op=ALU.mult), r=[bYv, bGs], w=[bYm[slot]])
                else:
                    E("dve", lambda e: e.scalar_tensor_tensor(out=ymT[:, slot, :], in0=src, scalar=wcol, in1=rb[:], op0=ALU.mult, op1=ALU.mult), r=[bSrc, bW, bRb], w=[bYm[slot]])

            def conv_group(g, mode):
                if mode == "halo":
                    cp, bc_ = proj_fm(2560 + g * 128, 128)
                    E("act", lambda e: e.copy(out=csb[:, 0:128], in_=cp), r=[bc_], w=[bCsb])
                    xp, bx_ = proj_fm(3072 + g * 128, 128)
                    E("dve", lambda e: e.tensor_tensor(out=y0[:, 0:128], in0=csb[:, 0:128], in1=xp, op=ALU.mult), r=[bCsb, bx_], w=[bY0])
                    E("dve", lambda e: e.tensor_scalar(out=u[:, g, 0:2], in0=y0[:, 126:128], scalar1=msk[:, 3:4], scalar2=None, op0=ALU.mult), r=[bY0, bMsk], w=[bU[g]])
                    return
                cp, bc_ = proj_fm(2560 + g * 128, 512)
                E("act", lambda e: e.copy(out=csb[:], in_=cp), r=[bc_], w=[bCsb])
                xp, bx_ = proj_fm(3072 + g * 128, 512)
                E("dve", lambda e: e.tensor_tensor(out=u[:, g, 2:514], in0=csb[:], in1=xp, op=ALU.mult), r=[bCsb, bx_], w=[bU[g]])
                bp, bb_ = proj_fm(2048 + g * 128, 512)
                E("dve", lambda e: e.tensor_scalar(out=y0[:], in0=u[:, g, 2:514], scalar1=cw[:, 2, g:g + 1], scalar2=None, op0=ALU.mult), r=[bU[g], bCw], w=[bY0])
                E("dve", lambda e: e.scalar_tensor_tensor(out=y1[:], in0=u[:, g, 1:513], scalar=cw[:, 1, g:g + 1], in1=y0[:], op0=ALU.mult, op1=ALU.add), r=[bU[g], bCw, bY0], w=[bY1])
                E("dve", lambda e: e.scalar_tensor_tensor(out=y0[:], in0=u[:, g, 0:512], scalar=cw[:, 0, g:g + 1], in1=y1[:], op0=ALU.mult, op1=ALU.add), r=[bU[g], bCw, bY1], w=[bY0])
                E("dve", lambda e: e.tensor_tensor(out=y1[:], in0=y0[:], in1=bp, op=ALU.mult), r=[bY0, bb_], w=[bY1])
                E("pool", lambda e: e.tensor_copy(out=u[:, g, 0:2], in_=u[:, g, 512:514]), r=[bU[g]], w=[bU[g]])
                groupnorm_out(y1[:], bY1, cnw[:, g:g + 1], bCnw, 4 + g, mulgs=False)

            def mixer_macro(src_d, row0, ntile, mode):
                n = ntile * 128
                for tt in range(ntile):
                    S.dma("sp", lambda e, tt=tt: e.dma_start(out=xt[:, tt, :], in_=src_d[row0 + tt * 128: row0 + (tt + 1) * 128, :]), bXt[tt], writes=[bXt[tt]])
                    norm_mod(xt[:, tt, :], bXt[tt], g1b, bG1, shift1, bMod)
                    transpose_hb(h1T[:, :, tt * 128:(tt + 1) * 128], bH1T[tt])
                if mode == "halo":
                    for g in range(4):
                        conv_group(g, "halo")
                    return
                for tt in range(ntile):
                    k = projbank()
                    for c in range(8):
                        E("pe", lambda e, c=c, k=k, tt=tt: e.matmul(bank(PA, k), lhsT=h1T[:, c, tt * 128:(tt + 1) * 128], rhs=win[:, c, 1024:1536], start=(c == 0), stop=(c == 7)), r=[bWin, bH1T[tt]], w=[bA[k]])
                    E("act", lambda e, k=k, tt=tt: e.copy(out=itok[:, tt, :], in_=bank(PA, k)), r=[bA[k]], w=[bItok[tt]])
                for h in range(4):
                    hgrn_head(h, ntile, mode)
                if mode != "main":
                    return
                for g in range(4):
                    conv_group(g, "main")
                for tt in range(ntile):
                    for half in range(2):
                        for cc in range(8):
                            E("pe", lambda e, cc=cc, half=half, tt=tt: e.matmul(bank(PA, half), lhsT=ymT[:, cc, tt * 128:(tt + 1) * 128], rhs=wout[:, cc, half * 512:(half + 1) * 512], start=(cc == 0), stop=(cc == 7)), r=[bYm[cc], bWout], w=[bA[half]])
                    for half in range(2):
                        hs = slice(half * 512, (half + 1) * 512)
                        E("dve", lambda e, half=half, hs=hs: e.tensor_tensor(out=t1[:, hs], in0=bank(PA, half), in1=gate1[:, hs], op=ALU.mult), r=[bA[half], bMod, bTt1], w=[bTt1])
                    E("pool", lambda e, tt=tt: e.tensor_tensor(out=xt[:, tt, :], in0=t1[:], in1=xt[:, tt, :], op=ALU.add), r=[bTt1, bXt[tt]], w=[bXt[tt]])
                    S.dma("sp", lambda e, tt=tt: e.dma_start(out=x1s_d[row0 + tt * 128: row0 + (tt + 1) * 128, :], in_=xt[:, tt, :]), bXt[tt], reads=[bXt[tt]], writes=[bX1s[(row0 + tt * 128) // 128]])

            bX1s = [Buf("x1s%d" % i) for i in range(16)]
            for seg in range(npre):
                for m in range(4):
                    mixer_macro(xpre_d, seg * TOK + m * 512, 4, "pre")
                for h in range(4):
                    E("dve", lambda e, h=h, seg=seg: e.tensor_scalar(out=Sst[:, h, :], in0=Sst[:, h, :], scalar1=msk[:, seg:seg + 1], scalar2=None, op0=ALU.mult), r=[bS[h], bMsk], w=[bS[h]])
            if npre > 0:
                mixer_macro(xpre_d, npre * TOK - 128, 1, "halo")
            for h in range(4):
                E("pool", lambda e, h=h: e.tensor_copy(out=Sbf[:, h, :], in_=Sst[:, h, :]), r=[bS[h]], w=[bSbf[h]])
            for m in range(4):
                mixer_macro(x_d, m * 512, 4, "main")
                if m == 0:
                    dump("ymT", ymT[:], [128, 8, 512], bYm[7], BF16)
                    dump("h1T", h1T[:], [128, 8, 512], bH1T[3], BF16)
                    dump("itok", itok[:], [128, 4, 512], bItok[3], BF16)
                    dump("Sst", Sst[:], [128, 4, 128], bS[3])
                    dump("u", u[:], [128, 4, 514], bU[3])
            mixer_tail_bufs = [bWin, bWout] + bXt + bH1T + bItok + [bSg, bLf, bKk, bQs, bGs] + bBtc + bExc + bKhTc + bKhc + bQtc + bKtc + bQhc + bScmc + bEbc + bNbmc + bT1s + bB0s + bC0s + bS + bSbf + [bSq, bRb, bYv] + bYm + bU + [bMod, bG1, bCrep]

        final_ops = []
        with contextlib.ExitStack() as st3:
            def sb3(name, shape, dt=F32):
                return st3.enter_context(nc.sbuf_tensor("t_" + name, shape, dt))
            bFence = Buf("fence")
            E("dve", lambda e: e.memset(ssq[:], 0.0), w=mixer_tail_bufs + [bFence, bSsq])
            E("act", lambda e: e.copy(out=junk[:, 0:1], in_=ssq[:]), r=[bFence, bSsq], w=[bFence, bJunk])
            E("pool", lambda e: e.tensor_copy(out=junk[:, 1:2], in_=ssq[:]), r=[bFence, bSsq], w=[bFence, bJunk])
            E("pe", lambda e: e.transpose(out=pT0[:, 0:128], in_=ident[:], identity=ident[:]), r=[bFence, bId], w=[bFence, bT0])
            S.dma("sp", lambda e: e.dma_start(out=msk[:], in_=msk_d), bMsk, reads=[bFence], writes=[bFence, bMsk])
            S.dma("pool", lambda e: e.dma_start(out=msk[:], in_=msk_d), bMsk, reads=[bFence], writes=[bFence, bMsk])

            skt = sb3("skt", [128, 16, 128], BF16); bSkt = Buf("skt")
            S.dma("pool", lambda e: e.dma_start(out=skt[:], in_=skt_d), bSkt, reads=[bFence], writes=[bSkt])
            wqb = [sb3("wqb%d" % i, [128, 8, 128], BF16) for i in range(2)]; bWq = [Buf("wqb%d" % i) for i in range(2)]
            x1t = sb3("x1t", [128, D]); bX1 = Buf("x1t")
            h2T = sb3("h2T", [128, 8, 128], BF16); bH2T = Buf("h2T")
            qT = sb3("qT", [128, 16, 128], BF16); bQT = Buf("qT")
            ssb = sb3("ssb", [128, 16, 128]); bSs = Buf("ssb")
            stmp = sb3("stmp", [128, 16, 128]); bStmp = Buf("stmp")
            tv = sb3("tv", [128, 16, 16]); bTv = Buf("tv")
            c8 = sb3("c8", [128, 8, 16]); bC8 = Buf("c8")
            negm = sb3("negm", [128, 8]); bNegm = Buf("negm")
            Zs = sb3("Zs", [128, 8]); bZs = Buf("Zs")
            rZ = sb3("rZ", [128, 8]); bRZ = Buf("rZ")
            ez = sb3("ez", [128, 16, 128]); bEz = Buf("ez")
            cand = stmp[:].rearrange("p (h a) b -> p h (a b)", a=2); bCand = bStmp
            cand2 = ez[:].rearrange("p (h a) b -> p h (a b)", a=2); bCand2 = bEz
            bigA = sb3("bigA", [128, 128, 128], BF16); bBigAh = [Buf("bigA%d" % i) for i in range(8)]
            XT = sb3("XT", [128, 128, 128], BF16); bXT = Buf("XT")
            YT = sb3("YT", [128, 128, 128], BF16); bYT = Buf("YT")
            NUV = 2
            utb = [sb3("utb%d" % i, [128, 4, 8, 128], BF16) for i in range(NUV)]; bUt = [Buf("utb%d" % i) for i in range(NUV)]
            vb = [sb3("vb%d" % i, [128, 4, D], BF16) for i in range(NUV)]; bVb = [Buf("vb%d" % i) for i in range(NUV)]
            ga = [sb3("ga%d" % i, [128, 512], BF16) for i in range(2)]; bGa = [Buf("ga%d" % i) for i in range(2)]
            wT = [sb3("wT%d" % i, [128, 4, 128], BF16) for i in range(2)]; bWT = [Buf("wT%d" % i) for i in range(2)]

            qTf = qT[:].rearrange("p a b -> p (a b)").bitcast(F32)
            qTu = qT[:].rearrange("p a b -> p (a b)").bitcast(mybir.dt.uint32)
            idxf = qTf[:, 0:128]; bIdxf = bQT
            idxT = qTf[:, 128:256]; bIdxT = bQT
            e16 = qTf[:, 256:384].rearrange("p (a b) -> p a b", a=8); bE16 = bQT
            idxu = qTu[:, 384:512].rearrange("p (a b) -> p a b", a=8); bIdxu = bQT
            x1ts = [x1t, sb3("x1tB", [128, D])]; bX1b = [bX1, Buf("x1tB")]
            h2Ts = [h2T, sb3("h2TB", [128, 8, 128], BF16)]; bH2Tb = [bH2T, Buf("h2TB")]
            bYTh = [Buf("YTh%d" % i) for i in range(8)]
            zbufs = [(stmp, bStmp), (ez, bEz)]

            def transposes(srcTok, bSrc, dstT, bDstW):
                for i8 in range(16):
                    pt, bpt = (pT0, bT0) if i8 % 2 == 0 else (pT1, bT1)
                    for j in range(8):
                        i = i8 * 8 + j
                        E("pe", lambda e, i=i, j=j, pt=pt: e.transpose(out=pt[:, j * 128:(j + 1) * 128], in_=srcTok[:, :, i], identity=ident[:]), r=bSrc + [bId], w=[bpt])
                    src = pt[:]
                    dst = dstT[:, i8 * 8:(i8 + 1) * 8, :].rearrange("p a b -> p (a b)")
                    if i8 % 2 == 0:
                        E("act", lambda e, src=src, dst=dst: e.copy(out=dst, in_=src), r=[bpt], w=bDstW)
                    else:
                        E("dve", lambda e, src=src, dst=dst: e.tensor_copy(out=dst, in_=src), r=[bpt], w=bDstW)
                    yield

            def front(gi):
                r0 = gi * 128
                xt_, bx_ = x1ts[gi % 2], bX1b[gi % 2]
                hT_, bh_ = h2Ts[gi % 2], bH2Tb[gi % 2]
                S.dma("act", lambda e: e.dma_start(out=xt_[:], in_=x1s_d[r0:r0 + 128, :]), bx_, reads=[bX1s[gi]], writes=[bx_])
                norm_mod(xt_[:], bx_, g2b, bG2, sh2b[:], bSh2)
                transpose_hb(hT_[:], bh_)
                yield
                S.dma("act", lambda e: e.dma_start(out=wqb[0][:], in_=wqs_d[0]), bWq[0], reads=[bWqs[0]], writes=[bWq[0]])
                for hp in range(16):
                    k = hp % 2
                    if hp + 1 < 16:
                        S.dma("act", lambda e, hp=hp: e.dma_start(out=wqb[(hp + 1) % 2][:], in_=wqs_d[hp + 1]), bWq[(hp + 1) % 2], reads=[bWqs[(hp + 1) // 8]], writes=[bWq[(hp + 1) % 2]])
                    bk = bB[(hp // 4) % 2]
                    dst = bank(PB, (hp // 4) % 2)[:, (hp % 4) * 128:(hp % 4 + 1) * 128]
                    for c in range(8):
                        E("pe", lambda e, c=c, k=k, dst=dst: e.matmul(dst, lhsT=wqb[k][:, c, :], rhs=hT_[:, c, :], start=(c == 0), stop=(c == 7)), r=[bWq[k], bh_], w=[bk])
                    if hp % 4 == 3:
                        q4 = hp // 4
                        E("act", lambda e, q4=q4: e.copy(out=qT[:, q4 * 4:(q4 + 1) * 4, :], in_=bank(PB, q4 % 2).rearrange("p (a t) -> p a t", a=4)), r=[bk], w=[bQT])
                    yield
                for hp in range(16):
                    bk = bB[(hp // 4) % 2]
                    dst = bank(PB, (hp // 4) % 2)[:, (hp % 4) * 128:(hp % 4 + 1) * 128]
                    E("pe", lambda e, hp=hp, dst=dst: e.matmul(dst, lhsT=qT[:, hp, :], rhs=skt[:, hp, :], start=True, stop=True), r=[bQT, bSkt], w=[bk])
                    if hp % 4 == 3:
                        q4 = hp // 4
                        E("act", lambda e, q4=q4: e.copy(out=ssb[:, q4 * 4:(q4 + 1) * 4, :], in_=bank(PB, q4 % 2).rearrange("p (a t) -> p a t", a=4)), r=[bk], w=[bSs])
                        yield
                bTvh = [Buf("tvh%d" % i) for i in range(16)]; bStmph = [Buf("stmph%d" % i) for i in range(16)]
                for hp in range(16):
                    E("dve", lambda e, hp=hp: e.max(out=tv[:, hp, 0:8], in_=ssb[:, hp, :]), r=[bSs, bTv], w=[bTvh[hp]])
                    if hp % 4 == 3:
                        yield
                for hp in range(16):
                    E("dve", lambda e, hp=hp: e.match_replace(out=stmp[:, hp, :], in_to_replace=tv[:, hp, 0:8], in_values=ssb[:, hp, :], imm_value=NEG), r=[bSs, bTvh[hp], bStmp], w=[bStmph[hp]])
                    if hp % 4 == 3:
                        yield
                for hp in range(16):
                    E("dve", lambda e, hp=hp: e.max(out=tv[:, hp, 8:16], in_=stmp[:, hp, :]), r=[bStmph[hp]], w=[bTvh[hp]])
                    if hp % 4 == 3:
                        yield
                E("dve", lambda e: e.memset(ssq[:], 0.0), r=bTvh + bStmph, w=[bTv, bStmp, bSsq])
                in0 = mk(tv, 0, [[32, 8], [1, 16], [0, 16]])
                in1 = mk(tv, 16, [[32, 8], [0, 16], [1, 16]])
                E("dve", lambda e, in0=in0, in1=in1: e.tensor_tensor(out=cand.rearrange("p h (a b) -> p h a b", a=16), in0=in0, in1=in1, op=ALU.add), r=[bTv], w=[bCand])
                yield
                bC8h = [Buf("c8h%d" % i) for i in range(8)]; bCand2h = [Buf("cand2h%d" % i) for i in range(8)]
                for h in range(8):
                    E("dve", lambda e, h=h: e.max(out=c8[:, h, 0:8], in_=cand[:, h, :]), r=[bCand, bC8], w=[bC8h[h]])
                yield
                for h in range(8):
                    E("dve", lambda e, h=h: e.match_replace(out=cand2[:, h, :], in_to_replace=c8[:, h, 0:8], in_values=cand[:, h, :], imm_value=NEG), r=[bCand, bC8h[h], bCand2], w=[bCand2h[h]])
                yield
                for h in range(8):
                    E("dve", lambda e, h=h: e.max(out=c8[:, h, 8:16], in_=cand2[:, h, :]), r=[bCand2h[h]], w=[bC8h[h]])
                E("dve", lambda e: e.memset(ssq[:], 0.0), r=bC8h + bCand2h, w=[bC8, bCand2, bSsq])
                E("dve", lambda e: e.tensor_scalar(out=negm[:], in0=c8[:, :, 0], scalar1=-1.0, scalar2=None, op0=ALU.mult), r=[bC8], w=[bNegm])
                E("dve", lambda e: e.tensor_tensor(out=e16, in0=c8[:], in1=mk(c8, 0, [[16, 8], [0, 16]]), op=ALU.subtract), r=[bC8], w=[bE16])
                E("act", lambda e: e.activation(out=e16, in_=e16, func=AF.Exp), r=[bE16], w=[bE16])
                E("dve", lambda e: e.tensor_reduce(out=Zs[:], in_=e16, axis=mybir.AxisListType.X, op=ALU.add), r=[bE16], w=[bZs])
                E("act", lambda e: e.activation(out=Zs[:], in_=Zs[:], func=AF.Ln), r=[bZs], w=[bZs])
                E("dve", lambda e: e.tensor_tensor(out=negm[:], in0=negm[:], in1=Zs[:], op=ALU.subtract), r=[bNegm, bZs], w=[bNegm])
                yield
                for h in range(8):
                    E("dve", lambda e, h=h: e.max_index(out=idxu[:, h, 0:8], in_max=tv[:, 2 * h, 0:8], in_values=ssb[:, 2 * h, :]), r=[bTv, bSs, bIdxu], w=[bIdxu])
                    E("dve", lambda e, h=h: e.max_index(out=idxu[:, h, 8:16], in_max=tv[:, 2 * h, 8:16], in_values=ssb[:, 2 * h, :]), r=[bTv, bSs, bIdxu], w=[bIdxu])
                    if h % 4 == 3:
                        yield
                E("dve", lambda e: e.tensor_copy(out=idxf, in_=idxu.rearrange("p a b -> p (a b)")), r=[bIdxu], w=[bIdxf])
                E("pe", lambda e: e.transpose(out=bank(PB, 0)[:, 0:128], in_=idxf, identity=identf[:]), r=[bIdxf, bId], w=[bB[0]])
                E("act", lambda e: e.copy(out=idxT, in_=bank(PB, 0)[:, 0:128]), r=[bB[0]], w=[bIdxT])
                yield
                for h in range(8):
                    zb, bZb = zbufs[h % 2]
                    zin0 = mk(ssb, (2 * h + 1) * 128, [[0, 16], [1, 128]])
                    zin1 = mk(tv, (2 * h) * 16, [[1, 16], [0, 128]])
                    slot = YT[:, h * 16:(h + 1) * 16, :].rearrange("p a b -> p (a b)")
                    E("pool", lambda e, zin0=zin0, zin1=zin1, zb=zb: e.tensor_tensor(out=zb[:], in0=zin0, in1=zin1, op=ALU.add), r=[bSs, bTv], w=[bZb])
                    E("act", lambda e, h=h, zb=zb, slot=slot: e.activation(out=slot, in_=zb[:].rearrange("p a b -> p (a b)"), func=AF.Exp, bias=negm[:, h:h + 1]), r=[bZb, bNegm], w=[bYTh[h]])
                    E("dve", lambda e, h=h, zb=zb, slot=slot: e.scalar_tensor_tensor(out=slot, in0=zb[:].rearrange("p a b -> p (a b)"), scalar=c8[:, h, 15:16], in1=slot, op0=ALU.is_ge, op1=ALU.mult), r=[bZb, bC8, bYTh[h]], w=[bYTh[h]])
                    yield
                yield from transposes(YT, bYTh, XT, [bXT])
                for q in range(4):
                    dst = YT[:, q * 32:(q + 1) * 32, :]
                    yi0 = mk(iotaf, q * 32, [[1, 32], [0, 128]])
                    yi1 = mk(idxT, 0, [[0, 32], [1, 128]])
                    E("dve", lambda e, dst=dst, yi0=yi0, yi1=yi1: e.tensor_tensor(out=dst, in0=yi0, in1=yi1, op=ALU.is_equal), r=[bIota, bIdxT] + bYTh, w=bYTh)
                    yield

            def tail(gi):
                for t4 in range(32):
                    P, bP = (PA, bA) if (t4 // 2) % 2 == 0 else (PB, bB)
                    half = t4 % 2
                    for j in range(4):
                        t = t4 * 4 + j
                        E("pe", lambda e, t=t, j=j, P=P, half=half: e.matmul(bank(P, half)[:, j * 128:(j + 1) * 128], lhsT=XT[:, :, t], rhs=YT[:, :, t], start=True, stop=True), r=[bXT] + bYTh, w=[bP[half]])
                    src = bank(P, half)
                    dst = bigA[:, t4 * 4:(t4 + 1) * 4, :].rearrange("p a b -> p (a b)")
                    if t4 % 2 == 0:
                        E("act", lambda e, src=src, dst=dst: e.copy(out=dst, in_=src), r=[bP[half]], w=bBigAh)
                    else:
                        E("dve", lambda e, src=src, dst=dst: e.tensor_copy(out=dst, in_=src), r=[bP[half]], w=bBigAh)
                if gi == 0:
                    dump("ssb", ssb[:], [128, 16, 128], bSs)
                    dump("tv", tv[:], [128, 16, 16], bTv)
                    dump("c8", c8[:], [128, 8, 16], bC8)
                    dump("Zs", Zs[:], [128, 8], bZs)
                    dump("GT", bigA[:], [128, 128, 128], bBigAh[0], BF16)

            def mainloop(gi):
                hT_, bh_ = h2Ts[gi % 2], bH2Tb[gi % 2]

                def emitA(j4):
                    k = j4 % NUV
                    k2 = j4 % 2
                    S.dma("sp", lambda e: e.dma_start(out=utb[k][:], in_=uts_d[j4 * 4:(j4 + 1) * 4].rearrange("j p c e -> p j c e")), bUt[k], reads=[bUs[j4 // 2]], writes=[bUt[k]])
                    S.dma("sp", lambda e: e.dma_start(out=vb[k][:], in_=vs_d[j4 * 512:(j4 + 1) * 512, :].rearrange("(j p) d -> p j d", p=128)), bVb[k], reads=[bVs[j4 // 2]], writes=[bVb[k]])
                    for jj in range(4):
                        for c in range(8):
                            E("pe", lambda e, c=c, jj=jj: e.matmul(bank(PA, k2)[:, jj * 128:(jj + 1) * 128], lhsT=utb[k][:, jj, c, :], rhs=hT_[:, c, :], start=(c == 0), stop=(c == 7)), r=[bUt[k], bh_], w=[bA[k2]])

                emitA(0)
                for j4 in range(32):
                    k = j4 % NUV
                    k2 = j4 % 2
                    E("act", lambda e, k2=k2: e.activation(out=ga[k2][:], in_=bank(PA, k2), func=AF.Gelu), r=[bA[k2]], w=[bGa[k2]])
                    E("pool", lambda e, k2=k2, j4=j4: e.tensor_tensor(out=wT[k2][:], in0=ga[k2][:].rearrange("p (a b) -> p a b", a=4), in1=mk(bigA, j4 * 4, [[1, 4], [128, 128]]), op=ALU.mult), r=[bGa[k2]] + bBigAh, w=[bWT[k2]])
                    if j4 + 1 < 32:
                        emitA(j4 + 1)
                    for jj in range(4):
                        j = j4 * 4 + jj
                        for half in range(2):
                            E("pe", lambda e, half=half, k=k, k2=k2, j=j, jj=jj: e.matmul(bank(PC, half), lhsT=wT[k2][:, jj, :], rhs=vb[k][:, jj, half * 512:(half + 1) * 512], start=(j == 0), stop=(j == 127)), r=[bWT[k2], bVb[k]], w=[bC[half]])
                    yield

            def epilogue(gi):
                r0 = gi * 128
                ot_, bo_ = x1ts[gi % 2], bX1b[gi % 2]
                if do_peer:
                    for half in range(2):
                        hs = slice(half * 512, (half + 1) * 512)
                        E("dve", lambda e, half=half, hs=hs: e.tensor_tensor(out=junk[:, hs], in0=bank(PC, half), in1=gt2b[:, hs], op=ALU.mult), r=[bC[half], bGt2, bJunk], w=[bJunk])
                    E("pool", lambda e: e.tensor_tensor(out=ot_[:], in0=junk[:], in1=ot_[:], op=ALU.add), r=[bJunk, bo_], w=[bo_])
                E("dve", lambda e: e.memset(ssq[:], 0.0), w=[bSsq])
                E("act", lambda e: e.activation(out=junk[:], in_=ot_[:], func=AF.Square, accum_out=ssq[:, 0:1]), r=[bo_, bSsq], w=[bJunk, bSsq])
                E("dve", lambda e: e.tensor_scalar(out=ssq[:], in0=ssq[:], scalar1=1.0 / D, scalar2=EPS, op0=ALU.mult, op1=ALU.add), r=[bSsq], w=[bSsq])
                E("act", lambda e: e.activation(out=ssq[:], in_=ssq[:], func=AF.Sqrt), r=[bSsq], w=[bSsq])
                E("dve", lambda e: e.reciprocal(out=ssq[:], in_=ssq[:]), r=[bSsq], w=[bSsq])
                E("dve", lambda e: e.scalar_tensor_tensor(out=ot_[:], in0=ot_[:], scalar=ssq[:, 0:1], in1=fwb[:], op0=ALU.mult, op1=ALU.mult), r=[bo_, bSsq, bFw], w=[bo_])
                final_ops.append(S.dma("sp", lambda e: e.dma_start(out=out_d[r0:r0 + 128, :], in_=ot_[:]), bo_, reads=[bo_]))

            def run_all(g):
                for _ in g:
                    pass

            if not do_peer:
                for gi in range(ngroups):
                    r0 = gi * 128
                    S.dma("sp", lambda e, r0=r0, gi=gi: e.dma_start(out=x1ts[gi % 2][:], in_=x1s_d[r0:r0 + 128, :]), bX1b[gi % 2], reads=[bX1s[gi]], writes=[bX1b[gi % 2]])
                    epilogue(gi)
            else:
                run_all(front(0))
                tail(0)
                if dbg:
                    dump("h2T", h2Ts[0][:], [128, 8, 128], bH2Tb[0], BF16)
                for gi in range(ngroups):
                    nxt = front(gi + 1) if gi + 1 < ngroups else iter(())
                    for it, _ in enumerate(mainloop(gi)):
                        for _k in range(1 if it < 17 else 4):
                            next(nxt, None)
                    run_all(nxt)
                    epilogue(gi)
                    if gi + 1 < ngroups:
                        tail(gi + 1)

            S.finalize(final_ops + dumps)
            S.run()
    return nc


def _prep_shared(w_ada, b_ada, norm1_w, w_in, hgrn_lb_logits, hgrn_onorm_w, conv_w, conv_onorm_w, w_out,
                 norm2_w, peer_w_query, peer_sub_keys, peer_u, peer_v, final_norm_w):
    f = np.float32
    c = np.ascontiguousarray

    def pm(w):
        return c(np.asarray(w, f).reshape(8, 128, -1).transpose(1, 0, 2))
    sh = {}
    sh["wada"] = pm(w_ada[0])
    sh["bada"] = c(np.asarray(b_ada, f).reshape(1, -1))
    sh["n1w"] = c(np.asarray(norm1_w, f).reshape(1, -1))
    sh["n2w"] = c(np.asarray(norm2_w, f).reshape(1, -1))
    sh["fw"] = c(np.asarray(final_norm_w, f).reshape(1, -1))
    sh["win"] = pm(w_in[0])
    sh["lbl"] = c(np.asarray(hgrn_lb_logits, f).reshape(2, 4, 128).transpose(2, 0, 1))
    sh["onw"] = c(np.asarray(hgrn_onorm_w, f).reshape(4, 128).T)
    sh["cnw"] = c(np.asarray(conv_onorm_w, f).reshape(4, 128).T)
    sh["cw"] = c(np.asarray(conv_w, f).reshape(3, 4, 128).transpose(2, 0, 1))
    sh["wout"] = pm(w_out[0])
    sh["wq"] = c(np.asarray(peer_w_query[0], f).reshape(8, 128, 16, 128).transpose(2, 1, 0, 3))
    sh["skt"] = c(np.asarray(peer_sub_keys, f).reshape(16, 128, 128).transpose(2, 0, 1))
    sh["ut"] = c(np.asarray(peer_u, f).reshape(128, 128, 8, 128).transpose(0, 3, 2, 1))
    sh["v"] = c(np.asarray(peer_v, f).reshape(16384, 1024))
    return sh


def _in_maps(x, c, sh, npre):
    f = np.float32
    x = np.asarray(x, f); cc = np.asarray(c, f)
    maps = []
    for core in range(8):
        b, jj = core // 4, core % 4
        m = dict(sh)
        m["x"] = np.ascontiguousarray(x[b, jj * TOK:(jj + 1) * TOK])
        xpre = np.zeros((max(npre, 1) * TOK, D), f)
        msk = np.zeros((128, 4), f)
        for s in range(npre):
            src = jj - npre + s
            if src >= 0:
                xpre[s * TOK:(s + 1) * TOK] = x[b, src * TOK:(src + 1) * TOK]
                msk[:, s] = 1.0
        msk[:, 3] = 1.0 if (jj >= 1 and npre >= 1) else 0.0
        m["xpre"] = xpre
        m["msk"] = msk
        m["cT"] = np.ascontiguousarray(cc[b].reshape(8, 128).T)
        maps.append(m)
    return maps


_NC_CACHE = {}


def kernel(x, c, w_ada, b_ada, norm1_w, w_in, hgrn_lb_logits, hgrn_onorm_w, conv_w, conv_onorm_w, w_out,
           norm2_w, peer_w_query, peer_sub_keys, peer_u, peer_v, final_norm_w):
    sh = _prep_shared(w_ada, b_ada, norm1_w, w_in, hgrn_lb_logits, hgrn_onorm_w, conv_w, conv_onorm_w, w_out,
                      norm2_w, peer_w_query, peer_sub_keys, peer_u, peer_v, final_norm_w)
    maps = _in_maps(x, c, sh, NPRE)
    nc = build_nc(NPRE)
    res = run_bass_kernel_spmd(nc, maps, core_ids=list(range(8)))
    out = np.zeros((2, 8192, D), np.float32)
    for core in range(8):
        b, jj = core // 4, core % 4
        out[b, jj * TOK:(jj + 1) * TOK] = res.results[core]["out"]
    return out
```
